# Optimizing a Trainium2 kernel written in Bass

```python
import jax, jax.numpy as jnp
from jax import lax
import numpy as np

D_MODEL = 1024
BATCH = 8
SEQ = 2048
DEPTH = 4

CTX_LEN = 256
GRID_W = 64
N_MIXERS = 3
EPS = 1e-6

A_HEADS = 8
A_DK = 64
A_DV = 128
A_QK = A_HEADS * A_DK
A_V = A_HEADS * A_DV
A_IN = 2 * A_QK + 2 * A_V + 4 * A_HEADS
A_CHUNK = 128
A_CONV = 3
A_FORGET_BIAS = 3.0

B_HEADS = 4
B_DK = D_MODEL // 2 // B_HEADS
B_DV = D_MODEL // B_HEADS
B_QK = B_HEADS * B_DK
B_V = B_HEADS * B_DV
B_RANK = 16
B_TAU = 16.0
B_IN = 2 * B_QK + 2 * B_V + 2 * B_RANK
B_CHUNK = 64

C_HEADS = 16
C_DH = D_MODEL // C_HEADS
C_WIN_ROWS = 8
C_WIN_COLS = 16
C_QBLOCK = 16
C_KSPAN = C_QBLOCK + C_WIN_COLS

FFN_DIM = 2816
FFN_CONV = 3

N_A = (DEPTH + 2) // 3
N_B = (DEPTH + 1) // 3
N_C = DEPTH // 3

kernel_name = "hybrid_mlstm_gla_natten_dit"


def rmsnorm(x, g):
    x32 = x.astype(jnp.float32)
    y = x32 * lax.rsqrt(jnp.mean(x32 * x32, axis=-1, keepdims=True) + EPS)
    return (y * g.astype(jnp.float32)).astype(x.dtype)


def dwconv3(x, w, b):
    xp = jnp.pad(x, ((0, 0), (1, 1), (0, 0)))
    return xp[:, :-2] * w[0] + xp[:, 1:-1] * w[1] + xp[:, 2:] * w[2] + b


def to_heads(x, h):
    b, t, _ = x.shape
    return x.reshape(b, t, h, -1).transpose(0, 2, 1, 3)


def from_heads(x):
    b, h, t, d = x.shape
    return x.transpose(0, 2, 1, 3).reshape(b, t, h * d)


def chunk_seq(x, L):
    b, h, t = x.shape[:3]
    x = x.reshape(b, h, t // L, L, *x.shape[3:])
    return jnp.moveaxis(x, 2, 0)


def unchunk(x):
    x = jnp.moveaxis(x, 0, 2)
    return x.reshape(x.shape[0], x.shape[1], -1, *x.shape[4:])


def mlstm_scan(q, k, v, ig, lf, state):
    L = A_CHUNK
    causal = jnp.tril(jnp.ones((L, L), bool))

    def step(carry, inp):
        C, n, m = carry
        qc, kc, vc, ic, fc = inp
        g = jnp.cumsum(fc, axis=-1)
        logD = jnp.where(causal, g[..., :, None] - g[..., None, :] + ic[..., None, :], -jnp.inf)
        m_inter = g + m[..., None]
        m_row = jnp.maximum(jnp.max(logD, axis=-1), m_inter)
        S = jnp.einsum('bhjd,bhsd->bhjs', qc, kc) * jnp.exp(logD - m_row[..., None])
        inter = jnp.exp(m_inter - m_row)
        num = jnp.einsum('bhjs,bhse->bhje', S, vc) + inter[..., None] * jnp.einsum('bhjd,bhde->bhje', qc, C)
        den = jnp.sum(S, axis=-1) + inter * jnp.einsum('bhjd,bhd->bhj', qc, n)
        h = num / jnp.maximum(jnp.abs(den), jnp.exp(-m_row))[..., None]
        gL = g[..., -1]
        logw = gL[..., None] - g + ic
        m_new = jnp.maximum(gL + m, jnp.max(logw, axis=-1))
        w = jnp.exp(logw - m_new[..., None])
        decay = jnp.exp(gL + m - m_new)
        C_new = decay[..., None, None] * C + jnp.einsum('bhs,bhsd,bhse->bhde', w, kc, vc)
        n_new = decay[..., None] * n + jnp.einsum('bhs,bhsd->bhd', w, kc)
        return (C_new, n_new, m_new), h

    xs = tuple(chunk_seq(t, L) for t in (q, k, v, ig, lf))
    state, h = lax.scan(step, state, xs)
    return unchunk(h), state


def gla_scan(q, k, v, g, S):
    L = B_CHUNK
    causal = jnp.tril(jnp.ones((L, L), bool))[..., None]

    def step(S, inp):
        qc, kc, vc, gc = inp
        G = jnp.cumsum(gc, axis=2)
        Dm = jnp.exp(jnp.where(causal, G[:, :, :, None, :] - G[:, :, None, :, :], -jnp.inf))
        A = jnp.einsum('bhjd,bhsd,bhjsd->bhjs', qc, kc, Dm)
        o = jnp.einsum('bhjs,bhse->bhje', A, vc) + jnp.einsum('bhjd,bhde->bhje', qc * jnp.exp(G), S)
        GL = G[:, :, -1]
        S_new = jnp.exp(GL)[..., None] * S + jnp.einsum('bhsd,bhse->bhde', kc * jnp.exp(GL[:, :, None] - G), vc)
        return S_new, o

    xs = tuple(chunk_seq(t, L) for t in (q, k, v, g))
    S, o = lax.scan(step, S, xs)
    return unchunk(o), S


def bidir_scan(scan_fn, ctx_f, lat_f, ctx_b, lat_b, state0):
    rev = lambda xs: tuple(jnp.flip(t, 2) for t in xs)
    hc_f, st = scan_fn(*ctx_f, state0)
    hl_f, _ = scan_fn(*lat_f, st)
    hc_b, st = scan_fn(*rev(ctx_b), state0)
    hl_b, _ = scan_fn(*rev(lat_b), st)
    return hc_f + jnp.flip(hc_b, 2), hl_f + jnp.flip(hl_b, 2)


def mixer_mlstm(h_ctx, h_lat, w_in, b_gate, conv_w, conv_b, w_out, ctx_out):
    def project(h):
        p = h @ w_in
        qk, v, o, gates = jnp.split(p, [2 * A_QK, 2 * A_QK + A_V, 2 * A_QK + 2 * A_V], axis=-1)
        qk = jax.nn.silu(dwconv3(qk, conv_w, conv_b))
        q, k = jnp.split(qk, 2, axis=-1)
        q = to_heads(q, A_HEADS).astype(jnp.float32) * (A_DK ** -0.5)
        k = to_heads(k, A_HEADS).astype(jnp.float32)
        v = to_heads(v, A_HEADS).astype(jnp.float32)
        b, t, _ = gates.shape
        g = (gates + b_gate).astype(jnp.float32).reshape(b, t, 4, A_HEADS).transpose(2, 0, 3, 1)
        fwd = (q, k, v, g[0], jax.nn.log_sigmoid(g[1]))
        bwd = (q, k, v, g[2], jax.nn.log_sigmoid(g[3]))
        return fwd, bwd, o

    cf, cb, o_c = project(h_ctx)
    lf, lb, o_l = project(h_lat)
    bn = h_lat.shape[0]
    state0 = (jnp.zeros((bn, A_HEADS, A_DK, A_DV), jnp.float32),
              jnp.zeros((bn, A_HEADS, A_DK), jnp.float32),
              jnp.zeros((bn, A_HEADS), jnp.float32))
    hc, hl = bidir_scan(mlstm_scan, cf, lf, cb, lb, state0)

    def out(hh, o):
        return (from_heads(hh).astype(o.dtype) * jax.nn.sigmoid(o)) @ w_out

    return (out(hc, o_c) if ctx_out else None), out(hl, o_l)


def mixer_gla(h_ctx, h_lat, w_in, w_a2, b_a, norm_g, w_out, ctx_out):
    def project(h):
        p = h @ w_in
        q, k, v, r, a = jnp.split(p, [B_QK, 2 * B_QK, 2 * B_QK + B_V, 2 * B_QK + 2 * B_V], axis=-1)
        q = to_heads(q, B_HEADS).astype(jnp.float32) * (B_DK ** -0.5)
        k = to_heads(k, B_HEADS).astype(jnp.float32)
        v = to_heads(v, B_HEADS).astype(jnp.float32)
        a = a.astype(jnp.float32)

        def decay(d):
            z = a[..., d * B_RANK:(d + 1) * B_RANK] @ w_a2[d].astype(jnp.float32) + b_a[d].astype(jnp.float32)
            return to_heads(jax.nn.log_sigmoid(z) / B_TAU, B_HEADS)

        return (q, k, v, decay(0)), (q, k, v, decay(1)), r

    cf, cb, r_c = project(h_ctx)
    lf, lb, r_l = project(h_lat)
    bn = h_lat.shape[0]
    state0 = jnp.zeros((bn, B_HEADS, B_DK, B_DV), jnp.float32)
    oc, ol = bidir_scan(gla_scan, cf, lf, cb, lb, state0)

    def out(oo, r):
        oo = oo * lax.rsqrt(jnp.mean(oo * oo, axis=-1, keepdims=True) + EPS) * norm_g.astype(jnp.float32)
        return (from_heads(oo).astype(r.dtype) * jax.nn.silu(r)) @ w_out

    return (out(oc, r_c) if ctx_out else None), out(ol, r_l)


def mixer_na(h_ctx, h_lat, w_in, rpb, w_out, ctx_out):
    bn, T, _ = h_lat.shape
    rows = T // GRID_W
    kr = min(C_WIN_ROWS, rows)
    scale = C_DH ** -0.5
    q_l, k_l, v_l = [t.reshape(bn, T, C_HEADS, C_DH) for t in jnp.split(h_lat @ w_in, 3, axis=-1)]
    q_c, k_c, v_c = [t.reshape(bn, -1, C_HEADS, C_DH) for t in jnp.split(h_ctx @ w_in, 3, axis=-1)]
    q_l = q_l * scale
    q_c = q_c * scale

    n_cb = GRID_W // C_QBLOCK
    col_q = np.arange(GRID_W).reshape(n_cb, C_QBLOCK)
    k_start = np.clip(np.arange(n_cb) * C_QBLOCK - C_WIN_COLS // 2, 0, GRID_W - C_KSPAN)
    col_k = k_start[:, None] + np.arange(C_KSPAN)
    c_start = np.clip(col_q - C_WIN_COLS // 2, 0, GRID_W - C_WIN_COLS)
    col_valid = (col_k[:, None, :] >= c_start[:, :, None]) & (col_k[:, None, :] < c_start[:, :, None] + C_WIN_COLS)
    dc = np.clip(col_k[:, None, :] - col_q[:, :, None] + C_WIN_COLS - 1, 0, 2 * C_WIN_COLS - 2)

    q_g = q_l.reshape(bn, rows, n_cb, C_QBLOCK, C_HEADS, C_DH)
    k_cols = k_l.reshape(bn, rows, GRID_W, C_HEADS, C_DH)[:, :, col_k]
    v_cols = v_l.reshape(bn, rows, GRID_W, C_HEADS, C_DH)[:, :, col_k]
    n_loc = kr * C_KSPAN

    def row_fn(r):
        rs = jnp.clip(r - kr // 2, 0, rows - kr)
        qr = lax.dynamic_index_in_dim(q_g, r, axis=1, keepdims=False)
        kw = lax.dynamic_slice_in_dim(k_cols, rs, kr, axis=1)
        vw = lax.dynamic_slice_in_dim(v_cols, rs, kr, axis=1)
        s_loc = jnp.einsum('bnqhd,brnkhd->bhnqrk', qr, kw).astype(jnp.float32)
        dr = rs + jnp.arange(kr) - r + (C_WIN_ROWS - 1)
        bias = rpb[:, dr[None, None, :, None], dc[:, :, None, :]].astype(jnp.float32)
        s_loc = jnp.where(col_valid[:, :, None, :], s_loc + bias, -jnp.inf)
        s_ctx = jnp.einsum('bnqhd,bjhd->bhnqj', qr, k_c).astype(jnp.float32)
        s = jnp.concatenate([s_loc.reshape(bn, C_HEADS, n_cb, C_QBLOCK, n_loc), s_ctx], axis=-1)
        p = jax.nn.softmax(s, axis=-1).astype(vw.dtype)
        p_loc = p[..., :n_loc].reshape(bn, C_HEADS, n_cb, C_QBLOCK, kr, C_KSPAN)
        o = (jnp.einsum('bhnqrk,brnkhd->bnqhd', p_loc, vw)
             + jnp.einsum('bhnqj,bjhd->bnqhd', p[..., n_loc:], v_c))
        return o.reshape(bn, GRID_W, C_HEADS, C_DH)

    o = lax.map(row_fn, jnp.arange(rows))
    y_lat = jnp.moveaxis(o, 0, 1).reshape(bn, T, D_MODEL) @ w_out
    y_ctx = None
    if ctx_out:
        s = jnp.einsum('bihd,bjhd->bhij', q_c, k_c).astype(jnp.float32)
        p = jax.nn.softmax(s, axis=-1).astype(v_c.dtype)
        y_ctx = jnp.einsum('bhij,bjhd->bihd', p, v_c).reshape(bn, -1, D_MODEL) @ w_out
    return y_ctx, y_lat


def conv_ffn(h, w_up, conv_w, conv_b, w_down):
    u = dwconv3(h @ w_up, conv_w, conv_b)
    a, g = jnp.split(u, 2, axis=-1)
    return (jax.nn.silu(g) * a) @ w_down


def setup_inputs(seed: int = 0) -> dict:
    key = jax.random.key(seed)
    ks = jax.random.split(key, 32)
    D = D_MODEL
    nrm = lambda k, shape, s: jax.random.normal(k, shape, jnp.float32) * s
    forget_off = jnp.tile(jnp.repeat(jnp.array([0.0, A_FORGET_BIAS], jnp.float32), A_HEADS), 2)
    return {
        "x": nrm(ks[0], (BATCH, SEQ, D), 1.0),
        "c": nrm(ks[1], (BATCH, D), 1.0),
        "ctx": nrm(ks[2], (BATCH, CTX_LEN, D), 1.0),
        "c_ctx": nrm(ks[3], (D,), 1.0),
        "w_ada": nrm(ks[4], (DEPTH, D, 6 * D), 0.5 * D ** -0.5),
        "b_ada": nrm(ks[5], (DEPTH, 6 * D), 0.02),
        "norm_mix": 1.0 + nrm(ks[6], (DEPTH, D), 0.02),
        "norm_ffn": 1.0 + nrm(ks[7], (DEPTH, D), 0.02),
        "w_up": nrm(ks[8], (DEPTH, D, 2 * FFN_DIM), D ** -0.5),
        "ffn_conv_w": nrm(ks[9], (DEPTH, FFN_CONV, 2 * FFN_DIM), FFN_CONV ** -0.5),
        "ffn_conv_b": nrm(ks[10], (DEPTH, 2 * FFN_DIM), 0.02),
        "w_down": nrm(ks[11], (DEPTH, FFN_DIM, D), FFN_DIM ** -0.5),
        "a_w_in": nrm(ks[12], (N_A, D, A_IN), D ** -0.5),
        "a_b_gate": nrm(ks[13], (N_A, 4 * A_HEADS), 0.1) + forget_off,
        "a_conv_w": nrm(ks[14], (N_A, A_CONV, 2 * A_QK), A_CONV ** -0.5),
        "a_conv_b": nrm(ks[15], (N_A, 2 * A_QK), 0.02),
        "a_w_out": nrm(ks[16], (N_A, A_V, D), A_V ** -0.5),
        "b_w_in": nrm(ks[17], (N_B, D, B_IN), D ** -0.5),
        "b_w_a2": nrm(ks[18], (N_B, 2, B_RANK, B_QK), B_RANK ** -0.5),
        "b_b_a": nrm(ks[19], (N_B, 2, B_QK), 0.1),
        "b_norm": 1.0 + nrm(ks[20], (N_B, B_DV), 0.02),
        "b_w_out": nrm(ks[21], (N_B, B_V, D), B_V ** -0.5),
        "c_w_in": nrm(ks[22], (N_C, D, 3 * D), D ** -0.5),
        "c_rpb": nrm(ks[23], (N_C, C_HEADS, 2 * C_WIN_ROWS - 1, 2 * C_WIN_COLS - 1), 0.1),
        "c_w_out": nrm(ks[24], (N_C, D, D), D ** -0.5),
        "norm_final": 1.0 + nrm(ks[25], (D,), 0.02),
    }


def reference(x, c, ctx, c_ctx, w_ada, b_ada, norm_mix, norm_ffn, w_up, ffn_conv_w, ffn_conv_b, w_down,
              a_w_in, a_b_gate, a_conv_w, a_conv_b, a_w_out,
              b_w_in, b_w_a2, b_b_a, b_norm, b_w_out,
              c_w_in, c_rpb, c_w_out, norm_final):
    x_lat, x_ctx = x, ctx
    s_lat = jax.nn.silu(c)
    s_ctx = jax.nn.silu(c_ctx)
    for i in range(DEPTH):
        last = i == DEPTH - 1
        m_lat = (s_lat @ w_ada[i] + b_ada[i])[:, None, :]
        m_ctx = s_ctx @ w_ada[i] + b_ada[i]
        sh1, sc1, g1, sh2, sc2, g2 = jnp.split(m_lat, 6, axis=-1)
        csh1, csc1, cg1, csh2, csc2, cg2 = jnp.split(m_ctx, 6, axis=-1)
        h_lat = rmsnorm(x_lat, norm_mix[i]) * (1.0 + sc1) + sh1
        h_ctx = rmsnorm(x_ctx, norm_mix[i]) * (1.0 + csc1) + csh1
        kind, j = i % N_MIXERS, i // N_MIXERS
        if kind == 0:
            y_ctx, y_lat = mixer_mlstm(h_ctx, h_lat, a_w_in[j], a_b_gate[j], a_conv_w[j], a_conv_b[j], a_w_out[j], not last)
        elif kind == 1:
            y_ctx, y_lat = mixer_gla(h_ctx, h_lat, b_w_in[j], b_w_a2[j], b_b_a[j], b_norm[j], b_w_out[j], not last)
        else:
            y_ctx, y_lat = mixer_na(h_ctx, h_lat, c_w_in[j], c_rpb[j], c_w_out[j], not last)
        x_lat = x_lat + g1 * y_lat
        h_lat = rmsnorm(x_lat, norm_ffn[i]) * (1.0 + sc2) + sh2
        x_lat = x_lat + g2 * conv_ffn(h_lat, w_up[i], ffn_conv_w[i], ffn_conv_b[i], w_down[i])
        if not last:
            x_ctx = x_ctx + cg1 * y_ctx
            h_ctx = rmsnorm(x_ctx, norm_ffn[i]) * (1.0 + csc2) + csh2
            x_ctx = x_ctx + cg2 * conv_ffn(h_ctx, w_up[i], ffn_conv_w[i], ffn_conv_b[i], w_down[i])
    return rmsnorm(x_lat, norm_final)
```

```python
import os
import numpy as np
import concourse.bass as bass
import concourse.mybir as mybir
from concourse.bass_utils import run_bass_kernel_spmd

F32 = mybir.dt.float32
BF16 = mybir.dt.bfloat16
AF = mybir.ActivationFunctionType
ALU = mybir.AluOpType
AX = mybir.AxisListType

ERA = 6000
NDMASEM = 12
INF = 1 << 60

D = 1024
T = 2048
TCX = 256
NTOK = T + TCX
NT = NTOK // 128
DEPTH = 4
FF = 2816
NFC = FF // 128
EPS = 1e-6
NEG = -30000.0


class Buf:
    def __init__(self, t, name):
        self.t = t
        self.name = name
        self.writes = []
        self.reads = []

    def __getitem__(self, idx):
        return self.t[idx]

    def ap(self):
        return self.t.ap()


class Acc:
    def __init__(self, buf, lo=0, hi=INF):
        self.buf, self.lo, self.hi = buf, lo, hi


def R(buf, lo=0, hi=INF):
    return Acc(buf, lo, hi)


def _acc(a):
    return a if isinstance(a, Acc) else Acc(a)


class KB:
    def __init__(self):
        self.nc = bass.Bass("TRN2", target_bir_lowering=False)
        nc = self.nc
        self.eng = {"pe": nc.tensor, "act": nc.scalar, "dve": nc.vector, "pool": nc.gpsimd, "sp": nc.sync}
        self.cnt = {e: 0 for e in self.eng}
        self.sems = {e: [] for e in self.eng}
        self.waited = {e: {} for e in self.eng}
        self.dma_n = {e: 0 for e in self.eng}
        self.dma_sems = {e: [] for e in self.eng}
        self.last_tok = {e: None for e in self.eng}
        self.dma_tok = {e: {} for e in self.eng}
        self._ctx = []
        self.nsem = 0
        self.sem_ids = {}

    def _enter(self, cm):
        v = cm.__enter__()
        self._ctx.append(cm)
        return v

    def mark(self):
        return len(self._ctx)

    def release(self, m):
        while len(self._ctx) > m:
            self._ctx.pop().__exit__(None, None, None)

    def close(self):
        self.release(0)

    def sem(self, name):
        self.nsem += 1
        s = self._enter(self.nc.semaphore(self._uname(name)))
        self.sem_ids[id(s)] = s
        return s

    def _uname(self, name):
        self.uid = getattr(self, "uid", 0) + 1
        return f"{name}_{self.uid}"

    def sb(self, name, shape, dt):
        name = self._uname(name)
        return Buf(self._enter(self.nc.sbuf_tensor(name, list(shape), dt)), name)

    def ps(self, name, shape, dt=F32):
        name = self._uname(name)
        return Buf(self._enter(self.nc.psum_tensor(name, list(shape), dt)), name)

    def dram(self, name, shape, dt, kind="Internal"):
        return Buf(self.nc.dram_tensor(name, list(shape), dt, kind=kind), name)

    def _eng_token(self, e):
        n = self.cnt[e]
        era = n // ERA
        while len(self.sems[e]) <= era:
            self.sems[e].append(self.sem(f"s_{e}_{len(self.sems[e])}"))
        self.cnt[e] = n + 1
        return (self.sems[e][era], (n % ERA) + 1)

    def _wait(self, e, token):
        sem, val = token
        w = self.waited[e]
        key = id(sem)
        if w.get(key, 0) >= val:
            return
        w[key] = val
        self.eng[e].wait_ge(sem, val)

    def _deps(self, e, reads, writes, is_dma):
        toks = []
        for a in reads:
            a = _acc(a)
            for (lo, hi, tok, we) in a.buf.writes:
                if lo < a.hi and a.lo < hi:
                    toks.append(tok)
        for a in writes:
            a = _acc(a)
            for (lo, hi, tok, we) in a.buf.writes:
                if lo < a.hi and a.lo < hi and (is_dma or we != e):
                    toks.append(tok)
            for (lo, hi, tok, re) in a.buf.reads:
                if lo < a.hi and a.lo < hi and (is_dma or re != e):
                    toks.append(tok)
        for t in toks:
            self._wait(e, t)

    def _record(self, e, tok, reads, writes, is_dma):
        tag = ("dma", e) if is_dma else e
        for a in reads:
            a = _acc(a)
            b = a.buf
            if not is_dma:
                b.reads = [r for r in b.reads if not (r[3] == tag and r[0] >= a.lo and r[1] <= a.hi)]
            b.reads.append((a.lo, a.hi, tok, tag))
        for a in writes:
            a = _acc(a)
            b = a.buf
            b.writes = [w for w in b.writes if not (w[0] >= a.lo and w[1] <= a.hi)]
            b.reads = [r for r in b.reads if not (r[0] >= a.lo and r[1] <= a.hi)]
            b.writes.append((a.lo, a.hi, tok, tag))

    def op(self, e, fn, reads=(), writes=(), rg=0):
        if e == "pe":
            if rg != getattr(self, "pe_rg", 0) and self.last_tok["pe"] is not None:
                self._wait("pe", self.last_tok["pe"])
            self.pe_rg = rg
        self._deps(e, reads, writes, False)
        ins = fn()
        tok = self._eng_token(e)
        ins.then_inc(tok[0], 1)
        self.last_tok[e] = tok
        self._record(e, tok, reads, writes, False)
        return tok

    def dma(self, e, out_ap, in_ap, reads=(), writes=(), **kw):
        n = self.dma_n[e]
        if not self.dma_sems[e]:
            self.dma_sems[e] = [self.sem(f"d_{e}_{i}") for i in range(NDMASEM)]
        sem = self.dma_sems[e][n % NDMASEM]
        prev = 16 * (n // NDMASEM)
        if prev:
            self._wait(e, (sem, prev))
        self._deps(e, reads, writes, True)
        self.eng[e].dma_start(out=out_ap, in_=in_ap, **kw).then_inc(sem, 16)
        self.dma_n[e] = n + 1
        tok = (sem, prev + 16)
        assert prev + 16 < 32000, "dma sem overflow"
        self.dma_tok[e][n % NDMASEM] = tok
        self._record(e, tok, reads, writes, True)
        return tok

    def barrier(self):
        toks = [t for t in self.last_tok.values() if t is not None]
        for e in self.dma_tok:
            toks += list(self.dma_tok[e].values())
        for e in self.eng:
            for t in toks:
                self._wait(e, t)

    def finish(self):
        self.barrier()


def bc_last(ap, n):
    return bass.AP(ap.tensor, ap.offset, [list(d) for d in ap.ap] + [[0, n]])


def bc_mid(ap, pos, n):
    l = [list(d) for d in ap.ap]
    l.insert(pos, [0, n])
    return bass.AP(ap.tensor, ap.offset, l)


class MK:
    def __init__(self, nlayers=DEPTH, debug_x=False, stop=None, layer_list=None):
        self.stop = stop
        self.layer_list = list(range(nlayers)) if layer_list is None else layer_list
        self.k = KB()
        self.nc = self.k.nc
        self.nlayers = nlayers
        self.debug_x = debug_x
        self.build()

    def decl_inputs(self):
        k = self.k
        I = lambda n, s: k.dram(n, s, F32, kind="ExternalInput")
        self.x_in = I("x", [T, D])
        self.ctx_in = I("ctx", [TCX, D])
        self.c_t = I("c_t", [128, 8])
        self.cctx_t = I("cctx_t", [128, 8])
        self.w_ada = I("w_ada", [DEPTH, D, 6 * D])
        self.b_ada = I("b_ada", [DEPTH, 6 * D])
        self.norm_mix = I("norm_mix", [DEPTH, D])
        self.norm_ffn = I("norm_ffn", [DEPTH, D])
        self.w_up = I("w_up", [DEPTH, D, 2 * FF])
        self.ffn_cw = I("ffn_cw", [DEPTH, 128, 4 * 44])
        self.w_down = I("w_down", [DEPTH, FF, D])
        self.a_w_in = I("a_w_in", [2, D, 3104])
        self.a_b_gate = I("a_b_gate", [2, 32])
        self.a_cw = I("a_cw", [2, 128, 4 * 8])
        self.a_w_out = I("a_w_out", [2, D, D])
        self.b_w_in = I("b_w_in", [1, D, 3104])
        self.b_w_a2 = I("b_w_a2e", [1, 2, 17, 512])
        self.b_norm = I("b_norm", [1, 256])
        self.b_w_out = I("b_w_out", [1, D, D])
        self.c_w_in = I("c_w_in", [1, D, 3 * D])
        self.c_rpbx = I("c_rpbx", [1, 16, 64, 15 * 64])
        self.cmask_in = I("cmask", [128, 64])
        self.c_w_out = I("c_w_out", [1, D, D])
        self.norm_final = I("norm_final", [1, D])
        self.consts = I("consts", [6, 128, 128])
        self.out = k.dram("out", [T, D], F32, kind="ExternalOutput")
        self.xres = k.dram("xres", [NTOK, D], F32, kind="ExternalOutput" if self.debug_x else "Internal")

    def xt_ap(self, t):
        return self.xres.ap()[t * 128:(t + 1) * 128, :]

    def bcast_row_dma(self, dstbuf, row_ap_1xn, n):
        src = bass.AP(row_ap_1xn.tensor, row_ap_1xn.offset, [[0, 128], [1, n]])
        self.k.dma("sp", dstbuf[:], src, writes=[dstbuf])

    def load_w(self, dstbuf, dst_ap, src2d, rng=None):
        src = src2d.rearrange("(kc k) n -> k kc n", k=128)
        w = [dstbuf] if rng is None else [R(dstbuf, *rng)]
        return self.k.dma("pool", dst_ap, src, writes=w)

    def setup_consts(self):
        k, nc = self.k, self.nc
        self.cf = k.sb("cf", [128, 6, 128], F32)
        self.cb = k.sb("cb", [128, 6, 128], BF16)
        k.dma("sp", self.cf[:], self.consts.ap().rearrange("c p n -> p c n"), writes=[self.cf])
        k.dma("pool", self.cb[:], self.consts.ap().rearrange("c p n -> p c n"), writes=[self.cb])
        self.ident_b = self.cb[:, 0, :]
        self.triu_f = self.cf[:, 1, :]
        self.tril_f = self.cf[:, 2, :]
        self.ones_f = self.cf[:, 3, :]
        self.sbc = k.sb("sbc", [128, 2, 8, 128], BF16)
        cs = k.sb("cs", [128, 2, 8], F32)
        k.dma("sp", cs[:, 0, :], self.c_t.ap(), writes=[cs])
        k.dma("sp", cs[:, 1, :], self.cctx_t.ap(), writes=[cs])
        cs2 = k.sb("cs2", [128, 2, 8], F32)
        k.op("act", lambda: nc.scalar.activation(cs2[:], cs[:], AF.Silu), reads=[cs], writes=[cs2])
        k.op("dve", lambda: nc.vector.tensor_copy(self.sbc[:].rearrange("p a b m -> p (a b) m"),
                                                  bc_last(cs2[:].rearrange("p a b -> p (a b)"), 128)),
             reads=[cs2], writes=[self.sbc])
        self.hT = k.sb("hT", [128, 8, NTOK], BF16)
        self.mods = k.sb("mods", [128, 6, D], F32)
        self.ssz = k.sb("ssz", [128, 4], F32)

    def ada(self, i, phase):
        k, nc = self.k, self.nc
        m = k.mark()
        wbuf = [k.sb(f"adaw{j}", [128, 8, 512], BF16) for j in range(2)]
        bb = k.sb("adab", [128, 3 * D], F32)
        gn = k.sb("adag", [128, D], F32)
        pp = [k.ps(f"adap{j}", [128, 512], F32) for j in range(2)]
        c0 = phase * 3 * D
        self.bcast_row_dma(bb, self.b_ada.ap()[i:i + 1, c0:c0 + 3 * D], 3 * D)
        g_src = (self.norm_mix if phase == 0 else self.norm_ffn).ap()[i:i + 1, :]
        self.bcast_row_dma(gn, g_src, D)
        n = 0
        for blk in range(6):
            wb = wbuf[blk % 2]
            self.load_w(wb, wb[:], self.w_ada.ap()[i, :, c0 + blk * 512:c0 + (blk + 1) * 512])
            part, half = blk // 2, blk % 2
            for lc in range(2):
                p = pp[n % 2]
                n += 1
                for kc in range(8):
                    k.op("pe", lambda kc=kc, p=p, wb=wb, lc=lc: nc.tensor.matmul(
                        p[:], self.sbc[:, lc, kc, :], wb[:, kc, :], start=(kc == 0), stop=(kc == 7)),
                        reads=[self.sbc, wb], writes=[p])
                bsl = bb[:, blk * 512:(blk + 1) * 512]
                dst = self.mods[:, lc * 3 + (1, 0, 2)[part], half * 512:(half + 1) * 512]
                if part == 1:
                    tmp = gn[:, half * 512:(half + 1) * 512]
                    k.op("dve", lambda p=p, bsl=bsl, dst=dst: nc.vector.scalar_tensor_tensor(
                        dst, p[:], 1.0, bsl, ALU.add, ALU.add), reads=[p, bb], writes=[self.mods])
                    k.op("dve", lambda dst=dst, tmp=tmp: nc.vector.tensor_tensor(dst, dst, tmp, ALU.mult),
                         reads=[self.mods, gn], writes=[self.mods])
                else:
                    k.op("dve", lambda p=p, bsl=bsl, dst=dst: nc.vector.tensor_tensor(dst, p[:], bsl, ALU.add),
                         reads=[p, bb], writes=[self.mods])
        k.barrier()
        k.release(m)

    def norm_to_hT(self, tiles, src_fn):
        k, nc = self.k, self.nc
        m = k.mark()
        xt = [k.sb(f"nx{j}", [128, D], F32) for j in range(2)]
        junk = k.sb("njunk", [128, D], BF16)
        tmp = [k.sb(f"ntmp{j}", [128, D], F32) for j in range(2)]
        hb = [k.sb(f"nhb{j}", [128, D], BF16) for j in range(2)]
        st = [k.sb(f"nst{j}", [128, 4], F32) for j in range(2)]
        pt = [k.ps(f"npt{j}", [128, 8, 128], BF16) for j in range(2)]
        for n, t in enumerate(tiles):
            lc = 0 if t < 16 else 1
            x, s, tm, h, p = xt[n % 2], st[n % 2], tmp[n % 2], hb[n % 2], pt[n % 2]
            k.dma("sp", x[:], src_fn(t), reads=[R(self.xres, t, t + 1)], writes=[x])
            k.op("dve", lambda s=s: nc.vector.memset(s[:], 0.0), writes=[s])
            k.op("act", lambda x=x, s=s: nc.scalar.activation(junk[:], x[:], AF.Square, accum_out=s[:, 0:1]),
                 reads=[x, s], writes=[junk, s])
            k.op("act", lambda s=s: nc.scalar.activation(s[:, 1:2], s[:, 0:1], AF.Ln, scale=1.0 / D, bias=EPS),
                 reads=[s], writes=[s])
            k.op("act", lambda s=s: nc.scalar.activation(s[:, 2:3], s[:, 1:2], AF.Exp, scale=-0.5),
                 reads=[s], writes=[s])
            k.op("dve", lambda x=x, s=s, tm=tm, lc=lc: nc.vector.scalar_tensor_tensor(
                tm[:], x[:], s[:, 2:3], self.mods[:, lc * 3 + 0, :], ALU.mult, ALU.mult),
                reads=[x, s, self.mods], writes=[tm])
            k.op("dve", lambda tm=tm, h=h, lc=lc: nc.vector.tensor_tensor(h[:], tm[:], self.mods[:, lc * 3 + 1, :], ALU.add),
                 reads=[tm, self.mods], writes=[h])
            for kc in range(8):
                k.op("pe", lambda kc=kc, h=h, p=p: nc.tensor.transpose(p[:, kc, :], h[:, kc * 128:(kc + 1) * 128], self.ident_b),
                     reads=[h, self.cb], writes=[p])
            k.op("act", lambda p=p, t=t: nc.scalar.copy(self.hT[:, :, t * 128:(t + 1) * 128], p[:]),
                 reads=[p], writes=[R(self.hT, t * 128, (t + 1) * 128)])
        k.barrier()
        k.release(m)

    def resid_update(self, t, ypsum, xb, tb):
        k, nc = self.k, self.nc
        lc = 0 if t < 16 else 1
        k.dma("sp", xb[:], self.xt_ap(t), reads=[R(self.xres, t, t + 1)], writes=[xb])
        k.op("dve", lambda: nc.vector.tensor_tensor(tb[:], ypsum[:], self.mods[:, lc * 3 + 2, :], ALU.mult),
             reads=[ypsum, self.mods], writes=[tb])
        k.op("dve", lambda: nc.vector.tensor_tensor(xb[:], xb[:], tb[:], ALU.add), reads=[xb, tb], writes=[xb])
        k.dma("sp", self.xt_ap(t), xb[:], reads=[xb], writes=[R(self.xres, t, t + 1)])

    def conv_evac(self, dst_f32, p, cw, j, ncols, has_left, has_right, nout):
        k, nc = self.k, self.nc
        o = 1 if has_left else 0
        k.op("act", lambda: nc.scalar.activation(dst_f32[:, 0:nout], p[:, o:o + nout], AF.Identity,
                                                 scale=cw[:, 1, j:j + 1], bias=cw[:, 3, j:j + 1]),
             reads=[p, cw], writes=[dst_f32])
        lo = 0 if has_left else 1
        k.op("dve", lambda: nc.vector.scalar_tensor_tensor(dst_f32[:, lo:nout], p[:, o + lo - 1:o + nout - 1],
                                                           cw[:, 0, j:j + 1], dst_f32[:, lo:nout], ALU.mult, ALU.add),
             reads=[p, cw, dst_f32], writes=[dst_f32])
        hi = nout if has_right else nout - 1
        k.op("dve", lambda: nc.vector.scalar_tensor_tensor(dst_f32[:, 0:hi], p[:, o + 1:o + hi + 1],
                                                           cw[:, 2, j:j + 1], dst_f32[:, 0:hi], ALU.mult, ALU.add),
             reads=[p, cw, dst_f32], writes=[dst_f32])

    @staticmethod
    def tblocks(tiles_lat=True, tiles_ctx=True):
        bl = []
        if tiles_lat:
            bl += [(s, s + 256, 0, T) for s in range(0, T, 256)]
        if tiles_ctx:
            bl += [(T, T + 256, T, NTOK)]
        return bl

    def ffn(self, i, last):
        k, nc = self.k, self.nc
        m = k.mark()
        cw = k.sb("fcw", [128, 4, 44], F32)
        k.dma("sp", cw[:], self.ffn_cw.ap()[i].rearrange("p (a j) -> p a j", a=4), writes=[cw])
        wd = k.sb("fwd", [128, NFC, D], BF16)
        for q in range(4):
            j0, j1 = (0, 6, 12, 17, 22)[q], (0, 6, 12, 17, 22)[q + 1]
            self.load_w(wd, wd[:, j0:j1, :], self.w_down.ap()[i, j0 * 128:j1 * 128, :], rng=(j0, j1))
        wa = [k.sb(f"fwa{j}", [128, 8, 256], BF16) for j in range(2)]
        wg = [k.sb(f"fwg{j}", [128, 8, 256], BF16) for j in range(2)]
        actT = k.sb("factT", [128, NFC, 768], BF16)
        pa = [k.ps(f"fpa{j}", [128, 512], F32) for j in range(2)]
        pg = [k.ps(f"fpg{j}", [128, 512], F32) for j in range(2)]
        py = [k.ps(f"fpy{j}", [128, D], F32) for j in range(2)]
        ua = [k.sb(f"fua{j}", [128, 256], F32) for j in range(2)]
        ug = [k.sb(f"fug{j}", [128, 256], F32) for j in range(2)]
        xb = [k.sb(f"fxb{j}", [128, D], F32) for j in range(2)]
        tb1 = k.sb("ftb", [128, D], F32)
        tb = [tb1, tb1]
        groups = [(j, j + 2) for j in range(0, NFC, 2)]
        lb = self.tblocks(True, False)
        tgs = [lb[0:3], lb[3:6], lb[6:8] + ([] if last else self.tblocks(False, True))]
        cnt = 0
        gi = 0
        for tg in tgs:
            base = tg[0][0]
            for (j0, j1) in groups:
                wab, wgb = wa[gi % 2], wg[gi % 2]
                gi += 1
                nj = j1 - j0
                self.load_w(wab, wab[:, :, 0:nj * 128], self.w_up.ap()[i, :, j0 * 128:j1 * 128])
                self.load_w(wgb, wgb[:, :, 0:nj * 128], self.w_up.ap()[i, :, FF + j0 * 128:FF + j1 * 128])
                for (s, e, ss, se) in tg:
                    cs, ce = max(s - 1, ss), min(e + 1, se)
                    hl, hr = cs < s, ce > e
                    ncol = ce - cs
                    off = (s - base) if s < T else (512 + s - T)
                    for j in range(j0, j1):
                        A, G, UA, UG = pa[cnt % 2], pg[cnt % 2], ua[cnt % 2], ug[cnt % 2]
                        cnt += 1
                        for (P, W) in ((A, wab), (G, wgb)):
                            for kc in range(8):
                                k.op("pe", lambda P=P, W=W, kc=kc, j=j: nc.tensor.matmul(
                                    P[:, 0:ncol], W[:, kc, (j - j0) * 128:(j - j0 + 1) * 128], self.hT[:, kc, cs:ce],
                                    start=(kc == 0), stop=(kc == 7)),
                                    reads=[W, R(self.hT, cs, ce)], writes=[P])
                        self.conv_evac(UA, A, cw, j, ncol, hl, hr, 256)
                        self.conv_evac(UG, G, cw, 22 + j, ncol, hl, hr, 256)
                        k.op("act", lambda UG=UG: nc.scalar.activation(UG[:], UG[:], AF.Silu), reads=[UG], writes=[UG])
                        k.op("dve", lambda UA=UA, UG=UG, j=j, off=off: nc.vector.tensor_tensor(
                            actT[:, j, off:off + 256], UA[:], UG[:], ALU.mult),
                            reads=[UA, UG], writes=[R(actT, off, off + 256)])
            tiles = sorted(set(s // 128 + d for (s, e, _, _) in tg for d in (0, 1)))
            for n, t in enumerate(tiles):
                Y = py[n % 2]
                off = (t * 128 - base) if t < 16 else (512 + t * 128 - T)
                for nh in range(2):
                    for j in range(NFC):
                        k.op("pe", lambda Y=Y, j=j, nh=nh, off=off: nc.tensor.matmul(
                            Y[:, nh * 512:(nh + 1) * 512], actT[:, j, off:off + 128], wd[:, j, nh * 512:(nh + 1) * 512],
                            start=(j == 0), stop=(j == NFC - 1)),
                            reads=[R(actT, off, off + 128), R(wd, j, j + 1)], writes=[R(Y, nh * 512, (nh + 1) * 512)])
                self.resid_update(t, Y, xb[n % 2], tb[n % 2])
        k.barrier()
        k.release(m)

    def out_proj_tile(self, t, ysb, nkc, wout, py, yT, ptr, xb, tb):
        k, nc = self.k, self.nc
        for kc in range(nkc):
            k.op("pe", lambda kc=kc: nc.tensor.transpose(ptr[:, kc, :], ysb[:, kc * 128:(kc + 1) * 128], self.ident_b),
                 reads=[ysb, self.cb], writes=[ptr])
        k.op("act", lambda: nc.scalar.copy(yT[:, 0:nkc, :], ptr[:, 0:nkc, :]), reads=[ptr], writes=[yT])
        for nh in range(2):
            for kc in range(nkc):
                k.op("pe", lambda kc=kc, nh=nh: nc.tensor.matmul(
                    py[:, nh * 512:(nh + 1) * 512], yT[:, kc, :], wout[:, kc, nh * 512:(nh + 1) * 512],
                    start=(kc == 0), stop=(kc == nkc - 1)),
                    reads=[yT, wout], writes=[R(py, nh * 512, (nh + 1) * 512)])
        self.resid_update(t, py, xb, tb)

    def proj_tok(self, p_ap, pbuf, t, w, c0, n, prange=None):
        k, nc = self.k, self.nc
        for kc in range(8):
            k.op("pe", lambda kc=kc: nc.tensor.matmul(p_ap, self.hT[:, kc, t * 128:(t + 1) * 128], w[:, kc, c0:c0 + n],
                                                      start=(kc == 0), stop=(kc == 7)),
                 reads=[R(self.hT, t * 128, (t + 1) * 128), w], writes=[pbuf if prange is None else R(pbuf, *prange)])

    def mlstm(self, i, jm, last):
        k, nc = self.k, self.nc
        m0 = k.mark()
        W = self.a_w_in.ap()[jm]
        GT = k.sb("mGT", [128, NT, 32], F32)
        GP = k.sb("mGP", [128, NT, 3, 16], F32)
        bg = k.sb("mbg", [128, 32], F32)
        self.bcast_row_dma(bg, self.a_b_gate.ap()[jm:jm + 1, :], 32)
        cw = k.sb("mcw", [128, 4, 8], F32)
        k.dma("sp", cw[:], self.a_cw.ap()[jm].rearrange("p (a j) -> p a j", a=4), writes=[cw])
        fwd_order = [16, 17] + list(range(16))
        bwd_order = [17, 16] + list(range(15, -1, -1))
        for hg in range(2):
            m1 = k.mark()
            qkT = k.sb("mqkT", [128, 4, NTOK], BF16)
            V = k.sb("mV", [128, NT, 4, 130], BF16)
            Kt = k.sb("mKt", [128, NT, 256], BF16)
            H = k.sb("mH", [128, NT, 512], BF16)
            wq = k.sb("mwq", [128, 8, 512], BF16)
            wv = k.sb("mwv", [128, 8, 512], BF16)
            wo = k.sb("mwo", [128, 8, 512], BF16)
            wg_ = k.sb("mwg", [128, 8, 32], BF16)
            wout = k.sb("mwout", [128, 4, D], BF16)
            self.load_w(wq, wq[:, :, 0:256], W[:, hg * 256:(hg + 1) * 256], rng=(0, 256))
            self.load_w(wq, wq[:, :, 256:512], W[:, 512 + hg * 256:512 + (hg + 1) * 256], rng=(256, 512))
            self.load_w(wv, wv[:], W[:, 1024 + hg * 512:1024 + (hg + 1) * 512])
            self.load_w(wo, wo[:], W[:, 2048 + hg * 512:2048 + (hg + 1) * 512])
            self.load_w(wout, wout[:], self.a_w_out.ap()[jm, hg * 512:(hg + 1) * 512, :])
            if hg == 0:
                self.load_w(wg_, wg_[:], W[:, 3072:3104])
            k.op("dve", lambda: nc.vector.memset(V[:, :, :, 128:130], 1.0), writes=[V])
            m2 = k.mark()
            pq = [k.ps(f"mpq{j}", [128, 512], F32) for j in range(2)]
            uq = [k.sb(f"muq{j}", [128, 256], F32) for j in range(2)]
            cnt = 0
            for (s, e, ss, se) in self.tblocks():
                cs, ce = max(s - 1, ss), min(e + 1, se)
                hl, hr = cs < s, ce > e
                ncol = ce - cs
                for ch in range(4):
                    P, U = pq[cnt % 2], uq[cnt % 2]
                    cnt += 1
                    for kc in range(8):
                        k.op("pe", lambda P=P, kc=kc, ch=ch: nc.tensor.matmul(
                            P[:, 0:ncol], wq[:, kc, ch * 128:(ch + 1) * 128], self.hT[:, kc, cs:ce],
                            start=(kc == 0), stop=(kc == 7)), reads=[wq, R(self.hT, cs, ce)], writes=[P])
                    jcol = (ch // 2) * 4 + hg * 2 + (ch % 2)
                    self.conv_evac(U, P, cw, jcol, ncol, hl, hr, 256)
                    k.op("act", lambda U=U: nc.scalar.activation(U[:], U[:], AF.Silu), reads=[U], writes=[U])
                    sc = 0.125 if ch < 2 else 1.0
                    k.op("dve", lambda U=U, ch=ch, s=s, sc=sc: nc.vector.tensor_scalar(
                        qkT[:, ch, s:s + 256], U[:], sc, None, ALU.mult), reads=[U], writes=[R(qkT, s, s + 256)])
            k.barrier()
            if self.stop == "m_qk":
                self.dump("qkT", qkT, qkT[:], [128, 4, NTOK])
            self.chk("m_qk")
            k.release(m2)
            m2 = k.mark()
            pv = [k.ps(f"mpv{j}", [128, 512], F32) for j in range(2)]
            pgt = [k.ps(f"mpg{j}", [128, 512], F32) for j in range(2)]
            pk = [k.ps(f"mpk{j}", [128, 8, 128], BF16) for j in range(2)]
            for t in range(NT):
                P = pv[t % 2]
                self.proj_tok(P[:], P, t, wv, 0, 512)
                k.op("act", lambda P=P, t=t: nc.scalar.copy(V[:, t, :, 0:128], P[:].rearrange("p (h e) -> p h e", h=4)),
                     reads=[P], writes=[R(V, t, t + 1)])
                if hg == 0:
                    Pg = pgt[t % 2]
                    self.proj_tok(Pg[:, 0:32], Pg, t, wg_, 0, 32)
                    k.op("dve", lambda Pg=Pg, t=t: nc.vector.tensor_tensor(GT[:, t, :], Pg[:, 0:32], bg[:], ALU.add),
                         reads=[Pg, bg], writes=[R(GT, t, t + 1)])
                Pk = pk[t % 2]
                for c2 in range(2):
                    k.op("pe", lambda Pk=Pk, c2=c2, t=t: nc.tensor.transpose(
                        Pk[:, c2, :], qkT[:, 2 + c2, t * 128:(t + 1) * 128], self.ident_b),
                        reads=[R(qkT, t * 128, (t + 1) * 128), self.cb], writes=[Pk])
                k.op("act", lambda Pk=Pk, t=t: nc.scalar.copy(Kt[:, t, :].rearrange("p (a b) -> p a b", a=2), Pk[:, 0:2, :]),
                     reads=[Pk], writes=[R(Kt, t, t + 1)])
            k.barrier()
            if self.stop == "m_v":
                self.dump("V", V, V[:], [128, NT, 4, 130])
                self.dump("Kt", Kt, Kt[:], [128, NT, 256])
                self.dump("GT", GT, GT[:], [128, NT, 32])
            self.chk("m_v")
            k.release(m2)
            if hg == 0:
                m2 = k.mark()
                e1 = [k.sb(f"me1{j}", [128, 2, 8], F32) for j in range(2)]
                sp_ = [k.sb(f"msp{j}", [128, 2, 8], F32) for j in range(2)]
                aa = [k.sb(f"maa{j}", [128, 2, 8], F32) for j in range(2)]
                pgp = [k.ps(f"mpgp{j}", [128, 2, 256], F32) for j in range(2)]
                for t in range(NT):
                    E1, SP, A_, PG = e1[t % 2], sp_[t % 2], aa[t % 2], pgp[t % 2]
                    gv = GT[:, t, :].rearrange("p (d w h) -> p d w h", d=2, w=2)
                    k.op("act", lambda E1=E1, gv=gv: nc.scalar.activation(E1[:], gv[:, :, 1, :], AF.Exp, scale=-1.0),
                         reads=[R(GT, t, t + 1)], writes=[E1])
                    k.op("act", lambda E1=E1, SP=SP: nc.scalar.activation(SP[:], E1[:], AF.Ln, bias=1.0), reads=[E1], writes=[SP])
                    k.op("pe", lambda PG=PG, SP=SP: nc.tensor.matmul(PG[:, 0, 0:8], self.triu_f, SP[:, 0, :], start=True, stop=True),
                         reads=[self.cf, SP], writes=[R(PG, 0, 8)])
                    k.op("pe", lambda PG=PG, SP=SP: nc.tensor.matmul(PG[:, 0, 8:16], self.tril_f, SP[:, 1, :], start=True, stop=True),
                         reads=[self.cf, SP], writes=[R(PG, 8, 16)])
                    k.op("pe", lambda PG=PG, SP=SP: nc.tensor.matmul(PG[:, 1, 0:16], self.ones_f, SP[:].rearrange("p a b -> p (a b)"), start=True, stop=True),
                         reads=[self.cf, SP], writes=[R(PG, 16, 32)])
                    k.op("dve", lambda A_=A_, PG=PG, gv=gv: nc.vector.tensor_tensor(
                        A_[:], PG[:, 0, 0:16].rearrange("p (a b) -> p a b", a=2), gv[:, :, 0, :], ALU.add),
                        reads=[PG, R(GT, t, t + 1)], writes=[A_])
                    k.op("act", lambda A_=A_, t=t: nc.scalar.activation(GP[:, t, 0, :], A_[:].rearrange("p a b -> p (a b)"), AF.Exp),
                         reads=[A_], writes=[R(GP, t, t + 1)])
                    k.op("act", lambda PG=PG, t=t: nc.scalar.activation(GP[:, t, 1:3, :], PG[:, :, 0:16], AF.Exp, scale=-1.0),
                         reads=[PG], writes=[R(GP, t, t + 1)])
                k.barrier()
                if self.stop == "m_gate":
                    self.dump("GP", GP, GP[:], [128, NT, 3, 16])
                self.chk("m_gate")
                k.release(m2)
            m2 = k.mark()
            C32 = [k.sb(f"mC32{d}", [128, 2, 130], F32) for d in range(2)]
            Cb = [k.sb(f"mCb{d}", [128, 2, 130], BF16) for d in range(2)]
            Vp = [k.sb(f"mVp{d}", [128, 4, 130], BF16) for d in range(2)]
            ST = [k.sb(f"mST{d}", [128, 4, 128], BF16) for d in range(2)]
            dn = [k.sb(f"mdn{d}", [128, 4, 4], F32) for d in range(2)]
            hc = [k.sb(f"mhc{d}", [128, 512], F32) for d in range(2)]
            tmpC = [k.sb(f"mtC{d}", [128, 2, 130], F32) for d in range(2)]
            pqk = [k.ps(f"mpqk{d}", [128, 4, 128], F32) for d in range(2)]
            pnd = [k.ps(f"mpnd{d}", [128, 2, 512], F32) for d in range(2)]
            pdc1 = k.ps("mpdc", [128, 2, 512], F32)
            pdc = [pdc1, pdc1]
            for d in range(2):
                k.op("dve", lambda d=d: nc.vector.memset(C32[d][:], 0.0), writes=[C32[d]])
                k.op("dve", lambda d=d: nc.vector.memset(Cb[d][:], 0.0), writes=[Cb[d]])
            LVL = int(os.environ.get("SCAN_LVL", "9"))
            for step in range(NT):
                for d in range(2):
                    t = (fwd_order, bwd_order)[d][step]
                    mask = self.cf[:, 4 + d, :]
                    ea = GP[:, t, 0, d * 8 + hg * 4:d * 8 + hg * 4 + 4]
                    inter = GP[:, t, 1, d * 8 + hg * 4:d * 8 + hg * 4 + 4]
                    egl = GP[0:64, t, 2, d * 8 + hg * 4:d * 8 + hg * 4 + 4]
                    k.op("dve", lambda d=d, t=t, ea=ea: nc.vector.tensor_tensor(
                        Vp[d][:, :, 0:129], V[:, t, :, 0:129], bc_last(ea, 129), ALU.mult),
                        reads=[R(V, t, t + 1), R(GP, t, t + 1)], writes=[Vp[d]])
                    if LVL < 2:
                        continue
                    for h in (0, 2, 1, 3):
                        pb = (h % 2) * 64
                        k.op("pe", lambda d=d, h=h, pb=pb, t=t: nc.tensor.matmul(
                            pqk[d][:, h, :], qkT[pb:pb + 64, 2 + h // 2, t * 128:(t + 1) * 128],
                            qkT[pb:pb + 64, h // 2, t * 128:(t + 1) * 128], start=True, stop=True),
                            reads=[R(qkT, t * 128, (t + 1) * 128)], writes=[R(pqk[d], h, h + 1)], rg=pb)
                    k.op("dve", lambda d=d, mask=mask: nc.vector.tensor_tensor(
                        ST[d][:], pqk[d][:], bc_mid(mask, 1, 4), ALU.mult), reads=[pqk[d], self.cf], writes=[ST[d]])
                    if LVL < 3:
                        continue
                    def qc_mm(h, start, stop):
                        pb = (h % 2) * 64
                        o_ap = pnd[d][:, h // 2, (h % 2) * 130:(h % 2) * 130 + 129]
                        k.op("pe", lambda d=d, h=h, pb=pb, t=t, o_ap=o_ap: nc.tensor.matmul(
                            o_ap, qkT[pb:pb + 64, h // 2, t * 128:(t + 1) * 128], Cb[d][pb:pb + 64, h // 2, 0:129], start=start, stop=stop),
                            reads=[R(qkT, t * 128, (t + 1) * 128), Cb[d]], writes=[R(pnd[d], h, h + 1)], rg=pb)

                    def sv_mm(h, start, stop):
                        o_ap = pnd[d][:, h // 2, (h % 2) * 130:(h % 2) * 130 + 129]
                        k.op("pe", lambda d=d, h=h, o_ap=o_ap: nc.tensor.matmul(
                            o_ap, ST[d][:, h, :], Vp[d][:, h, 0:129], start=start, stop=stop),
                            reads=[ST[d], Vp[d]], writes=[R(pnd[d], h, h + 1)])
                    qc_mm(1, True, False)
                    qc_mm(3, True, False)
                    sv_mm(1, False, True)
                    sv_mm(3, False, True)
                    sv_mm(0, True, False)
                    sv_mm(2, True, False)
                    qc_mm(0, False, True)
                    qc_mm(2, False, True)
                    if self.stop == "m_scan" and step == 1 and d == 0 and self.debug_x and os.environ.get("DBG_PND"):
                        dbgt = k.sb("dbgt", [128, 2, 512], F32)
                        k.op("act", lambda: nc.scalar.copy(dbgt[:], pnd[d][:]), reads=[pnd[d]], writes=[dbgt])
                        self.dump("pnd", dbgt, dbgt[:], [128, 2, 512])
                        self.dump("ST", ST[d], ST[d][:], [128, 4, 128])
                        self.dump("Vp", Vp[d], Vp[d][:], [128, 4, 130])
                        self.dump("Cb", Cb[d], Cb[d][:], [128, 2, 130])
                    if LVL < 4:
                        continue
                    den_ap = bass.AP(pnd[d][:, 0, 128:129].tensor, pnd[d][:, 0, 128:129].offset, [list(pnd[d][:, 0, 128:129].ap[0]), [512, 2], [130, 2]])
                    k.op("dve", lambda d=d, den_ap=den_ap, inter=inter: nc.vector.tensor_tensor(
                        dn[d][:, 0, :].rearrange("p (a b) -> p a b", a=2), den_ap, inter.rearrange("p (a b) -> p a b", a=2), ALU.mult),
                        reads=[pnd[d], R(GP, t, t + 1)], writes=[R(dn[d], 0, 1)])
                    k.op("dve", lambda d=d: nc.vector.scalar_tensor_tensor(dn[d][:, 1, :], dn[d][:, 0, :], -1.0, dn[d][:, 0, :], ALU.mult, ALU.max),
                         reads=[R(dn[d], 0, 1)], writes=[R(dn[d], 1, 2)])
                    k.op("dve", lambda d=d: nc.vector.tensor_scalar_max(dn[d][:, 1, :], dn[d][:, 1, :], 1.0),
                         reads=[R(dn[d], 1, 2)], writes=[R(dn[d], 1, 2)])
                    k.op("dve", lambda d=d: nc.vector.reciprocal(dn[d][:, 2, :], dn[d][:, 1, :]),
                         reads=[R(dn[d], 1, 2)], writes=[R(dn[d], 2, 3)])
                    k.op("dve", lambda d=d, inter=inter: nc.vector.tensor_tensor(dn[d][:, 3, :], dn[d][:, 2, :], inter, ALU.mult),
                         reads=[R(dn[d], 2, 3), R(GP, t, t + 1)], writes=[R(dn[d], 3, 4)])
                    if LVL < 5:
                        continue
                    num_ap = bass.AP(pnd[d][:, 0, 0:1].tensor, pnd[d][:, 0, 0:1].offset, [list(pnd[d][:, 0, 0:1].ap[0]), [512, 2], [130, 2], [1, 128]])
                    rec_b = bc_last(dn[d][:, 3, :].rearrange("p (a b) -> p a b", a=2), 128)
                    sf, sb_ = fwd_order.index(t), bwd_order.index(t)
                    first = (sf <= sb_) if d == 0 else (sb_ < sf)
                    if first:
                        k.op("dve", lambda t=t, num_ap=num_ap, rec_b=rec_b: nc.vector.tensor_tensor(
                            H[:, t, :].rearrange("p (a b e) -> p a b e", a=2, b=2), num_ap, rec_b, ALU.mult),
                            reads=[pnd[d], R(dn[d], 3, 4)], writes=[R(H, t, t + 1)])
                    else:
                        k.op("dve", lambda d=d, num_ap=num_ap, rec_b=rec_b: nc.vector.tensor_tensor(
                            hc[d][:].rearrange("p (a b e) -> p a b e", a=2, b=2), num_ap, rec_b, ALU.mult),
                            reads=[pnd[d], R(dn[d], 3, 4)], writes=[hc[d]])
                        k.op("dve", lambda d=d, t=t: nc.vector.tensor_tensor(H[:, t, :], H[:, t, :], hc[d][:], ALU.add),
                             reads=[hc[d], R(H, t, t + 1)], writes=[R(H, t, t + 1)])
                    if LVL < 6:
                        continue
                    for h in (0, 2, 1, 3):
                        pb = (h % 2) * 64
                        o_ap = pdc[d][pb:pb + 64, h // 2, 0:129]
                        k.op("pe", lambda d=d, h=h, t=t, o_ap=o_ap, pb=pb: nc.tensor.matmul(
                            o_ap, Kt[:, t, h * 64:(h + 1) * 64], Vp[d][:, h, 0:129], start=True, stop=True,
                            tile_position=((0, pb) if pb else None)),
                            reads=[R(Kt, t, t + 1), Vp[d]], writes=[R(pdc[d], h, h + 1)], rg=("c", pb))
                    for hf in range(2):
                        pb = hf * 64
                        c0_ = d * 8 + hg * 4 + hf
                        eglh = GP[pb:pb + 64, t, 2, c0_:c0_ + 3:2]
                        k.op("dve", lambda d=d, pb=pb: nc.vector.tensor_tensor(
                            tmpC[d][pb:pb + 64, :, 0:129], pdc[d][pb:pb + 64, :, 0:129], C32[d][pb:pb + 64, :, 0:129], ALU.add),
                            reads=[pdc[d], C32[d]], writes=[R(tmpC[d], hf, hf + 1)])
                        k.op("dve", lambda d=d, pb=pb, eglh=eglh: nc.vector.tensor_tensor(
                            C32[d][pb:pb + 64, :, 0:129], tmpC[d][pb:pb + 64, :, 0:129], bc_last(eglh, 129), ALU.mult),
                            reads=[R(tmpC[d], hf, hf + 1), R(GP, t, t + 1)], writes=[C32[d]])
                    k.op("act", lambda d=d: nc.scalar.copy(Cb[d][:], C32[d][:]), reads=[C32[d]], writes=[Cb[d]])
            k.barrier()
            if self.stop == "m_scan":
                self.dump("H", H, H[:], [128, NT, 512])
                self.dump("qkT", qkT, qkT[:], [128, 4, NTOK])
                self.dump("V", V, V[:], [128, NT, 4, 130])
                self.dump("Kt", Kt, Kt[:], [128, NT, 256])
                self.dump("GT", GT, GT[:], [128, NT, 32])
                self.dump("GP", GP, GP[:], [128, NT, 3, 16])
                self.dump("ST", ST[0], ST[0][:], [128, 4, 128])
                self.dump("C32", C32[0], C32[0][:], [128, 2, 130])
                self.dump("Vp", Vp[0], Vp[0][:], [128, 4, 130])
            self.chk("m_scan")
            k.release(m2)
            m2 = k.mark()
            po = [k.ps(f"mpo{j}", [128, 512], F32) for j in range(2)]
            py = [k.ps(f"mpy{j}", [128, D], F32) for j in range(2)]
            ptr = [k.ps(f"mptr{j}", [128, 8, 128], BF16) for j in range(2)]
            sg = [k.sb(f"msg{j}", [128, 512], BF16) for j in range(2)]
            ysb = [k.sb(f"mys{j}", [128, 512], BF16) for j in range(2)]
            yT = [k.sb(f"myT{j}", [128, 4, 128], BF16) for j in range(2)]
            xb = [k.sb(f"mxb{j}", [128, D], F32) for j in range(2)]
            tb1 = k.sb("mtb", [128, D], F32)
            tb = [tb1, tb1]
            for n, t in enumerate(range(16 if last else NT)):
                b = n % 2
                self.proj_tok(po[b][:], po[b], t, wo, 0, 512)
                k.op("act", lambda b=b: nc.scalar.activation(sg[b][:], po[b][:], AF.Sigmoid), reads=[po[b]], writes=[sg[b]])
                k.op("dve", lambda b=b, t=t: nc.vector.tensor_tensor(ysb[b][:], H[:, t, :], sg[b][:], ALU.mult),
                     reads=[R(H, t, t + 1), sg[b]], writes=[ysb[b]])
                self.out_proj_tile(t, ysb[b], 4, wout, py[b], yT[b], ptr[b], xb[b], tb[b])
            k.barrier()
            k.release(m2)
            k.release(m1)
        k.release(m0)

    def proj_feat(self, p_ap, pbuf, w, c0, m, cs, ce):
        k, nc = self.k, self.nc
        for kc in range(8):
            k.op("pe", lambda kc=kc: nc.tensor.matmul(p_ap, w[:, kc, c0:c0 + m], self.hT[:, kc, cs:ce],
                                                      start=(kc == 0), stop=(kc == 7)),
                 reads=[w, R(self.hT, cs, ce)], writes=[pbuf])

    def gla(self, i, jm, last):
        k, nc = self.k, self.nc
        m0 = k.mark()
        W = self.b_w_in.ap()[jm]
        fwd_order = [16, 17] + list(range(16))
        bwd_order = [17, 16] + list(range(15, -1, -1))
        blocks = [(0, 512), (512, 1024), (1024, 1536), (1536, 2048), (2048, 2304)]
        gnb = k.sb("ggn", [128, 256], F32)
        self.bcast_row_dma(gnb, self.b_norm.ap()[jm:jm + 1, :], 256)
        wa2 = k.sb("gwa2", [17, 2, 512], F32)
        k.dma("sp", wa2[:], self.b_w_a2.ap()[jm].rearrange("d r c -> r d c"), writes=[wa2])
        for hg in range(2):
            m1 = k.mark()
            qT = k.sb("gqT", [128, 2, NTOK], BF16)
            kT = k.sb("gkT", [128, 2, NTOK], BF16)
            V = k.sb("gV", [128, NT, 512], BF16)
            SP = k.sb("gSP", [128, NT, 2, 256], F32)
            O = k.sb("gO", [128, NT, 512], BF16)
            m2 = k.mark()
            wqk = k.sb("gwqk", [128, 8, 512], BF16)
            wv = k.sb("gwv", [128, 8, 512], BF16)
            wa = k.sb("gwa", [128, 8, 32], BF16)
            aT = [k.sb(f"gaT{d}", [17, NTOK], F32) for d in range(2)]
            self.load_w(wqk, wqk[:, :, 0:256], W[:, hg * 256:(hg + 1) * 256], rng=(0, 256))
            self.load_w(wqk, wqk[:, :, 256:512], W[:, 512 + hg * 256:512 + (hg + 1) * 256], rng=(256, 512))
            self.load_w(wv, wv[:], W[:, 1024 + hg * 512:1024 + (hg + 1) * 512])
            self.load_w(wa, wa[:], W[:, 3072:3104])
            pq = [k.ps(f"gpq{j}", [128, 512], F32) for j in range(2)]
            pa_ = [k.ps(f"gpa{j}", [128, 512], F32) for j in range(2)]
            pz = [k.ps(f"gpz{j}", [128, 512], F32) for j in range(2)]
            e1 = [k.sb(f"ge1{j}", [128, 512], F32) for j in range(2)]
            for d in range(2):
                k.op("dve", lambda d=d: nc.vector.memset(aT[d][:], 1.0), writes=[aT[d]])
            cnt = 0
            for (cs, ce) in blocks:
                n = ce - cs
                for ch in range(4):
                    P = pq[cnt % 2]
                    cnt += 1
                    self.proj_feat(P[:, 0:n], P, wqk, ch * 128, 128, cs, ce)
                    if ch < 2:
                        k.op("act", lambda P=P, ch=ch: nc.scalar.activation(qT[:, ch, cs:ce], P[:, 0:n], AF.Copy, scale=128.0 ** -0.5),
                             reads=[P], writes=[R(qT, cs, ce)])
                    else:
                        k.op("dve", lambda P=P, ch=ch: nc.vector.tensor_copy(kT[:, ch - 2, cs:ce], P[:, 0:n]),
                             reads=[P], writes=[R(kT, cs, ce)])
                for d in range(2):
                    P = pa_[d]
                    self.proj_feat(P[0:16, 0:n], P, wa, d * 16, 16, cs, ce)
                    k.op("act", lambda P=P, d=d: nc.scalar.copy(aT[d][0:16, cs:ce], P[0:16, 0:n]), reads=[P], writes=[R(aT[d], cs, ce)])
            for t in range(NT):
                P = pq[t % 2]
                self.proj_tok(P[:], P, t, wv, 0, 512)
                k.op("act", lambda P=P, t=t: nc.scalar.copy(V[:, t, :], P[:]), reads=[P], writes=[R(V, t, t + 1)])
                for d in range(2):
                    Z = pz[d]
                    k.op("pe", lambda Z=Z, d=d, t=t: nc.tensor.matmul(Z[:, 0:256], aT[d][0:17, t * 128:(t + 1) * 128],
                                                                   wa2[0:17, d, hg * 256:(hg + 1) * 256], start=True, stop=True),
                         reads=[R(aT[d], t * 128, (t + 1) * 128), wa2], writes=[Z])
                    E = e1[d]
                    k.op("act", lambda Z=Z, E=E: nc.scalar.activation(E[:, 0:256], Z[:, 0:256], AF.Exp, scale=-1.0), reads=[Z], writes=[E])
                    k.op("act", lambda E=E: nc.scalar.activation(E[:, 256:512], E[:, 0:256], AF.Ln, bias=1.0), reads=[E], writes=[E])
                    k.op("dve", lambda E=E, d=d, t=t: nc.vector.tensor_scalar(SP[:, t, d, :], E[:, 256:512], 1.0 / 16.0, None, ALU.mult),
                         reads=[E], writes=[R(SP, t, t + 1)])
            k.barrier()
            k.release(m2)
            m2 = k.mark()
            S32 = [k.sb(f"gS32{d}", [128, 2, 256], F32) for d in range(2)]
            tS = [k.sb(f"gtS{d}", [128, 2, 256], F32) for d in range(2)]
            Sb = [k.sb(f"gSb{d}", [128, 2, 256], BF16) for d in range(2)]
            EG = [k.sb(f"gEG{d}", [128, 2, 128], F32) for d in range(2)]
            EN = [k.sb(f"gEN{d}", [128, 2, 128], F32) for d in range(2)]
            qt = [k.sb(f"gqt{d}", [128, 2, 128], BF16) for d in range(2)]
            kt = [k.sb(f"gkt{d}", [128, 2, 128], BF16) for d in range(2)]
            AT = [k.sb(f"gAT{d}", [128, 2, 128], BF16) for d in range(2)]
            Ktok = [k.sb(f"gKtok{d}", [128, 2, 128], BF16) for d in range(2)]
            pP = [k.ps(f"gpP{d}", [128, 4, 128], F32) for d in range(2)]
            pO = [k.ps(f"gpO{d}", [128, 2, 256], F32) for d in range(2)]
            pS = [k.ps(f"gpS{d}", [128, 2, 256], F32) for d in range(2)]
            pA = k.ps("gpA", [128, 4, 128], F32)
            pKt = k.ps("gpKt", [128, 8, 128], BF16)
            for d in range(2):
                k.op("dve", lambda d=d: nc.vector.memset(S32[d][:], 0.0), writes=[S32[d]])
                k.op("dve", lambda d=d: nc.vector.memset(Sb[d][:], 0.0), writes=[Sb[d]])
            for step in range(NT):
                for d in range(2):
                    t = (fwd_order, bwd_order)[d][step]
                    tri = self.cf[:, 1 + d, :]
                    mask = self.cf[:, 4 + d, :]
                    lastc = 127 if d == 0 else 0
                    tk = slice(t * 128, (t + 1) * 128)
                    for h in range(2):
                        k.op("pe", lambda d=d, h=h, t=t, tri=tri: nc.tensor.matmul(
                            pP[d][:, h, :], SP[:, t, d, h * 128:(h + 1) * 128], tri, start=True, stop=True),
                            reads=[R(SP, t, t + 1), self.cf], writes=[R(pP[d], h, h + 1)])
                    k.op("act", lambda d=d: nc.scalar.activation(EG[d][:], pP[d][:, 0:2, :], AF.Exp, scale=-1.0), reads=[pP[d]], writes=[EG[d]])
                    k.op("act", lambda d=d: nc.scalar.activation(EN[d][:], pP[d][:, 0:2, :], AF.Exp), reads=[pP[d]], writes=[EN[d]])
                    k.op("dve", lambda d=d, tk=tk: nc.vector.tensor_tensor(qt[d][:], qT[:, :, tk], EG[d][:], ALU.mult),
                         reads=[R(qT, t * 128, (t + 1) * 128), EG[d]], writes=[qt[d]])
                    k.op("dve", lambda d=d, tk=tk: nc.vector.tensor_tensor(kt[d][:], kT[:, :, tk], EN[d][:], ALU.mult),
                         reads=[R(kT, t * 128, (t + 1) * 128), EN[d]], writes=[kt[d]])
                    for h in range(2):
                        k.op("pe", lambda d=d, h=h: nc.tensor.matmul(pA[:, h, :], kt[d][:, h, :], qt[d][:, h, :], start=True, stop=True),
                             reads=[kt[d], qt[d]], writes=[R(pA, h, h + 1)])
                    for h in range(2):
                        k.op("pe", lambda d=d, h=h: nc.tensor.transpose(pKt[:, h, :], kt[d][:, h, :], self.ident_b),
                             reads=[kt[d], self.cb], writes=[R(pKt, h, h + 1)])
                    k.op("dve", lambda d=d, mask=mask: nc.vector.tensor_tensor(AT[d][:], pA[:, 0:2, :], bc_mid(mask, 1, 2), ALU.mult),
                         reads=[pA, self.cf], writes=[AT[d]])
                    k.op("act", lambda d=d: nc.scalar.copy(Ktok[d][:], pKt[:, 0:2, :]), reads=[pKt], writes=[Ktok[d]])
                    for h in range(2):
                        k.op("pe", lambda d=d, h=h, t=t: nc.tensor.matmul(pO[d][:, h, :], AT[d][:, h, :], V[:, t, h * 256:(h + 1) * 256],
                                                                      start=True, stop=False),
                             reads=[AT[d], R(V, t, t + 1)], writes=[R(pO[d], h, h + 1)])
                        k.op("pe", lambda d=d, h=h: nc.tensor.matmul(pO[d][:, h, :], qt[d][:, h, :], Sb[d][:, h, :], start=False, stop=True),
                             reads=[qt[d], Sb[d]], writes=[R(pO[d], h, h + 1)])
                    for h in range(2):
                        k.op("pe", lambda d=d, h=h, t=t: nc.tensor.matmul(pS[d][:, h, :], Ktok[d][:, h, :], V[:, t, h * 256:(h + 1) * 256],
                                                                      start=True, stop=True),
                             reads=[Ktok[d], R(V, t, t + 1)], writes=[R(pS[d], h, h + 1)])
                    sf, sb_ = fwd_order.index(t), bwd_order.index(t)
                    first = (sf <= sb_) if d == 0 else (sb_ < sf)
                    if first:
                        k.op("dve", lambda d=d, t=t: nc.vector.tensor_copy(O[:, t, :], pO[d][:].rearrange("p a b -> p (a b)")),
                             reads=[pO[d]], writes=[R(O, t, t + 1)])
                    else:
                        k.op("dve", lambda d=d, t=t: nc.vector.tensor_tensor(O[:, t, :], pO[d][:].rearrange("p a b -> p (a b)"), O[:, t, :], ALU.add),
                             reads=[pO[d], R(O, t, t + 1)], writes=[R(O, t, t + 1)])
                    k.op("dve", lambda d=d: nc.vector.tensor_tensor(tS[d][:], pS[d][:], S32[d][:], ALU.add),
                         reads=[pS[d], S32[d]], writes=[tS[d]])
                    k.op("dve", lambda d=d, lastc=lastc: nc.vector.tensor_tensor(S32[d][:], tS[d][:], bc_last(EG[d][:, :, lastc], 256), ALU.mult),
                         reads=[tS[d], EG[d]], writes=[S32[d]])
                    k.op("act", lambda d=d: nc.scalar.copy(Sb[d][:], S32[d][:]), reads=[S32[d]], writes=[Sb[d]])
            k.barrier()
            if self.stop == "g_scan":
                self.dump("O", O, O[:], [128, NT, 512])
            self.chk("g_scan")
            k.release(m2)
            m2 = k.mark()
            wr = k.sb("gwr", [128, 8, 512], BF16)
            wout = k.sb("gwout", [128, 4, D], BF16)
            self.load_w(wr, wr[:], W[:, 2048 + hg * 512:2048 + (hg + 1) * 512])
            self.load_w(wout, wout[:], self.b_w_out.ap()[jm, hg * 512:(hg + 1) * 512, :])
            pr = [k.ps(f"gpr{j}", [128, 512], F32) for j in range(2)]
            py = [k.ps(f"gpy{j}", [128, D], F32) for j in range(2)]
            ptr = [k.ps(f"gptr{j}", [128, 8, 128], BF16) for j in range(2)]
            sr = [k.sb(f"gsr{j}", [128, 512], BF16) for j in range(2)]
            y32 = [k.sb(f"gy32{j}", [128, 2, 256], F32) for j in range(2)]
            ysb = [k.sb(f"gys{j}", [128, 512], BF16) for j in range(2)]
            yT = [k.sb(f"gyT{j}", [128, 4, 128], BF16) for j in range(2)]
            xb = [k.sb(f"gxb{j}", [128, D], F32) for j in range(2)]
            tb1 = k.sb("gtb", [128, D], F32)
            st = [k.sb(f"gst{j}", [128, 8], F32) for j in range(2)]
            junk = k.sb("gjunk", [128, 256], BF16)
            for n, t in enumerate(range(16 if last else NT)):
                b = n % 2
                self.proj_tok(pr[b][:], pr[b], t, wr, 0, 512)
                k.op("act", lambda b=b: nc.scalar.activation(sr[b][:], pr[b][:], AF.Silu), reads=[pr[b]], writes=[sr[b]])
                k.op("dve", lambda b=b: nc.vector.memset(st[b][:], 0.0), writes=[st[b]])
                for h in range(2):
                    k.op("act", lambda b=b, h=h, t=t: nc.scalar.activation(junk[:], O[:, t, h * 256:(h + 1) * 256], AF.Square, accum_out=st[b][:, h:h + 1]),
                         reads=[R(O, t, t + 1), st[b]], writes=[junk, st[b]])
                k.op("act", lambda b=b: nc.scalar.activation(st[b][:, 2:4], st[b][:, 0:2], AF.Ln, scale=1.0 / 256, bias=EPS), reads=[st[b]], writes=[st[b]])
                k.op("act", lambda b=b: nc.scalar.activation(st[b][:, 4:6], st[b][:, 2:4], AF.Exp, scale=-0.5), reads=[st[b]], writes=[st[b]])
                k.op("dve", lambda b=b, t=t: nc.vector.tensor_tensor(y32[b][:], O[:, t, :].rearrange("p (a e) -> p a e", a=2),
                                                                   bc_last(st[b][:, 4:6], 256), ALU.mult),
                     reads=[R(O, t, t + 1), st[b]], writes=[y32[b]])
                k.op("dve", lambda b=b: nc.vector.tensor_tensor(y32[b][:], y32[b][:], bc_mid(gnb[:], 1, 2), ALU.mult),
                     reads=[y32[b], gnb], writes=[y32[b]])
                k.op("dve", lambda b=b: nc.vector.tensor_tensor(ysb[b][:], y32[b][:].rearrange("p a e -> p (a e)"), sr[b][:], ALU.mult),
                     reads=[y32[b], sr[b]], writes=[ysb[b]])
                self.out_proj_tile(t, ysb[b], 4, wout, py[b], yT[b], ptr[b], xb[b], tb1)
            k.barrier()
            k.release(m2)
            k.release(m1)
        k.release(m0)

    def na(self, i, jm, last):
        k, nc = self.k, self.nc
        m0 = k.mark()
        W = self.c_w_in.ap()[jm]
        blocks = [(0, 512), (512, 1024), (1024, 1536), (1536, 2048), (2048, 2304)]
        cmask = k.sb("ncm", [128, 64], F32)
        k.dma("sp", cmask[:], self.cmask_in.ap(), writes=[cmask])
        for hg in range(2):
            m1 = k.mark()
            qT = k.sb("nqT", [128, 4, NTOK], BF16)
            kT = k.sb("nkT", [128, 4, NTOK], BF16)
            V = k.sb("nV", [128, NT, 512], BF16)
            BM = k.sb("nBM", [128, 8, 15, 64], BF16)
            src = self.c_rpbx.ap()[jm, hg * 8:(hg + 1) * 8].rearrange("h q (r c) -> q h r c", r=15)
            k.dma("pool", BM[0:64], src, writes=[R(BM, 0, 1)])
            k.dma("pool", BM[64:128], src, writes=[R(BM, 1, 2)])
            k.op("dve", lambda: nc.vector.tensor_tensor(BM[:].rearrange("p h r c -> p (h r) c"), BM[:].rearrange("p h r c -> p (h r) c"),
                                                       bc_mid(cmask[:], 1, 120), ALU.add), reads=[BM, cmask], writes=[BM])
            m2 = k.mark()
            wq = k.sb("nwq", [128, 8, 512], BF16)
            wk = k.sb("nwk", [128, 8, 512], BF16)
            wv = k.sb("nwv", [128, 8, 512], BF16)
            self.load_w(wq, wq[:], W[:, hg * 512:(hg + 1) * 512])
            self.load_w(wk, wk[:], W[:, D + hg * 512:D + (hg + 1) * 512])
            self.load_w(wv, wv[:], W[:, 2 * D + hg * 512:2 * D + (hg + 1) * 512])
            pq = [k.ps(f"npq{j}", [128, 512], F32) for j in range(2)]
            cnt = 0
            for (cs, ce) in blocks:
                n = ce - cs
                for ch in range(8):
                    P = pq[cnt % 2]
                    cnt += 1
                    self.proj_feat(P[:, 0:n], P, wq if ch < 4 else wk, (ch % 4) * 128, 128, cs, ce)
                    if ch < 4:
                        k.op("act", lambda P=P, ch=ch: nc.scalar.activation(qT[:, ch, cs:ce], P[:, 0:n], AF.Copy, scale=0.125),
                             reads=[P], writes=[R(qT, cs, ce)])
                    else:
                        k.op("dve", lambda P=P, ch=ch: nc.vector.tensor_copy(kT[:, ch - 4, cs:ce], P[:, 0:n]),
                             reads=[P], writes=[R(kT, cs, ce)])
            for t in range(NT):
                P = pq[t % 2]
                self.proj_tok(P[:], P, t, wv, 0, 512)
                k.op("act", lambda P=P, t=t: nc.scalar.copy(V[:, t, :], P[:]), reads=[P], writes=[R(V, t, t + 1)])
            k.barrier()
            k.release(m2)
            m2 = k.mark()
            wout = k.sb("nwout", [128, 4, D], BF16)
            self.load_w(wout, wout[:], self.c_w_out.ap()[jm, hg * 512:(hg + 1) * 512, :])
            Sc = [k.sb(f"nSc{j}", [128, 896], F32) for j in range(2)]
            Pb = [k.sb(f"nPb{j}", [128, 896], BF16) for j in range(2)]
            PT = [k.sb(f"nPT{j}", [128, 7, 128], BF16) for j in range(2)]
            st = [k.sb(f"nst{j}", [128, 4], F32) for j in range(2)]
            pS = [k.ps(f"npS{j}", [128, 1024], F32) for j in range(2)]
            pT = [k.ps(f"npT{j}", [128, 8, 128], BF16) for j in range(1)]
            pO = k.ps("npO", [128, 8, 64], F32)
            py = [k.ps(f"npy{j}", [128, D], F32) for j in range(1)]
            ptr = pT
            ysb = [k.sb(f"nys{j}", [128, 512], BF16) for j in range(2)]
            yT = [k.sb(f"nyT{j}", [128, 4, 128], BF16) for j in range(2)]
            xb = [k.sb(f"nxb{j}", [128, D], F32) for j in range(2)]
            tb1 = k.sb("ntb", [128, D], F32)
            cnt = 0
            for n_, t in enumerate(range(16 if last else NT)):
                if t < 16:
                    rs = [min(max(2 * t - 4, 0), 24), min(max(2 * t - 3, 0), 24)]
                    ks = rs[0]
                    nl = 4 if t in (0, 1, 14, 15) else 5
                    c_lo = 0
                    chunks = list(range(nl)) + [5, 6]
                    for sb_ in Sc:
                        for hf in range(2):
                            i0 = rs[hf] - ks
                            if i0 > 0:
                                k.op("dve", lambda sb_=sb_, hf=hf, i0=i0: nc.vector.memset(sb_[hf * 64:(hf + 1) * 64, 0:i0 * 64], NEG), writes=[sb_])
                            if (i0 + 8) * 64 < 640:
                                k.op("dve", lambda sb_=sb_, hf=hf, i0=i0: nc.vector.memset(sb_[hf * 64:(hf + 1) * 64, (i0 + 8) * 64:640], NEG), writes=[sb_])
                else:
                    nl = 0
                    c_lo = 640
                    chunks = [5, 6]
                for h in range(8):
                    b = cnt % 2
                    cnt += 1
                    pb = (h % 2) * 64
                    c = h // 2
                    S_, P_, PT_, s_ = Sc[b], Pb[b], PT[b], st[b]
                    ps_ = pS[b]
                    qa = qT[pb:pb + 64, c, t * 128:(t + 1) * 128]
                    if nl:
                        k0 = ks * 64
                        k.op("pe", lambda ps_=ps_, qa=qa, pb=pb, c=c, k0=k0: nc.tensor.matmul(
                            ps_[:, 0:512], qa, kT[pb:pb + 64, c, k0:k0 + 512], start=True, stop=True),
                            reads=[R(qT, t * 128, (t + 1) * 128), R(kT, k0, k0 + 512)], writes=[R(ps_, 0, 512)], rg=pb)
                        if nl == 5:
                            k.op("pe", lambda ps_=ps_, qa=qa, pb=pb, c=c, k0=k0: nc.tensor.matmul(
                                ps_[:, 512:640], qa, kT[pb:pb + 64, c, k0 + 512:k0 + 640], start=True, stop=True),
                                reads=[R(qT, t * 128, (t + 1) * 128), R(kT, k0 + 512, k0 + 640)], writes=[R(ps_, 512, 640)], rg=pb)
                    k.op("pe", lambda ps_=ps_, qa=qa, pb=pb, c=c: nc.tensor.matmul(
                        ps_[:, 640:896], qa, kT[pb:pb + 64, c, T:NTOK], start=True, stop=True),
                        reads=[R(qT, t * 128, (t + 1) * 128), R(kT, T, NTOK)], writes=[R(ps_, 640, 896)], rg=pb)
                    if nl:
                        for hf in range(2):
                            r = 2 * t + hf
                            i0 = rs[hf] - ks
                            dr0 = rs[hf] - r + 7
                            k.op("dve", lambda S_=S_, ps_=ps_, hf=hf, i0=i0, dr0=dr0, h=h: nc.vector.tensor_tensor(
                                S_[hf * 64:(hf + 1) * 64, i0 * 64:(i0 + 8) * 64], ps_[hf * 64:(hf + 1) * 64, i0 * 64:(i0 + 8) * 64],
                                BM[hf * 64:(hf + 1) * 64, h, dr0:dr0 + 8, :].rearrange("p r c -> p (r c)"), ALU.add),
                                reads=[ps_, BM], writes=[S_])
                    k.op("act", lambda S_=S_, ps_=ps_: nc.scalar.copy(S_[:, 640:896], ps_[:, 640:896]), reads=[ps_], writes=[S_])
                    k.op("dve", lambda s_=s_: nc.vector.memset(s_[:], 0.0), writes=[s_])
                    k.op("dve", lambda S_=S_, s_=s_, c_lo=c_lo: nc.vector.tensor_reduce(s_[:, 0:1], S_[:, c_lo:896], AX.X, ALU.max, negate=True),
                         reads=[S_], writes=[s_])
                    k.op("act", lambda S_=S_, P_=P_, s_=s_, c_lo=c_lo: nc.scalar.activation(
                        P_[:, c_lo:896], S_[:, c_lo:896], AF.Exp, bias=s_[:, 0:1], accum_out=s_[:, 1:2]),
                        reads=[S_, s_], writes=[P_, s_])
                    pT_ = pT[0]
                    for cc in chunks:
                        k.op("pe", lambda P_=P_, pT_=pT_, cc=cc: nc.tensor.transpose(pT_[:, cc, :], P_[:, cc * 128:(cc + 1) * 128], self.ident_b),
                             reads=[P_, self.cb], writes=[R(pT_, cc, cc + 1)])
                    if nl:
                        k.op("act", lambda PT_=PT_, pT_=pT_, nl=nl: nc.scalar.copy(PT_[:, 0:nl, :], pT_[:, 0:nl, :]), reads=[pT_], writes=[PT_])
                    k.op("act", lambda PT_=PT_, pT_=pT_: nc.scalar.copy(PT_[:, 5:7, :], pT_[:, 5:7, :]), reads=[pT_], writes=[PT_])
                    for n2, cc in enumerate(chunks):
                        vt = (ks // 2 + cc) if cc < 5 else (16 + cc - 5)
                        k.op("pe", lambda PT_=PT_, cc=cc, vt=vt, h=h, n2=n2: nc.tensor.matmul(
                            pO[:, h, :], PT_[:, cc, :], V[:, vt, h * 64:(h + 1) * 64], start=(n2 == 0), stop=(n2 == len(chunks) - 1)),
                            reads=[PT_, R(V, vt, vt + 1)], writes=[R(pO, h, h + 1)])
                    k.op("dve", lambda s_=s_: nc.vector.reciprocal(s_[:, 2:3], s_[:, 1:2]), reads=[s_], writes=[s_])
                    yb = ysb[n_ % 2]
                    k.op("dve", lambda s_=s_, yb=yb, h=h: nc.vector.tensor_scalar(yb[:, h * 64:(h + 1) * 64], pO[:, h, :], s_[:, 2:3], None, ALU.mult),
                         reads=[R(pO, h, h + 1), s_], writes=[yb])
                self.out_proj_tile(t, ysb[n_ % 2], 4, wout, py[0], yT[n_ % 2], ptr[0], xb[n_ % 2], tb1)
            k.barrier()
            k.release(m2)
            k.release(m1)
        k.release(m0)

    def final_norm(self):
        k, nc = self.k, self.nc
        m = k.mark()
        gn = k.sb("fng", [128, D], F32)
        self.bcast_row_dma(gn, self.norm_final.ap()[0:1, :], D)
        xt = [k.sb(f"fx{j}", [128, D], F32) for j in range(2)]
        yt = [k.sb(f"fy{j}", [128, D], F32) for j in range(2)]
        junk = k.sb("fjunk", [128, D], BF16)
        st = [k.sb(f"fst{j}", [128, 4], F32) for j in range(2)]
        for t in range(16):
            x, y, s = xt[t % 2], yt[t % 2], st[t % 2]
            k.dma("sp", x[:], self.xt_ap(t), reads=[R(self.xres, t, t + 1)], writes=[x])
            k.op("dve", lambda s=s: nc.vector.memset(s[:], 0.0), writes=[s])
            k.op("act", lambda x=x, s=s: nc.scalar.activation(junk[:], x[:], AF.Square, accum_out=s[:, 0:1]),
                 reads=[x, s], writes=[junk, s])
            k.op("act", lambda s=s: nc.scalar.activation(s[:, 1:2], s[:, 0:1], AF.Ln, scale=1.0 / D, bias=EPS), reads=[s], writes=[s])
            k.op("act", lambda s=s: nc.scalar.activation(s[:, 2:3], s[:, 1:2], AF.Exp, scale=-0.5), reads=[s], writes=[s])
            k.op("dve", lambda x=x, y=y, s=s: nc.vector.scalar_tensor_tensor(y[:], x[:], s[:, 2:3], gn[:], ALU.mult, ALU.mult),
                 reads=[x, s, gn], writes=[y])
            k.dma("sp", self.out.ap()[t * 128:(t + 1) * 128, :], y[:], reads=[y], writes=[R(self.out, t, t + 1)])
        k.barrier()
        k.release(m)

    def build(self):
        k = self.k
        self.decl_inputs()
        self.setup_consts()
        for t in range(16):
            k.dma("sp", self.xres.ap()[t * 128:(t + 1) * 128, :], self.x_in.ap()[t * 128:(t + 1) * 128, :], writes=[R(self.xres, t, t + 1)])
        for t in range(2):
            k.dma("sp", self.xres.ap()[T + t * 128:T + (t + 1) * 128, :], self.ctx_in.ap()[t * 128:(t + 1) * 128, :], writes=[R(self.xres, 16 + t, 17 + t)])
        try:
            self.layers()
        except StopIteration:
            pass
        k.finish()
        k.close()

    def dump(self, name, buf, ap, shape):
        if not self.debug_x:
            return
        o = self.k.dram("dbg_" + name, list(shape), F32, kind="ExternalOutput")
        self.k.dma("pool", o.ap(), ap, reads=[buf], writes=[o])

    def chk(self, name):
        if self.stop == name:
            if name == "norm":
                self.dump("hT", self.hT, self.hT[:], [128, 8, NTOK])
                self.dump("mods", self.mods, self.mods[:], [128, 6, D])
            raise StopIteration

    def layers(self):
        self.chk("copy")
        for i in self.layer_list:
            last = i == DEPTH - 1
            kind, j = i % 3, i // 3
            tiles = list(range(NT))
            self.ada(i, 0)
            self.chk("ada")
            self.norm_to_hT(tiles, self.xt_ap)
            self.chk("norm")
            if kind == 0:
                self.mlstm(i, j, last)
            elif kind == 1:
                self.gla(i, j, last)
            else:
                self.na(i, j, last)
            self.chk("mixer")
            self.ada(i, 1)
            self.norm_to_hT(list(range(16)) if last else tiles, self.xt_ap)
            self.ffn(i, last)
            self.chk("ffn")
        self.final_norm()


def _consts():
    i = np.arange(128)
    ident = np.eye(128, dtype=np.float32)
    triu = (i[:, None] <= i[None, :]).astype(np.float32)
    tril = (i[:, None] >= i[None, :]).astype(np.float32)
    ones = np.ones((128, 128), np.float32)
    return np.stack([ident, triu, tril, ones, triu, tril]).astype(np.float32)


def prep_inputs(inp):
    f = lambda a: np.ascontiguousarray(np.asarray(a, dtype=np.float32))
    shared = {}
    for n in ("w_ada", "b_ada", "norm_mix", "norm_ffn", "w_up", "w_down", "a_w_in", "a_b_gate", "a_w_out",
              "b_w_in", "b_norm", "b_w_out", "c_w_in", "c_w_out"):
        shared[n] = f(inp[n])
    shared["norm_final"] = f(inp["norm_final"]).reshape(1, D)
    cw = np.concatenate([f(inp["ffn_conv_w"]), f(inp["ffn_conv_b"])[:, None, :]], axis=1)
    shared["ffn_cw"] = f(cw.reshape(DEPTH, 4, 44, 128).transpose(0, 3, 1, 2).reshape(DEPTH, 128, 4 * 44))
    acw = np.concatenate([f(inp["a_conv_w"]), f(inp["a_conv_b"])[:, None, :]], axis=1)
    shared["a_cw"] = f(acw.reshape(2, 4, 8, 128).transpose(0, 3, 1, 2).reshape(2, 128, 32))
    shared["b_w_a2e"] = f(np.concatenate([f(inp["b_w_a2"]), f(inp["b_b_a"])[:, :, None, :]], axis=2))
    qc = np.arange(64)
    dc = np.clip(qc[None, :] - qc[:, None] + 15, 0, 30)
    rp = f(inp["c_rpb"])
    rx = rp[:, :, :, dc]
    shared["c_rpbx"] = f(rx.transpose(0, 1, 3, 2, 4).reshape(1, 16, 64, 15 * 64))
    cst = np.clip(qc - 8, 0, 48)
    cm = np.where((qc[None, :] >= cst[:, None]) & (qc[None, :] < cst[:, None] + 16), 0.0, NEG).astype(np.float32)
    shared["cmask"] = f(np.concatenate([cm, cm], axis=0))
    shared["consts"] = _consts()
    cctx_t = f(np.asarray(inp["c_ctx"]).reshape(8, 128).T)
    maps = []
    for b in range(8):
        mdict = dict(shared)
        mdict["x"] = f(inp["x"][b])
        mdict["ctx"] = f(inp["ctx"][b])
        mdict["c_t"] = f(np.asarray(inp["c"][b]).reshape(8, 128).T)
        mdict["cctx_t"] = cctx_t
        maps.append(mdict)
    return maps


_CACHE = {}


def kernel(**inputs):
    if "mk" not in _CACHE:
        _CACHE["mk"] = MK()
    mk = _CACHE["mk"]
    maps = prep_inputs(inputs)
    res = run_bass_kernel_spmd(mk.nc, maps, core_ids=list(range(8)))
    return np.stack([np.asarray(r["out"], dtype=np.float32) for r in res.results], axis=0)
```

```python
import os
import numpy as np
import concourse.bass as bass
import concourse.mybir as mybir
from concourse.bass_utils import run_bass_kernel_spmd

F32 = mybir.dt.float32
BF16 = mybir.dt.bfloat16
AF = mybir.ActivationFunctionType
ALU = mybir.AluOpType
AX = mybir.AxisListType

ERA = 6000
NDMASEM = 12
INF = 1 << 60

D = 1024
T = 2048
TCX = 256
NTOK = T + TCX
NT = NTOK // 128
DEPTH = 4
FF = 2816
NFC = FF // 128
EPS = 1e-6
NEG = -30000.0


class Buf:
    def __init__(self, t, name):
        self.t = t
        self.name = name
        self.writes = []
        self.reads = []

    def __getitem__(self, idx):
        return self.t[idx]

    def ap(self):
        return self.t.ap()


class Acc:
    def __init__(self, buf, lo=0, hi=INF):
        self.buf, self.lo, self.hi = buf, lo, hi


def R(buf, lo=0, hi=INF):
    return Acc(buf, lo, hi)


def _acc(a):
    return a if isinstance(a, Acc) else Acc(a)


class KB:
    def __init__(self):
        self.nc = bass.Bass("TRN2", target_bir_lowering=False)
        nc = self.nc
        self.eng = {"pe": nc.tensor, "act": nc.scalar, "dve": nc.vector, "pool": nc.gpsimd, "sp": nc.sync}
        self.cnt = {e: 0 for e in self.eng}
        self.sems = {e: [] for e in self.eng}
        self.waited = {e: {} for e in self.eng}
        self.dma_n = {e: 0 for e in self.eng}
        self.dma_sems = {e: [] for e in self.eng}
        self.last_tok = {e: None for e in self.eng}
        self.dma_tok = {e: {} for e in self.eng}
        self._ctx = []
        self.nsem = 0
        self.sem_ids = {}

    def _enter(self, cm):
        v = cm.__enter__()
        self._ctx.append(cm)
        return v

    def mark(self):
        return len(self._ctx)

    def release(self, m):
        while len(self._ctx) > m:
            self._ctx.pop().__exit__(None, None, None)

    def close(self):
        self.release(0)

    def sem(self, name):
        self.nsem += 1
        s = self._enter(self.nc.semaphore(self._uname(name)))
        self.sem_ids[id(s)] = s
        return s

    def _uname(self, name):
        self.uid = getattr(self, "uid", 0) + 1
        return f"{name}_{self.uid}"

    def sb(self, name, shape, dt):
        name = self._uname(name)
        return Buf(self._enter(self.nc.sbuf_tensor(name, list(shape), dt)), name)

    def ps(self, name, shape, dt=F32):
        name = self._uname(name)
        return Buf(self._enter(self.nc.psum_tensor(name, list(shape), dt)), name)

    def dram(self, name, shape, dt, kind="Internal"):
        return Buf(self.nc.dram_tensor(name, list(shape), dt, kind=kind), name)

    def _eng_token(self, e):
        n = self.cnt[e]
        era = n // ERA
        while len(self.sems[e]) <= era:
            self.sems[e].append(self.sem(f"s_{e}_{len(self.sems[e])}"))
        self.cnt[e] = n + 1
        return (self.sems[e][era], (n % ERA) + 1)

    def _wait(self, e, token):
        sem, val = token
        w = self.waited[e]
        key = id(sem)
        if w.get(key, 0) >= val:
            return
        w[key] = val
        self.eng[e].wait_ge(sem, val)

    def _deps(self, e, reads, writes, is_dma):
        toks = []
        for a in reads:
            a = _acc(a)
            for (lo, hi, tok, we) in a.buf.writes:
                if lo < a.hi and a.lo < hi:
                    toks.append(tok)
        for a in writes:
            a = _acc(a)
            for (lo, hi, tok, we) in a.buf.writes:
                if lo < a.hi and a.lo < hi and (is_dma or we != e):
                    toks.append(tok)
            for (lo, hi, tok, re) in a.buf.reads:
                if lo < a.hi and a.lo < hi and (is_dma or re != e):
                    toks.append(tok)
        for t in toks:
            self._wait(e, t)

    def _record(self, e, tok, reads, writes, is_dma):
        tag = ("dma", e) if is_dma else e
        for a in reads:
            a = _acc(a)
            b = a.buf
            if not is_dma:
                b.reads = [r for r in b.reads if not (r[3] == tag and r[0] >= a.lo and r[1] <= a.hi)]
            b.reads.append((a.lo, a.hi, tok, tag))
        for a in writes:
            a = _acc(a)
            b = a.buf
            b.writes = [w for w in b.writes if not (w[0] >= a.lo and w[1] <= a.hi)]
            b.reads = [r for r in b.reads if not (r[0] >= a.lo and r[1] <= a.hi)]
            b.writes.append((a.lo, a.hi, tok, tag))

    def op(self, e, fn, reads=(), writes=(), rg=0):
        if e == "pe":
            if rg != getattr(self, "pe_rg", 0) and self.last_tok["pe"] is not None:
                self._wait("pe", self.last_tok["pe"])
            self.pe_rg = rg
        self._deps(e, reads, writes, False)
        ins = fn()
        tok = self._eng_token(e)
        ins.then_inc(tok[0], 1)
        self.last_tok[e] = tok
        self._record(e, tok, reads, writes, False)
        return tok

    def dma(self, e, out_ap, in_ap, reads=(), writes=(), **kw):
        n = self.dma_n[e]
        if not self.dma_sems[e]:
            self.dma_sems[e] = [self.sem(f"d_{e}_{i}") for i in range(NDMASEM)]
        sem = self.dma_sems[e][n % NDMASEM]
        prev = 16 * (n // NDMASEM)
        if prev:
            self._wait(e, (sem, prev))
        self._deps(e, reads, writes, True)
        self.eng[e].dma_start(out=out_ap, in_=in_ap, **kw).then_inc(sem, 16)
        self.dma_n[e] = n + 1
        tok = (sem, prev + 16)
        assert prev + 16 < 32000, "dma sem overflow"
        self.dma_tok[e][n % NDMASEM] = tok
        self._record(e, tok, reads, writes, True)
        return tok

    def barrier(self):
        toks = [t for t in self.last_tok.values() if t is not None]
        for e in self.dma_tok:
            toks += list(self.dma_tok[e].values())
        for e in self.eng:
            for t in toks:
                self._wait(e, t)

    def finish(self):
        self.barrier()


def bc_last(ap, n):
    return bass.AP(ap.tensor, ap.offset, [list(d) for d in ap.ap] + [[0, n]])


def bc_mid(ap, pos, n):
    l = [list(d) for d in ap.ap]
    l.insert(pos, [0, n])
    return bass.AP(ap.tensor, ap.offset, l)


class MK:
    def __init__(self, nlayers=DEPTH, debug_x=False, stop=None, layer_list=None):
        self.stop = stop
        self.layer_list = list(range(nlayers)) if layer_list is None else layer_list
        self.k = KB()
        self.nc = self.k.nc
        self.nlayers = nlayers
        self.debug_x = debug_x
        self.build()

    def decl_inputs(self):
        k = self.k
        I = lambda n, s: k.dram(n, s, F32, kind="ExternalInput")
        self.x_in = I("x", [T, D])
        self.ctx_in = I("ctx", [TCX, D])
        self.c_t = I("c_t", [128, 8])
        self.cctx_t = I("cctx_t", [128, 8])
        self.w_ada = I("w_ada", [DEPTH, D, 6 * D])
        self.b_ada = I("b_ada", [DEPTH, 6 * D])
        self.norm_mix = I("norm_mix", [DEPTH, D])
        self.norm_ffn = I("norm_ffn", [DEPTH, D])
        self.w_up = I("w_up", [DEPTH, D, 2 * FF])
        self.ffn_cw = I("ffn_cw", [DEPTH, 128, 4 * 44])
        self.w_down = I("w_down", [DEPTH, FF, D])
        self.a_w_in = I("a_w_in", [2, D, 3104])
        self.a_b_gate = I("a_b_gate", [2, 32])
        self.a_cw = I("a_cw", [2, 128, 4 * 8])
        self.a_w_out = I("a_w_out", [2, D, D])
        self.b_w_in = I("b_w_in", [1, D, 3104])
        self.b_w_a2 = I("b_w_a2e", [1, 2, 17, 512])
        self.b_norm = I("b_norm", [1, 256])
        self.b_w_out = I("b_w_out", [1, D, D])
        self.c_w_in = I("c_w_in", [1, D, 3 * D])
        self.c_rpbx = I("c_rpbx", [1, 16, 64, 15 * 64])
        self.cmask_in = I("cmask", [128, 64])
        self.c_w_out = I("c_w_out", [1, D, D])
        self.norm_final = I("norm_final", [1, D])
        self.consts = I("consts", [6, 128, 128])
        self.out = k.dram("out", [T, D], F32, kind="ExternalOutput")
        self.xres = k.dram("xres", [NTOK, D], F32, kind="ExternalOutput" if self.debug_x else "Internal")

    @staticmethod
    def swpipe(n, stages, skew=1):
        S = len(stages)
        for i in range(n + (S - 1) * skew):
            for si, fn in enumerate(stages):
                u = i - si * skew
                if 0 <= u < n:
                    fn(u)

    def xt_ap(self, t):
        return self.xres.ap()[t * 128:(t + 1) * 128, :]

    def bcast_row_dma(self, dstbuf, row_ap_1xn, n):
        src = bass.AP(row_ap_1xn.tensor, row_ap_1xn.offset, [[0, 128], [1, n]])
        self.k.dma("sp", dstbuf[:], src, writes=[dstbuf])

    def load_w(self, dstbuf, dst_ap, src2d, rng=None):
        src = src2d.rearrange("(kc k) n -> k kc n", k=128)
        w = [dstbuf] if rng is None else [R(dstbuf, *rng)]
        return self.k.dma("pool", dst_ap, src, writes=w)

    def setup_consts(self):
        k, nc = self.k, self.nc
        self.cf = k.sb("cf", [128, 6, 128], F32)
        self.cb = k.sb("cb", [128, 6, 128], BF16)
        k.dma("sp", self.cf[:], self.consts.ap().rearrange("c p n -> p c n"), writes=[self.cf])
        k.dma("pool", self.cb[:], self.consts.ap().rearrange("c p n -> p c n"), writes=[self.cb])
        self.ident_b = self.cb[:, 0, :]
        self.triu_f = self.cf[:, 1, :]
        self.tril_f = self.cf[:, 2, :]
        self.ones_f = self.cf[:, 3, :]
        self.sbc = k.sb("sbc", [128, 2, 8, 128], BF16)
        cs = k.sb("cs", [128, 2, 8], F32)
        k.dma("sp", cs[:, 0, :], self.c_t.ap(), writes=[cs])
        k.dma("sp", cs[:, 1, :], self.cctx_t.ap(), writes=[cs])
        cs2 = k.sb("cs2", [128, 2, 8], F32)
        k.op("act", lambda: nc.scalar.activation(cs2[:], cs[:], AF.Silu), reads=[cs], writes=[cs2])
        k.op("dve", lambda: nc.vector.tensor_copy(self.sbc[:].rearrange("p a b m -> p (a b) m"),
                                                  bc_last(cs2[:].rearrange("p a b -> p (a b)"), 128)),
             reads=[cs2], writes=[self.sbc])
        self.hT = k.sb("hT", [128, 8, NTOK], BF16)
        self.mods = k.sb("mods", [128, 6, D], F32)
        self.ssz = k.sb("ssz", [128, 4], F32)

    def ada(self, i, phase):
        k, nc = self.k, self.nc
        m = k.mark()
        wbuf = [k.sb(f"adaw{j}", [128, 8, 512], BF16) for j in range(2)]
        bb = k.sb("adab", [128, 3 * D], F32)
        gn = k.sb("adag", [128, D], F32)
        pp = [k.ps(f"adap{j}", [128, 512], F32) for j in range(2)]
        c0 = phase * 3 * D
        self.bcast_row_dma(bb, self.b_ada.ap()[i:i + 1, c0:c0 + 3 * D], 3 * D)
        g_src = (self.norm_mix if phase == 0 else self.norm_ffn).ap()[i:i + 1, :]
        self.bcast_row_dma(gn, g_src, D)
        n = 0
        for blk in range(6):
            wb = wbuf[blk % 2]
            self.load_w(wb, wb[:], self.w_ada.ap()[i, :, c0 + blk * 512:c0 + (blk + 1) * 512])
            part, half = blk // 2, blk % 2
            for lc in range(2):
                p = pp[n % 2]
                n += 1
                for kc in range(8):
                    k.op("pe", lambda kc=kc, p=p, wb=wb, lc=lc: nc.tensor.matmul(
                        p[:], self.sbc[:, lc, kc, :], wb[:, kc, :], start=(kc == 0), stop=(kc == 7)),
                        reads=[self.sbc, wb], writes=[p])
                bsl = bb[:, blk * 512:(blk + 1) * 512]
                dst = self.mods[:, lc * 3 + (1, 0, 2)[part], half * 512:(half + 1) * 512]
                if part == 1:
                    tmp = gn[:, half * 512:(half + 1) * 512]
                    k.op("dve", lambda p=p, bsl=bsl, dst=dst: nc.vector.scalar_tensor_tensor(
                        dst, p[:], 1.0, bsl, ALU.add, ALU.add), reads=[p, bb], writes=[self.mods])
                    k.op("dve", lambda dst=dst, tmp=tmp: nc.vector.tensor_tensor(dst, dst, tmp, ALU.mult),
                         reads=[self.mods, gn], writes=[self.mods])
                else:
                    k.op("dve", lambda p=p, bsl=bsl, dst=dst: nc.vector.tensor_tensor(dst, p[:], bsl, ALU.add),
                         reads=[p, bb], writes=[self.mods])
        k.barrier()
        k.release(m)

    def norm_to_hT(self, tiles, src_fn):
        k, nc = self.k, self.nc
        m = k.mark()
        NB = 3
        xt = [k.sb(f"nx{j}", [128, D], F32) for j in range(NB)]
        junk = k.sb("njunk", [128, D], BF16)
        tmp = [k.sb(f"ntmp{j}", [128, D], F32) for j in range(NB)]
        hb = [k.sb(f"nhb{j}", [128, D], BF16) for j in range(NB)]
        st = [k.sb(f"nst{j}", [128, 4], F32) for j in range(NB)]
        pt = [k.ps(f"npt{j}", [128, 8, 128], BF16) for j in range(2)]

        def stA(n):
            t = tiles[n]
            lc = 0 if t < 16 else 1
            x, s, tm, h = xt[n % NB], st[n % NB], tmp[n % NB], hb[n % NB]
            k.dma("sp", x[:], src_fn(t), reads=[R(self.xres, t, t + 1)], writes=[x])
            k.op("pool", lambda: nc.gpsimd.memset(s[:], 0.0), writes=[s])
            k.op("act", lambda: nc.scalar.activation(junk[:], x[:], AF.Square, accum_out=s[:, 0:1]), reads=[x, s], writes=[junk, s])
            k.op("act", lambda: nc.scalar.activation(s[:, 1:2], s[:, 0:1], AF.Ln, scale=1.0 / D, bias=EPS), reads=[s], writes=[s])
            k.op("act", lambda: nc.scalar.activation(s[:, 2:3], s[:, 1:2], AF.Exp, scale=-0.5), reads=[s], writes=[s])
            k.op("dve", lambda: nc.vector.scalar_tensor_tensor(tm[:], x[:], s[:, 2:3], self.mods[:, lc * 3 + 0, :], ALU.mult, ALU.mult),
                 reads=[x, s, self.mods], writes=[tm])
            k.op("pool", lambda: nc.gpsimd.tensor_tensor(h[:], tm[:], self.mods[:, lc * 3 + 1, :], ALU.add),
                 reads=[tm, self.mods], writes=[h])

        def stB(n):
            t = tiles[n]
            h, p = hb[n % NB], pt[n % 2]
            for kc in range(8):
                k.op("pe", lambda kc=kc: nc.tensor.transpose(p[:, kc, :], h[:, kc * 128:(kc + 1) * 128], self.ident_b),
                     reads=[h, self.cb], writes=[p])
            k.op("act", lambda: nc.scalar.copy(self.hT[:, :, t * 128:(t + 1) * 128], p[:]),
                 reads=[p], writes=[R(self.hT, t * 128, (t + 1) * 128)])

        self.swpipe(len(tiles), [stA, stB])
        k.barrier()
        k.release(m)

    def resid_update(self, t, ypsum, xb, tb):
        k, nc = self.k, self.nc
        lc = 0 if t < 16 else 1
        k.dma("sp", xb[:], self.xt_ap(t), reads=[R(self.xres, t, t + 1)], writes=[xb])
        k.op("dve", lambda: nc.vector.tensor_tensor(tb[:], ypsum[:], self.mods[:, lc * 3 + 2, :], ALU.mult),
             reads=[ypsum, self.mods], writes=[tb])
        k.op("dve", lambda: nc.vector.tensor_tensor(xb[:], xb[:], tb[:], ALU.add), reads=[xb, tb], writes=[xb])
        k.dma("sp", self.xt_ap(t), xb[:], reads=[xb], writes=[R(self.xres, t, t + 1)])

    def conv_evac(self, dst_f32, p, cw, j, ncols, has_left, has_right, nout):
        k, nc = self.k, self.nc
        o = 1 if has_left else 0
        k.op("act", lambda: nc.scalar.activation(dst_f32[:, 0:nout], p[:, o:o + nout], AF.Identity,
                                                 scale=cw[:, 1, j:j + 1], bias=cw[:, 3, j:j + 1]),
             reads=[p, cw], writes=[dst_f32])
        lo = 0 if has_left else 1
        k.op("dve", lambda: nc.vector.scalar_tensor_tensor(dst_f32[:, lo:nout], p[:, o + lo - 1:o + nout - 1],
                                                           cw[:, 0, j:j + 1], dst_f32[:, lo:nout], ALU.mult, ALU.add),
             reads=[p, cw, dst_f32], writes=[dst_f32])
        hi = nout if has_right else nout - 1
        k.op("dve", lambda: nc.vector.scalar_tensor_tensor(dst_f32[:, 0:hi], p[:, o + 1:o + hi + 1],
                                                           cw[:, 2, j:j + 1], dst_f32[:, 0:hi], ALU.mult, ALU.add),
             reads=[p, cw, dst_f32], writes=[dst_f32])

    @staticmethod
    def tblocks(tiles_lat=True, tiles_ctx=True):
        bl = []
        if tiles_lat:
            bl += [(s, s + 256, 0, T) for s in range(0, T, 256)]
        if tiles_ctx:
            bl += [(T, T + 256, T, NTOK)]
        return bl

    def ffn(self, i, last):
        k, nc = self.k, self.nc
        m = k.mark()
        cw = k.sb("fcw", [128, 4, 44], F32)
        k.dma("sp", cw[:], self.ffn_cw.ap()[i].rearrange("p (a j) -> p a j", a=4), writes=[cw])
        wd = k.sb("fwd", [128, NFC, D], BF16)
        for q in range(4):
            j0, j1 = (0, 6, 12, 17, 22)[q], (0, 6, 12, 17, 22)[q + 1]
            self.load_w(wd, wd[:, j0:j1, :], self.w_down.ap()[i, j0 * 128:j1 * 128, :], rng=(j0, j1))
        wa = [k.sb(f"fwa{j}", [128, 8, 256], BF16) for j in range(2)]
        wg = [k.sb(f"fwg{j}", [128, 8, 256], BF16) for j in range(2)]
        actT = k.sb("factT", [128, NFC, 768], BF16)
        pa = [k.ps(f"fpa{j}", [128, 512], F32) for j in range(2)]
        pg = [k.ps(f"fpg{j}", [128, 512], F32) for j in range(2)]
        py = [k.ps(f"fpy{j}", [128, D], F32) for j in range(2)]
        ua = [k.sb(f"fua{j}", [128, 384], F32) for j in range(2)]
        ug = [k.sb(f"fug{j}", [128, 384], F32) for j in range(2)]
        xb = [k.sb(f"fxb{j}", [128, D], F32) for j in range(2)]
        tb1 = k.sb("ftb", [128, D], F32)
        tb = [tb1, tb1]
        groups = [(j, j + 2) for j in range(0, NFC, 2)]
        tgs = [[(0, 384, 0, T), (384, 768, 0, T)], [(768, 1152, 0, T), (1152, 1536, 0, T)],
               [(1536, 1920, 0, T), (1920, 2048, 0, T)] + ([] if last else [(T, NTOK, T, NTOK)])]
        cnt = 0
        gi = 0
        for tg in tgs:
            base = tg[0][0]
            for (j0, j1) in groups:
                wab, wgb = wa[gi % 2], wg[gi % 2]
                gi += 1
                nj = j1 - j0
                self.load_w(wab, wab[:, :, 0:nj * 128], self.w_up.ap()[i, :, j0 * 128:j1 * 128])
                self.load_w(wgb, wgb[:, :, 0:nj * 128], self.w_up.ap()[i, :, FF + j0 * 128:FF + j1 * 128])
                for (s, e, ss, se) in tg:
                    cs, ce = max(s - 1, ss), min(e + 1, se)
                    hl, hr = cs < s, ce > e
                    ncol = ce - cs
                    off = (s - base) if s < T else (512 + s - T)
                    for j in range(j0, j1):
                        A, G, UA, UG = pa[cnt % 2], pg[cnt % 2], ua[cnt % 2], ug[cnt % 2]
                        cnt += 1
                        for (P, W) in ((A, wab), (G, wgb)):
                            for kc in range(8):
                                k.op("pe", lambda P=P, W=W, kc=kc, j=j: nc.tensor.matmul(
                                    P[:, 0:ncol], W[:, kc, (j - j0) * 128:(j - j0 + 1) * 128], self.hT[:, kc, cs:ce],
                                    start=(kc == 0), stop=(kc == 7)),
                                    reads=[W, R(self.hT, cs, ce)], writes=[P])
                        nt_ = e - s
                        self.conv_evac(UA, A, cw, j, ncol, hl, hr, nt_)
                        self.conv_evac(UG, G, cw, 22 + j, ncol, hl, hr, nt_)
                        k.op("act", lambda UG=UG, nt_=nt_: nc.scalar.activation(UG[:, 0:nt_], UG[:, 0:nt_], AF.Silu), reads=[UG], writes=[UG])
                        k.op("dve", lambda UA=UA, UG=UG, j=j, off=off, nt_=nt_: nc.vector.tensor_tensor(
                            actT[:, j, off:off + nt_], UA[:, 0:nt_], UG[:, 0:nt_], ALU.mult),
                            reads=[UA, UG], writes=[R(actT, off, off + nt_)])
            tiles = sorted(set(tt for (s, e, _, _) in tg for tt in range(s // 128, e // 128)))
            for n, t in enumerate(tiles):
                Y = py[n % 2]
                off = (t * 128 - base) if t < 16 else (512 + t * 128 - T)
                for nh in range(2):
                    for j in range(NFC):
                        k.op("pe", lambda Y=Y, j=j, nh=nh, off=off: nc.tensor.matmul(
                            Y[:, nh * 512:(nh + 1) * 512], actT[:, j, off:off + 128], wd[:, j, nh * 512:(nh + 1) * 512],
                            start=(j == 0), stop=(j == NFC - 1)),
                            reads=[R(actT, off, off + 128), R(wd, j, j + 1)], writes=[R(Y, nh * 512, (nh + 1) * 512)])
                self.resid_update(t, Y, xb[n % 2], tb[n % 2])
        k.barrier()
        k.release(m)

    def out_proj_tile(self, t, ysb, nkc, wout, py, yT, ptr, xb, tb):
        k, nc = self.k, self.nc
        for kc in range(nkc):
            k.op("pe", lambda kc=kc: nc.tensor.transpose(ptr[:, kc, :], ysb[:, kc * 128:(kc + 1) * 128], self.ident_b),
                 reads=[ysb, self.cb], writes=[ptr])
        k.op("act", lambda: nc.scalar.copy(yT[:, 0:nkc, :], ptr[:, 0:nkc, :]), reads=[ptr], writes=[yT])
        for nh in range(2):
            for kc in range(nkc):
                k.op("pe", lambda kc=kc, nh=nh: nc.tensor.matmul(
                    py[:, nh * 512:(nh + 1) * 512], yT[:, kc, :], wout[:, kc, nh * 512:(nh + 1) * 512],
                    start=(kc == 0), stop=(kc == nkc - 1)),
                    reads=[yT, wout], writes=[R(py, nh * 512, (nh + 1) * 512)])
        if xb is not None:
            self.resid_update(t, py, xb, tb)

    def proj_tok(self, p_ap, pbuf, t, w, c0, n, prange=None):
        k, nc = self.k, self.nc
        for kc in range(8):
            k.op("pe", lambda kc=kc: nc.tensor.matmul(p_ap, self.hT[:, kc, t * 128:(t + 1) * 128], w[:, kc, c0:c0 + n],
                                                      start=(kc == 0), stop=(kc == 7)),
                 reads=[R(self.hT, t * 128, (t + 1) * 128), w], writes=[pbuf if prange is None else R(pbuf, *prange)])

    def mlstm(self, i, jm, last):
        k, nc = self.k, self.nc
        m0 = k.mark()
        W = self.a_w_in.ap()[jm]
        GT = k.sb("mGT", [128, NT, 32], F32)
        GP = k.sb("mGP", [128, NT, 3, 16], F32)
        bg = k.sb("mbg", [128, 32], F32)
        self.bcast_row_dma(bg, self.a_b_gate.ap()[jm:jm + 1, :], 32)
        cw = k.sb("mcw", [128, 4, 8], F32)
        k.dma("sp", cw[:], self.a_cw.ap()[jm].rearrange("p (a j) -> p a j", a=4), writes=[cw])
        fwd_order = [16, 17] + list(range(16))
        bwd_order = [17, 16] + list(range(15, -1, -1))
        for hg in range(2):
            m1 = k.mark()
            qkT = k.sb("mqkT", [128, 4, NTOK], BF16)
            V = k.sb("mV", [128, NT, 4, 130], BF16)
            Kt = k.sb("mKt", [128, NT, 256], BF16)
            H = k.sb("mH", [128, NT, 512], BF16)
            wq = k.sb("mwq", [128, 8, 512], BF16)
            wv = k.sb("mwv", [128, 8, 512], BF16)
            wo = k.sb("mwo", [128, 8, 512], BF16)
            wg_ = k.sb("mwg", [128, 8, 32], BF16)
            wout = k.sb("mwout", [128, 4, D], BF16)
            self.load_w(wq, wq[:, :, 0:256], W[:, hg * 256:(hg + 1) * 256], rng=(0, 256))
            self.load_w(wq, wq[:, :, 256:512], W[:, 512 + hg * 256:512 + (hg + 1) * 256], rng=(256, 512))
            self.load_w(wv, wv[:], W[:, 1024 + hg * 512:1024 + (hg + 1) * 512])
            self.load_w(wo, wo[:], W[:, 2048 + hg * 512:2048 + (hg + 1) * 512])
            self.load_w(wout, wout[:], self.a_w_out.ap()[jm, hg * 512:(hg + 1) * 512, :])
            if hg == 0:
                self.load_w(wg_, wg_[:], W[:, 3072:3104])
            k.op("dve", lambda: nc.vector.memset(V[:, :, :, 128:130], 1.0), writes=[V])
            m2 = k.mark()
            pq = [k.ps(f"mpq{j}", [128, 512], F32) for j in range(2)]
            uq = [k.sb(f"muq{j}", [128, 256], F32) for j in range(2)]
            cnt = 0
            for (s, e, ss, se) in self.tblocks():
                cs, ce = max(s - 1, ss), min(e + 1, se)
                hl, hr = cs < s, ce > e
                ncol = ce - cs
                for ch in range(4):
                    P, U = pq[cnt % 2], uq[cnt % 2]
                    cnt += 1
                    for kc in range(8):
                        k.op("pe", lambda P=P, kc=kc, ch=ch: nc.tensor.matmul(
                            P[:, 0:ncol], wq[:, kc, ch * 128:(ch + 1) * 128], self.hT[:, kc, cs:ce],
                            start=(kc == 0), stop=(kc == 7)), reads=[wq, R(self.hT, cs, ce)], writes=[P])
                    jcol = (ch // 2) * 4 + hg * 2 + (ch % 2)
                    self.conv_evac(U, P, cw, jcol, ncol, hl, hr, 256)
                    k.op("act", lambda U=U: nc.scalar.activation(U[:], U[:], AF.Silu), reads=[U], writes=[U])
                    sc = 0.125 if ch < 2 else 1.0
                    k.op("dve", lambda U=U, ch=ch, s=s, sc=sc: nc.vector.tensor_scalar(
                        qkT[:, ch, s:s + 256], U[:], sc, None, ALU.mult), reads=[U], writes=[R(qkT, s, s + 256)])
            k.barrier()
            if self.stop == "m_qk":
                self.dump("qkT", qkT, qkT[:], [128, 4, NTOK])
            self.chk("m_qk")
            k.release(m2)
            m2 = k.mark()
            pv = [k.ps(f"mpv{j}", [128, 512], F32) for j in range(2)]
            pgt = [k.ps(f"mpg{j}", [128, 512], F32) for j in range(2)]
            pk = [k.ps(f"mpk{j}", [128, 8, 128], BF16) for j in range(2)]
            for t in range(NT):
                P = pv[t % 2]
                self.proj_tok(P[:], P, t, wv, 0, 512)
                k.op("act", lambda P=P, t=t: nc.scalar.copy(V[:, t, :, 0:128], P[:].rearrange("p (h e) -> p h e", h=4)),
                     reads=[P], writes=[R(V, t, t + 1)])
                if hg == 0:
                    Pg = pgt[t % 2]
                    self.proj_tok(Pg[:, 0:32], Pg, t, wg_, 0, 32)
                    k.op("dve", lambda Pg=Pg, t=t: nc.vector.tensor_tensor(GT[:, t, :], Pg[:, 0:32], bg[:], ALU.add),
                         reads=[Pg, bg], writes=[R(GT, t, t + 1)])
                Pk = pk[t % 2]
                for c2 in range(2):
                    k.op("pe", lambda Pk=Pk, c2=c2, t=t: nc.tensor.transpose(
                        Pk[:, c2, :], qkT[:, 2 + c2, t * 128:(t + 1) * 128], self.ident_b),
                        reads=[R(qkT, t * 128, (t + 1) * 128), self.cb], writes=[Pk])
                k.op("act", lambda Pk=Pk, t=t: nc.scalar.copy(Kt[:, t, :].rearrange("p (a b) -> p a b", a=2), Pk[:, 0:2, :]),
                     reads=[Pk], writes=[R(Kt, t, t + 1)])
            k.barrier()
            if self.stop == "m_v":
                self.dump("V", V, V[:], [128, NT, 4, 130])
                self.dump("Kt", Kt, Kt[:], [128, NT, 256])
                self.dump("GT", GT, GT[:], [128, NT, 32])
            self.chk("m_v")
            k.release(m2)
            if hg == 0:
                m2 = k.mark()
                e1 = [k.sb(f"me1{j}", [128, 2, 8], F32) for j in range(2)]
                sp_ = [k.sb(f"msp{j}", [128, 2, 8], F32) for j in range(2)]
                aa = [k.sb(f"maa{j}", [128, 2, 8], F32) for j in range(2)]
                pgp = [k.ps(f"mpgp{j}", [128, 2, 256], F32) for j in range(2)]
                for t in range(NT):
                    E1, SP, A_, PG = e1[t % 2], sp_[t % 2], aa[t % 2], pgp[t % 2]
                    gv = GT[:, t, :].rearrange("p (d w h) -> p d w h", d=2, w=2)
                    k.op("act", lambda E1=E1, gv=gv: nc.scalar.activation(E1[:], gv[:, :, 1, :], AF.Exp, scale=-1.0),
                         reads=[R(GT, t, t + 1)], writes=[E1])
                    k.op("act", lambda E1=E1, SP=SP: nc.scalar.activation(SP[:], E1[:], AF.Ln, bias=1.0), reads=[E1], writes=[SP])
                    k.op("pe", lambda PG=PG, SP=SP: nc.tensor.matmul(PG[:, 0, 0:8], self.triu_f, SP[:, 0, :], start=True, stop=True),
                         reads=[self.cf, SP], writes=[R(PG, 0, 8)])
                    k.op("pe", lambda PG=PG, SP=SP: nc.tensor.matmul(PG[:, 0, 8:16], self.tril_f, SP[:, 1, :], start=True, stop=True),
                         reads=[self.cf, SP], writes=[R(PG, 8, 16)])
                    k.op("pe", lambda PG=PG, SP=SP: nc.tensor.matmul(PG[:, 1, 0:16], self.ones_f, SP[:].rearrange("p a b -> p (a b)"), start=True, stop=True),
                         reads=[self.cf, SP], writes=[R(PG, 16, 32)])
                    k.op("dve", lambda A_=A_, PG=PG, gv=gv: nc.vector.tensor_tensor(
                        A_[:], PG[:, 0, 0:16].rearrange("p (a b) -> p a b", a=2), gv[:, :, 0, :], ALU.add),
                        reads=[PG, R(GT, t, t + 1)], writes=[A_])
                    k.op("act", lambda A_=A_, t=t: nc.scalar.activation(GP[:, t, 0, :], A_[:].rearrange("p a b -> p (a b)"), AF.Exp),
                         reads=[A_], writes=[R(GP, t, t + 1)])
                    k.op("act", lambda PG=PG, t=t: nc.scalar.activation(GP[:, t, 1:3, :], PG[:, :, 0:16], AF.Exp, scale=-1.0),
                         reads=[PG], writes=[R(GP, t, t + 1)])
                k.barrier()
                if self.stop == "m_gate":
                    self.dump("GP", GP, GP[:], [128, NT, 3, 16])
                self.chk("m_gate")
                k.release(m2)
            m2 = k.mark()
            C32 = [k.sb(f"mC32{d}", [128, 2, 130], F32) for d in range(2)]
            Cb = [k.sb(f"mCb{d}", [128, 2, 130], BF16) for d in range(2)]
            Vp = [k.sb(f"mVp{d}", [128, 4, 130], BF16) for d in range(2)]
            ST = [k.sb(f"mST{d}", [128, 4, 128], BF16) for d in range(2)]
            dn = [k.sb(f"mdn{d}", [128, 4, 4], F32) for d in range(2)]
            hc = [k.sb(f"mhc{d}", [128, 512], F32) for d in range(2)]
            tmpC = [k.sb(f"mtC{d}", [128, 2, 130], F32) for d in range(2)]
            pqk = [k.ps(f"mpqk{d}", [128, 4, 128], F32) for d in range(2)]
            pnd = [k.ps(f"mpnd{d}", [128, 2, 512], F32) for d in range(2)]
            pdc1 = k.ps("mpdc", [128, 2, 512], F32)
            pdc = [pdc1, pdc1]
            for d in range(2):
                k.op("dve", lambda d=d: nc.vector.memset(C32[d][:], 0.0), writes=[C32[d]])
                k.op("dve", lambda d=d: nc.vector.memset(Cb[d][:], 0.0), writes=[Cb[d]])
            LVL = int(os.environ.get("SCAN_LVL", "9"))
            for step in range(NT):
                for d in range(2):
                    t = (fwd_order, bwd_order)[d][step]
                    mask = self.cf[:, 4 + d, :]
                    ea = GP[:, t, 0, d * 8 + hg * 4:d * 8 + hg * 4 + 4]
                    inter = GP[:, t, 1, d * 8 + hg * 4:d * 8 + hg * 4 + 4]
                    egl = GP[0:64, t, 2, d * 8 + hg * 4:d * 8 + hg * 4 + 4]
                    k.op("dve", lambda d=d, t=t, ea=ea: nc.vector.tensor_tensor(
                        Vp[d][:, :, 0:129], V[:, t, :, 0:129], bc_last(ea, 129), ALU.mult),
                        reads=[R(V, t, t + 1), R(GP, t, t + 1)], writes=[Vp[d]])
                    if LVL < 2:
                        continue
                    for h in (0, 2, 1, 3):
                        pb = (h % 2) * 64
                        k.op("pe", lambda d=d, h=h, pb=pb, t=t: nc.tensor.matmul(
                            pqk[d][:, h, :], qkT[pb:pb + 64, 2 + h // 2, t * 128:(t + 1) * 128],
                            qkT[pb:pb + 64, h // 2, t * 128:(t + 1) * 128], start=True, stop=True),
                            reads=[R(qkT, t * 128, (t + 1) * 128)], writes=[R(pqk[d], h, h + 1)], rg=pb)
                    k.op("dve", lambda d=d, mask=mask: nc.vector.tensor_tensor(
                        ST[d][:], pqk[d][:], bc_mid(mask, 1, 4), ALU.mult), reads=[pqk[d], self.cf], writes=[ST[d]])
                    if LVL < 3:
                        continue
                    def qc_mm(h, start, stop):
                        pb = (h % 2) * 64
                        o_ap = pnd[d][:, h // 2, (h % 2) * 130:(h % 2) * 130 + 129]
                        k.op("pe", lambda d=d, h=h, pb=pb, t=t, o_ap=o_ap: nc.tensor.matmul(
                            o_ap, qkT[pb:pb + 64, h // 2, t * 128:(t + 1) * 128], Cb[d][pb:pb + 64, h // 2, 0:129], start=start, stop=stop),
                            reads=[R(qkT, t * 128, (t + 1) * 128), Cb[d]], writes=[R(pnd[d], h, h + 1)], rg=pb)

                    def sv_mm(h, start, stop):
                        o_ap = pnd[d][:, h // 2, (h % 2) * 130:(h % 2) * 130 + 129]
                        k.op("pe", lambda d=d, h=h, o_ap=o_ap: nc.tensor.matmul(
                            o_ap, ST[d][:, h, :], Vp[d][:, h, 0:129], start=start, stop=stop),
                            reads=[ST[d], Vp[d]], writes=[R(pnd[d], h, h + 1)])
                    qc_mm(1, True, False)
                    qc_mm(3, True, False)
                    sv_mm(1, False, True)
                    sv_mm(3, False, True)
                    sv_mm(0, True, False)
                    sv_mm(2, True, False)
                    qc_mm(0, False, True)
                    qc_mm(2, False, True)
                    if self.stop == "m_scan" and step == 1 and d == 0 and self.debug_x and os.environ.get("DBG_PND"):
                        dbgt = k.sb("dbgt", [128, 2, 512], F32)
                        k.op("act", lambda: nc.scalar.copy(dbgt[:], pnd[d][:]), reads=[pnd[d]], writes=[dbgt])
                        self.dump("pnd", dbgt, dbgt[:], [128, 2, 512])
                        self.dump("ST", ST[d], ST[d][:], [128, 4, 128])
                        self.dump("Vp", Vp[d], Vp[d][:], [128, 4, 130])
                        self.dump("Cb", Cb[d], Cb[d][:], [128, 2, 130])
                    if LVL < 4:
                        continue
                    den_ap = bass.AP(pnd[d][:, 0, 128:129].tensor, pnd[d][:, 0, 128:129].offset, [list(pnd[d][:, 0, 128:129].ap[0]), [512, 2], [130, 2]])
                    k.op("dve", lambda d=d, den_ap=den_ap, inter=inter: nc.vector.tensor_tensor(
                        dn[d][:, 0, :].rearrange("p (a b) -> p a b", a=2), den_ap, inter.rearrange("p (a b) -> p a b", a=2), ALU.mult),
                        reads=[pnd[d], R(GP, t, t + 1)], writes=[R(dn[d], 0, 1)])
                    k.op("dve", lambda d=d: nc.vector.scalar_tensor_tensor(dn[d][:, 1, :], dn[d][:, 0, :], -1.0, dn[d][:, 0, :], ALU.mult, ALU.max),
                         reads=[R(dn[d], 0, 1)], writes=[R(dn[d], 1, 2)])
                    k.op("dve", lambda d=d: nc.vector.tensor_scalar_max(dn[d][:, 1, :], dn[d][:, 1, :], 1.0),
                         reads=[R(dn[d], 1, 2)], writes=[R(dn[d], 1, 2)])
                    k.op("dve", lambda d=d: nc.vector.reciprocal(dn[d][:, 2, :], dn[d][:, 1, :]),
                         reads=[R(dn[d], 1, 2)], writes=[R(dn[d], 2, 3)])
                    k.op("dve", lambda d=d, inter=inter: nc.vector.tensor_tensor(dn[d][:, 3, :], dn[d][:, 2, :], inter, ALU.mult),
                         reads=[R(dn[d], 2, 3), R(GP, t, t + 1)], writes=[R(dn[d], 3, 4)])
                    if LVL < 5:
                        continue
                    num_ap = bass.AP(pnd[d][:, 0, 0:1].tensor, pnd[d][:, 0, 0:1].offset, [list(pnd[d][:, 0, 0:1].ap[0]), [512, 2], [130, 2], [1, 128]])
                    rec_b = bc_last(dn[d][:, 3, :].rearrange("p (a b) -> p a b", a=2), 128)
                    sf, sb_ = fwd_order.index(t), bwd_order.index(t)
                    first = (sf <= sb_) if d == 0 else (sb_ < sf)
                    if first:
                        k.op("dve", lambda t=t, num_ap=num_ap, rec_b=rec_b: nc.vector.tensor_tensor(
                            H[:, t, :].rearrange("p (a b e) -> p a b e", a=2, b=2), num_ap, rec_b, ALU.mult),
                            reads=[pnd[d], R(dn[d], 3, 4)], writes=[R(H, t, t + 1)])
                    else:
                        k.op("dve", lambda d=d, num_ap=num_ap, rec_b=rec_b: nc.vector.tensor_tensor(
                            hc[d][:].rearrange("p (a b e) -> p a b e", a=2, b=2), num_ap, rec_b, ALU.mult),
                            reads=[pnd[d], R(dn[d], 3, 4)], writes=[hc[d]])
                        k.op("dve", lambda d=d, t=t: nc.vector.tensor_tensor(H[:, t, :], H[:, t, :], hc[d][:], ALU.add),
                             reads=[hc[d], R(H, t, t + 1)], writes=[R(H, t, t + 1)])
                    if LVL < 6:
                        continue
                    for h in (0, 2, 1, 3):
                        pb = (h % 2) * 64
                        o_ap = pdc[d][pb:pb + 64, h // 2, 0:129]
                        k.op("pe", lambda d=d, h=h, t=t, o_ap=o_ap, pb=pb: nc.tensor.matmul(
                            o_ap, Kt[:, t, h * 64:(h + 1) * 64], Vp[d][:, h, 0:129], start=True, stop=True,
                            tile_position=((0, pb) if pb else None)),
                            reads=[R(Kt, t, t + 1), Vp[d]], writes=[R(pdc[d], h, h + 1)], rg=("c", pb))
                    for hf in range(2):
                        pb = hf * 64
                        c0_ = d * 8 + hg * 4 + hf
                        eglh = GP[pb:pb + 64, t, 2, c0_:c0_ + 3:2]
                        k.op("dve", lambda d=d, pb=pb: nc.vector.tensor_tensor(
                            tmpC[d][pb:pb + 64, :, 0:129], pdc[d][pb:pb + 64, :, 0:129], C32[d][pb:pb + 64, :, 0:129], ALU.add),
                            reads=[pdc[d], C32[d]], writes=[R(tmpC[d], hf, hf + 1)])
                        k.op("dve", lambda d=d, pb=pb, eglh=eglh: nc.vector.tensor_tensor(
                            C32[d][pb:pb + 64, :, 0:129], tmpC[d][pb:pb + 64, :, 0:129], bc_last(eglh, 129), ALU.mult),
                            reads=[R(tmpC[d], hf, hf + 1), R(GP, t, t + 1)], writes=[C32[d]])
                    k.op("act", lambda d=d: nc.scalar.copy(Cb[d][:], C32[d][:]), reads=[C32[d]], writes=[Cb[d]])
            k.barrier()
            if self.stop == "m_scan":
                self.dump("H", H, H[:], [128, NT, 512])
                self.dump("qkT", qkT, qkT[:], [128, 4, NTOK])
                self.dump("V", V, V[:], [128, NT, 4, 130])
                self.dump("Kt", Kt, Kt[:], [128, NT, 256])
                self.dump("GT", GT, GT[:], [128, NT, 32])
                self.dump("GP", GP, GP[:], [128, NT, 3, 16])
                self.dump("ST", ST[0], ST[0][:], [128, 4, 128])
                self.dump("C32", C32[0], C32[0][:], [128, 2, 130])
                self.dump("Vp", Vp[0], Vp[0][:], [128, 4, 130])
            self.chk("m_scan")
            k.release(m2)
            m2 = k.mark()
            po = [k.ps(f"mpo{j}", [128, 512], F32) for j in range(2)]
            py = [k.ps(f"mpy{j}", [128, D], F32) for j in range(2)]
            ptr = [k.ps(f"mptr{j}", [128, 8, 128], BF16) for j in range(2)]
            sg = [k.sb(f"msg{j}", [128, 512], BF16) for j in range(2)]
            ysb = [k.sb(f"mys{j}", [128, 512], BF16) for j in range(2)]
            yT = [k.sb(f"myT{j}", [128, 4, 128], BF16) for j in range(2)]
            xb = [k.sb(f"mxb{j}", [128, D], F32) for j in range(2)]
            tb1 = k.sb("mtb", [128, D], F32)
            tb = [tb1, tb1]
            otiles = list(range(16 if last else NT))

            def oA(n):
                t, b = otiles[n], n % 2
                self.proj_tok(po[b][:], po[b], t, wo, 0, 512)
                k.op("act", lambda: nc.scalar.activation(sg[b][:], po[b][:], AF.Sigmoid), reads=[po[b]], writes=[sg[b]])
                k.op("dve", lambda: nc.vector.tensor_tensor(ysb[b][:], H[:, t, :], sg[b][:], ALU.mult),
                     reads=[R(H, t, t + 1), sg[b]], writes=[ysb[b]])

            def oB(n):
                t, b = otiles[n], n % 2
                self.out_proj_tile(t, ysb[b], 4, wout, py[b], yT[b], ptr[b], None, None)

            def oC(n):
                t, b = otiles[n], n % 2
                self.resid_update(t, py[b], xb[b], tb[b])

            self.swpipe(len(otiles), [oA, oB, oC])
            k.barrier()
            k.release(m2)
            k.release(m1)
        k.release(m0)

    def proj_feat(self, p_ap, pbuf, w, c0, m, cs, ce):
        k, nc = self.k, self.nc
        for kc in range(8):
            k.op("pe", lambda kc=kc: nc.tensor.matmul(p_ap, w[:, kc, c0:c0 + m], self.hT[:, kc, cs:ce],
                                                      start=(kc == 0), stop=(kc == 7)),
                 reads=[w, R(self.hT, cs, ce)], writes=[pbuf])

    def gla(self, i, jm, last):
        k, nc = self.k, self.nc
        m0 = k.mark()
        W = self.b_w_in.ap()[jm]
        fwd_order = [16, 17] + list(range(16))
        bwd_order = [17, 16] + list(range(15, -1, -1))
        blocks = [(0, 512), (512, 1024), (1024, 1536), (1536, 2048), (2048, 2304)]
        gnb = k.sb("ggn", [128, 256], F32)
        self.bcast_row_dma(gnb, self.b_norm.ap()[jm:jm + 1, :], 256)
        wa2 = k.sb("gwa2", [17, 2, 512], F32)
        k.dma("sp", wa2[:], self.b_w_a2.ap()[jm].rearrange("d r c -> r d c"), writes=[wa2])
        for hg in range(2):
            m1 = k.mark()
            qT = k.sb("gqT", [128, 2, NTOK], BF16)
            kT = k.sb("gkT", [128, 2, NTOK], BF16)
            V = k.sb("gV", [128, NT, 512], BF16)
            SP = k.sb("gSP", [128, NT, 2, 256], F32)
            O = k.sb("gO", [128, NT, 512], BF16)
            m2 = k.mark()
            wqk = k.sb("gwqk", [128, 8, 512], BF16)
            wv = k.sb("gwv", [128, 8, 512], BF16)
            wa = k.sb("gwa", [128, 8, 32], BF16)
            aT = [k.sb(f"gaT{d}", [17, NTOK], F32) for d in range(2)]
            self.load_w(wqk, wqk[:, :, 0:256], W[:, hg * 256:(hg + 1) * 256], rng=(0, 256))
            self.load_w(wqk, wqk[:, :, 256:512], W[:, 512 + hg * 256:512 + (hg + 1) * 256], rng=(256, 512))
            self.load_w(wv, wv[:], W[:, 1024 + hg * 512:1024 + (hg + 1) * 512])
            self.load_w(wa, wa[:], W[:, 3072:3104])
            pq = [k.ps(f"gpq{j}", [128, 512], F32) for j in range(2)]
            pa_ = [k.ps(f"gpa{j}", [128, 512], F32) for j in range(2)]
            pz = [k.ps(f"gpz{j}", [128, 512], F32) for j in range(2)]
            e1 = [k.sb(f"ge1{j}", [128, 512], F32) for j in range(2)]
            for d in range(2):
                k.op("dve", lambda d=d: nc.vector.memset(aT[d][:], 1.0), writes=[aT[d]])
            cnt = 0
            for (cs, ce) in blocks:
                n = ce - cs
                for ch in range(4):
                    P = pq[cnt % 2]
                    cnt += 1
                    self.proj_feat(P[:, 0:n], P, wqk, ch * 128, 128, cs, ce)
                    if ch < 2:
                        k.op("act", lambda P=P, ch=ch: nc.scalar.activation(qT[:, ch, cs:ce], P[:, 0:n], AF.Copy, scale=128.0 ** -0.5),
                             reads=[P], writes=[R(qT, cs, ce)])
                    else:
                        k.op("dve", lambda P=P, ch=ch: nc.vector.tensor_copy(kT[:, ch - 2, cs:ce], P[:, 0:n]),
                             reads=[P], writes=[R(kT, cs, ce)])
                for d in range(2):
                    P = pa_[d]
                    self.proj_feat(P[0:16, 0:n], P, wa, d * 16, 16, cs, ce)
                    k.op("act", lambda P=P, d=d: nc.scalar.copy(aT[d][0:16, cs:ce], P[0:16, 0:n]), reads=[P], writes=[R(aT[d], cs, ce)])
            for t in range(NT):
                P = pq[t % 2]
                self.proj_tok(P[:], P, t, wv, 0, 512)
                k.op("act", lambda P=P, t=t: nc.scalar.copy(V[:, t, :], P[:]), reads=[P], writes=[R(V, t, t + 1)])
                for d in range(2):
                    Z = pz[d]
                    k.op("pe", lambda Z=Z, d=d, t=t: nc.tensor.matmul(Z[:, 0:256], aT[d][0:17, t * 128:(t + 1) * 128],
                                                                   wa2[0:17, d, hg * 256:(hg + 1) * 256], start=True, stop=True),
                         reads=[R(aT[d], t * 128, (t + 1) * 128), wa2], writes=[Z])
                    E = e1[d]
                    k.op("act", lambda Z=Z, E=E: nc.scalar.activation(E[:, 0:256], Z[:, 0:256], AF.Exp, scale=-1.0), reads=[Z], writes=[E])
                    k.op("act", lambda E=E: nc.scalar.activation(E[:, 256:512], E[:, 0:256], AF.Ln, bias=1.0), reads=[E], writes=[E])
                    k.op("dve", lambda E=E, d=d, t=t: nc.vector.tensor_scalar(SP[:, t, d, :], E[:, 256:512], 1.0 / 16.0, None, ALU.mult),
                         reads=[E], writes=[R(SP, t, t + 1)])
            k.barrier()
            k.release(m2)
            m2 = k.mark()
            S32 = [k.sb(f"gS32{d}", [128, 2, 256], F32) for d in range(2)]
            tS = [k.sb(f"gtS{d}", [128, 2, 256], F32) for d in range(2)]
            Sb = [k.sb(f"gSb{d}", [128, 2, 256], BF16) for d in range(2)]
            EG = [k.sb(f"gEG{d}", [128, 2, 128], F32) for d in range(2)]
            EN = [k.sb(f"gEN{d}", [128, 2, 128], F32) for d in range(2)]
            qt = [k.sb(f"gqt{d}", [128, 2, 128], BF16) for d in range(2)]
            kt = [k.sb(f"gkt{d}", [128, 2, 128], BF16) for d in range(2)]
            AT = [k.sb(f"gAT{d}", [128, 2, 128], BF16) for d in range(2)]
            Ktok = [k.sb(f"gKtok{d}", [128, 2, 128], BF16) for d in range(2)]
            pP = [k.ps(f"gpP{d}", [128, 4, 128], F32) for d in range(2)]
            pO = [k.ps(f"gpO{d}", [128, 2, 256], F32) for d in range(2)]
            pS = [k.ps(f"gpS{d}", [128, 2, 256], F32) for d in range(2)]
            pA = k.ps("gpA", [128, 4, 128], F32)
            pKt = k.ps("gpKt", [128, 8, 128], BF16)
            for d in range(2):
                k.op("dve", lambda d=d: nc.vector.memset(S32[d][:], 0.0), writes=[S32[d]])
                k.op("dve", lambda d=d: nc.vector.memset(Sb[d][:], 0.0), writes=[Sb[d]])
            for step in range(NT):
                for d in range(2):
                    t = (fwd_order, bwd_order)[d][step]
                    tri = self.cf[:, 1 + d, :]
                    mask = self.cf[:, 4 + d, :]
                    lastc = 127 if d == 0 else 0
                    tk = slice(t * 128, (t + 1) * 128)
                    for h in range(2):
                        k.op("pe", lambda d=d, h=h, t=t, tri=tri: nc.tensor.matmul(
                            pP[d][:, h, :], SP[:, t, d, h * 128:(h + 1) * 128], tri, start=True, stop=True),
                            reads=[R(SP, t, t + 1), self.cf], writes=[R(pP[d], h, h + 1)])
                    k.op("act", lambda d=d: nc.scalar.activation(EG[d][:], pP[d][:, 0:2, :], AF.Exp, scale=-1.0), reads=[pP[d]], writes=[EG[d]])
                    k.op("act", lambda d=d: nc.scalar.activation(EN[d][:], pP[d][:, 0:2, :], AF.Exp), reads=[pP[d]], writes=[EN[d]])
                    k.op("dve", lambda d=d, tk=tk: nc.vector.tensor_tensor(qt[d][:], qT[:, :, tk], EG[d][:], ALU.mult),
                         reads=[R(qT, t * 128, (t + 1) * 128), EG[d]], writes=[qt[d]])
                    k.op("dve", lambda d=d, tk=tk: nc.vector.tensor_tensor(kt[d][:], kT[:, :, tk], EN[d][:], ALU.mult),
                         reads=[R(kT, t * 128, (t + 1) * 128), EN[d]], writes=[kt[d]])
                    for h in range(2):
                        k.op("pe", lambda d=d, h=h: nc.tensor.matmul(pA[:, h, :], kt[d][:, h, :], qt[d][:, h, :], start=True, stop=True),
                             reads=[kt[d], qt[d]], writes=[R(pA, h, h + 1)])
                    for h in range(2):
                        k.op("pe", lambda d=d, h=h: nc.tensor.transpose(pKt[:, h, :], kt[d][:, h, :], self.ident_b),
                             reads=[kt[d], self.cb], writes=[R(pKt, h, h + 1)])
                    k.op("dve", lambda d=d, mask=mask: nc.vector.tensor_tensor(AT[d][:], pA[:, 0:2, :], bc_mid(mask, 1, 2), ALU.mult),
                         reads=[pA, self.cf], writes=[AT[d]])
                    k.op("act", lambda d=d: nc.scalar.copy(Ktok[d][:], pKt[:, 0:2, :]), reads=[pKt], writes=[Ktok[d]])
                    for h in range(2):
                        k.op("pe", lambda d=d, h=h, t=t: nc.tensor.matmul(pO[d][:, h, :], AT[d][:, h, :], V[:, t, h * 256:(h + 1) * 256],
                                                                      start=True, stop=False),
                             reads=[AT[d], R(V, t, t + 1)], writes=[R(pO[d], h, h + 1)])
                        k.op("pe", lambda d=d, h=h: nc.tensor.matmul(pO[d][:, h, :], qt[d][:, h, :], Sb[d][:, h, :], start=False, stop=True),
                             reads=[qt[d], Sb[d]], writes=[R(pO[d], h, h + 1)])
                    for h in range(2):
                        k.op("pe", lambda d=d, h=h, t=t: nc.tensor.matmul(pS[d][:, h, :], Ktok[d][:, h, :], V[:, t, h * 256:(h + 1) * 256],
                                                                      start=True, stop=True),
                             reads=[Ktok[d], R(V, t, t + 1)], writes=[R(pS[d], h, h + 1)])
                    sf, sb_ = fwd_order.index(t), bwd_order.index(t)
                    first = (sf <= sb_) if d == 0 else (sb_ < sf)
                    if first:
                        k.op("dve", lambda d=d, t=t: nc.vector.tensor_copy(O[:, t, :], pO[d][:].rearrange("p a b -> p (a b)")),
                             reads=[pO[d]], writes=[R(O, t, t + 1)])
                    else:
                        k.op("dve", lambda d=d, t=t: nc.vector.tensor_tensor(O[:, t, :], pO[d][:].rearrange("p a b -> p (a b)"), O[:, t, :], ALU.add),
                             reads=[pO[d], R(O, t, t + 1)], writes=[R(O, t, t + 1)])
                    k.op("dve", lambda d=d: nc.vector.tensor_tensor(tS[d][:], pS[d][:], S32[d][:], ALU.add),
                         reads=[pS[d], S32[d]], writes=[tS[d]])
                    k.op("dve", lambda d=d, lastc=lastc: nc.vector.tensor_tensor(S32[d][:], tS[d][:], bc_last(EG[d][:, :, lastc], 256), ALU.mult),
                         reads=[tS[d], EG[d]], writes=[S32[d]])
                    k.op("act", lambda d=d: nc.scalar.copy(Sb[d][:], S32[d][:]), reads=[S32[d]], writes=[Sb[d]])
            k.barrier()
            if self.stop == "g_scan":
                self.dump("O", O, O[:], [128, NT, 512])
            self.chk("g_scan")
            k.release(m2)
            m2 = k.mark()
            wr = k.sb("gwr", [128, 8, 512], BF16)
            wout = k.sb("gwout", [128, 4, D], BF16)
            self.load_w(wr, wr[:], W[:, 2048 + hg * 512:2048 + (hg + 1) * 512])
            self.load_w(wout, wout[:], self.b_w_out.ap()[jm, hg * 512:(hg + 1) * 512, :])
            pr = [k.ps(f"gpr{j}", [128, 512], F32) for j in range(2)]
            py = [k.ps(f"gpy{j}", [128, D], F32) for j in range(2)]
            ptr = [k.ps(f"gptr{j}", [128, 8, 128], BF16) for j in range(2)]
            sr = [k.sb(f"gsr{j}", [128, 512], BF16) for j in range(2)]
            y32 = [k.sb(f"gy32{j}", [128, 2, 256], F32) for j in range(2)]
            ysb = [k.sb(f"gys{j}", [128, 512], BF16) for j in range(2)]
            yT = [k.sb(f"gyT{j}", [128, 4, 128], BF16) for j in range(2)]
            xb = [k.sb(f"gxb{j}", [128, D], F32) for j in range(2)]
            tb1 = k.sb("gtb", [128, D], F32)
            st = [k.sb(f"gst{j}", [128, 8], F32) for j in range(2)]
            junk = k.sb("gjunk", [128, 256], BF16)
            otiles = list(range(16 if last else NT))

            def oA(n):
                t, b = otiles[n], n % 2
                self.proj_tok(pr[b][:], pr[b], t, wr, 0, 512)
                k.op("act", lambda: nc.scalar.activation(sr[b][:], pr[b][:], AF.Silu), reads=[pr[b]], writes=[sr[b]])
                k.op("pool", lambda: nc.gpsimd.memset(st[b][:], 0.0), writes=[st[b]])
                for h in range(2):
                    k.op("act", lambda h=h: nc.scalar.activation(junk[:], O[:, t, h * 256:(h + 1) * 256], AF.Square, accum_out=st[b][:, h:h + 1]),
                         reads=[R(O, t, t + 1), st[b]], writes=[junk, st[b]])
                k.op("act", lambda: nc.scalar.activation(st[b][:, 2:4], st[b][:, 0:2], AF.Ln, scale=1.0 / 256, bias=EPS), reads=[st[b]], writes=[st[b]])
                k.op("act", lambda: nc.scalar.activation(st[b][:, 4:6], st[b][:, 2:4], AF.Exp, scale=-0.5), reads=[st[b]], writes=[st[b]])
                k.op("dve", lambda: nc.vector.tensor_tensor(y32[b][:], O[:, t, :].rearrange("p (a e) -> p a e", a=2),
                                                           bc_last(st[b][:, 4:6], 256), ALU.mult),
                     reads=[R(O, t, t + 1), st[b]], writes=[y32[b]])
                k.op("pool", lambda: nc.gpsimd.tensor_tensor(y32[b][:], y32[b][:], bc_mid(gnb[:], 1, 2), ALU.mult),
                     reads=[y32[b], gnb], writes=[y32[b]])
                k.op("dve", lambda: nc.vector.tensor_tensor(ysb[b][:], y32[b][:].rearrange("p a e -> p (a e)"), sr[b][:], ALU.mult),
                     reads=[y32[b], sr[b]], writes=[ysb[b]])

            def oB(n):
                t, b = otiles[n], n % 2
                self.out_proj_tile(t, ysb[b], 4, wout, py[b], yT[b], ptr[b], None, None)

            def oC(n):
                t, b = otiles[n], n % 2
                self.resid_update(t, py[b], xb[b], tb1)

            self.swpipe(len(otiles), [oA, oB, oC])
            k.barrier()
            k.release(m2)
            k.release(m1)
        k.release(m0)

    def na(self, i, jm, last):
        k, nc = self.k, self.nc
        m0 = k.mark()
        W = self.c_w_in.ap()[jm]
        blocks = [(0, 512), (512, 1024), (1024, 1536), (1536, 2048), (2048, 2304)]
        cmask = k.sb("ncm", [128, 64], F32)
        k.dma("sp", cmask[:], self.cmask_in.ap(), writes=[cmask])
        for hg in range(2):
            m1 = k.mark()
            qT = k.sb("nqT", [128, 4, NTOK], BF16)
            kT = k.sb("nkT", [128, 4, NTOK], BF16)
            V = k.sb("nV", [128, NT, 512], BF16)
            BM = k.sb("nBM", [128, 8, 15, 64], BF16)
            src = self.c_rpbx.ap()[jm, hg * 8:(hg + 1) * 8].rearrange("h q (r c) -> q h r c", r=15)
            k.dma("pool", BM[0:64], src, writes=[R(BM, 0, 1)])
            k.dma("pool", BM[64:128], src, writes=[R(BM, 1, 2)])
            k.op("dve", lambda: nc.vector.tensor_tensor(BM[:].rearrange("p h r c -> p (h r) c"), BM[:].rearrange("p h r c -> p (h r) c"),
                                                       bc_mid(cmask[:], 1, 120), ALU.add), reads=[BM, cmask], writes=[BM])
            m2 = k.mark()
            wq = k.sb("nwq", [128, 8, 512], BF16)
            wk = k.sb("nwk", [128, 8, 512], BF16)
            wv = k.sb("nwv", [128, 8, 512], BF16)
            self.load_w(wq, wq[:], W[:, hg * 512:(hg + 1) * 512])
            self.load_w(wk, wk[:], W[:, D + hg * 512:D + (hg + 1) * 512])
            self.load_w(wv, wv[:], W[:, 2 * D + hg * 512:2 * D + (hg + 1) * 512])
            pq = [k.ps(f"npq{j}", [128, 512], F32) for j in range(2)]
            cnt = 0
            for (cs, ce) in blocks:
                n = ce - cs
                for ch in range(8):
                    P = pq[cnt % 2]
                    cnt += 1
                    self.proj_feat(P[:, 0:n], P, wq if ch < 4 else wk, (ch % 4) * 128, 128, cs, ce)
                    if ch < 4:
                        k.op("act", lambda P=P, ch=ch: nc.scalar.activation(qT[:, ch, cs:ce], P[:, 0:n], AF.Copy, scale=0.125),
                             reads=[P], writes=[R(qT, cs, ce)])
                    else:
                        k.op("dve", lambda P=P, ch=ch: nc.vector.tensor_copy(kT[:, ch - 4, cs:ce], P[:, 0:n]),
                             reads=[P], writes=[R(kT, cs, ce)])
            for t in range(NT):
                P = pq[t % 2]
                self.proj_tok(P[:], P, t, wv, 0, 512)
                k.op("act", lambda P=P, t=t: nc.scalar.copy(V[:, t, :], P[:]), reads=[P], writes=[R(V, t, t + 1)])
            k.barrier()
            k.release(m2)
            m2 = k.mark()
            wout = k.sb("nwout", [128, 4, D], BF16)
            self.load_w(wout, wout[:], self.c_w_out.ap()[jm, hg * 512:(hg + 1) * 512, :])
            Sc = [k.sb(f"nSc{j}", [128, 896], F32) for j in range(2)]
            Pb = [k.sb(f"nPb{j}", [128, 896], BF16) for j in range(2)]
            PT = [k.sb(f"nPT{j}", [128, 7, 128], BF16) for j in range(2)]
            pS = [k.ps(f"npS{j}", [128, 1024], F32) for j in range(2)]
            pT = [k.ps(f"npT{j}", [128, 8, 128], BF16) for j in range(1)]
            pO = k.ps("npO", [128, 8, 64], F32)
            py = [k.ps(f"npy{j}", [128, D], F32) for j in range(1)]
            ptr = pT
            ysb = [k.sb(f"nys{j}", [128, 512], BF16) for j in range(2)]
            yT = [k.sb(f"nyT{j}", [128, 4, 128], BF16) for j in range(2)]
            xb = [k.sb(f"nxb{j}", [128, D], F32) for j in range(2)]
            tb1 = k.sb("ntb", [128, D], F32)
            tiles_ = list(range(16 if last else NT))
            units = [(n_, t, h) for n_, t in enumerate(tiles_) for h in range(8)]
            st = [k.sb(f"nst4{j}", [128, 4], F32) for j in range(4)]
            Pb = Pb + [k.sb("nPb2", [128, 896], BF16)]

            def geo(t):
                if t < 16:
                    rs = [min(max(2 * t - 4, 0), 24), min(max(2 * t - 3, 0), 24)]
                    nl = 4 if t in (0, 1, 14, 15) else 5
                    return rs, rs[0], nl, 0, list(range(nl)) + [5, 6]
                return None, 0, 0, 640, [5, 6]

            def stA(u):
                n_, t, h = units[u]
                rs, ks, nl, c_lo, chunks = geo(t)
                if h == 0 and nl:
                    for sb_ in Sc:
                        for hf in range(2):
                            i0 = rs[hf] - ks
                            if i0 > 0:
                                k.op("dve", lambda sb_=sb_, hf=hf, i0=i0: nc.vector.memset(sb_[hf * 64:(hf + 1) * 64, 0:i0 * 64], NEG), writes=[sb_])
                            if (i0 + 8) * 64 < 640:
                                k.op("dve", lambda sb_=sb_, hf=hf, i0=i0: nc.vector.memset(sb_[hf * 64:(hf + 1) * 64, (i0 + 8) * 64:640], NEG), writes=[sb_])
                pb = (h % 2) * 64
                c = h // 2
                S_, P_, s_, ps_ = Sc[u % 2], Pb[u % 3], st[u % 4], pS[u % 2]
                qa = qT[pb:pb + 64, c, t * 128:(t + 1) * 128]
                if nl:
                    k0 = ks * 64
                    k.op("pe", lambda: nc.tensor.matmul(ps_[:, 0:512], qa, kT[pb:pb + 64, c, k0:k0 + 512], start=True, stop=True),
                         reads=[R(qT, t * 128, (t + 1) * 128), R(kT, k0, k0 + 512)], writes=[R(ps_, 0, 512)], rg=pb)
                    if nl == 5:
                        k.op("pe", lambda: nc.tensor.matmul(ps_[:, 512:640], qa, kT[pb:pb + 64, c, k0 + 512:k0 + 640], start=True, stop=True),
                             reads=[R(qT, t * 128, (t + 1) * 128), R(kT, k0 + 512, k0 + 640)], writes=[R(ps_, 512, 640)], rg=pb)
                k.op("pe", lambda: nc.tensor.matmul(ps_[:, 640:896], qa, kT[pb:pb + 64, c, T:NTOK], start=True, stop=True),
                     reads=[R(qT, t * 128, (t + 1) * 128), R(kT, T, NTOK)], writes=[R(ps_, 640, 896)], rg=pb)
                if nl:
                    for hf in range(2):
                        r = 2 * t + hf
                        i0 = rs[hf] - ks
                        dr0 = rs[hf] - r + 7
                        k.op("dve", lambda hf=hf, i0=i0, dr0=dr0: nc.vector.tensor_tensor(
                            S_[hf * 64:(hf + 1) * 64, i0 * 64:(i0 + 8) * 64], ps_[hf * 64:(hf + 1) * 64, i0 * 64:(i0 + 8) * 64],
                            BM[hf * 64:(hf + 1) * 64, h, dr0:dr0 + 8, :].rearrange("p r c -> p (r c)"), ALU.add),
                            reads=[ps_, BM], writes=[S_])
                k.op("act", lambda: nc.scalar.copy(S_[:, 640:896], ps_[:, 640:896]), reads=[ps_], writes=[S_])
                k.op("dve", lambda: nc.vector.memset(s_[:], 0.0), writes=[s_])
                k.op("dve", lambda: nc.vector.tensor_reduce(s_[:, 0:1], S_[:, c_lo:896], AX.X, ALU.max, negate=True), reads=[S_], writes=[s_])
                k.op("act", lambda: nc.scalar.activation(P_[:, c_lo:896], S_[:, c_lo:896], AF.Exp, bias=s_[:, 0:1], accum_out=s_[:, 1:2]),
                     reads=[S_, s_], writes=[P_, s_])

            def stB(u):
                n_, t, h = units[u]
                rs, ks, nl, c_lo, chunks = geo(t)
                P_, PT_, pT_ = Pb[u % 3], PT[u % 2], pT[0]
                for cc in chunks:
                    k.op("pe", lambda cc=cc: nc.tensor.transpose(pT_[:, cc, :], P_[:, cc * 128:(cc + 1) * 128], self.ident_b),
                         reads=[P_, self.cb], writes=[R(pT_, cc, cc + 1)])
                if nl:
                    k.op("act", lambda: nc.scalar.copy(PT_[:, 0:nl, :], pT_[:, 0:nl, :]), reads=[pT_], writes=[PT_])
                k.op("dve", lambda: nc.vector.tensor_copy(PT_[:, 5:7, :], pT_[:, 5:7, :]), reads=[pT_], writes=[PT_])

            def stC(u):
                n_, t, h = units[u]
                rs, ks, nl, c_lo, chunks = geo(t)
                PT_, s_ = PT[u % 2], st[u % 4]
                for n2, cc in enumerate(chunks):
                    vt = (ks // 2 + cc) if cc < 5 else (16 + cc - 5)
                    k.op("pe", lambda cc=cc, vt=vt, n2=n2: nc.tensor.matmul(
                        pO[:, h, :], PT_[:, cc, :], V[:, vt, h * 64:(h + 1) * 64], start=(n2 == 0), stop=(n2 == len(chunks) - 1)),
                        reads=[PT_, R(V, vt, vt + 1)], writes=[R(pO, h, h + 1)])
                k.op("dve", lambda: nc.vector.reciprocal(s_[:, 2:3], s_[:, 1:2]), reads=[s_], writes=[s_])
                yb = ysb[n_ % 2]
                k.op("dve", lambda: nc.vector.tensor_scalar(yb[:, h * 64:(h + 1) * 64], pO[:, h, :], s_[:, 2:3], None, ALU.mult),
                     reads=[R(pO, h, h + 1), s_], writes=[yb])
                if h == 7:
                    self.out_proj_tile(t, ysb[n_ % 2], 4, wout, py[0], yT[n_ % 2], ptr[0], xb[n_ % 2], tb1)

            mode = os.environ.get("NA_PIPE", "4")
            if mode == "1":
                self.swpipe(len(units), [lambda u: (stA(u), stB(u), stC(u))])
            elif mode == "2":
                self.swpipe(len(units), [stA, lambda u: (stB(u), stC(u))])
            elif mode == "4":
                n_u = len(units)
                for i_ in range(n_u + 2):
                    if 0 <= i_ - 2 < n_u:
                        stC(i_ - 2)
                    if 0 <= i_ - 1 < n_u:
                        stB(i_ - 1)
                    if i_ < n_u:
                        stA(i_)
            else:
                self.swpipe(len(units), [stA, stB, stC])
            k.barrier()
            k.release(m2)
            k.release(m1)
        k.release(m0)

    def final_norm(self):
        k, nc = self.k, self.nc
        m = k.mark()
        gn = k.sb("fng", [128, D], F32)
        self.bcast_row_dma(gn, self.norm_final.ap()[0:1, :], D)
        xt = [k.sb(f"fx{j}", [128, D], F32) for j in range(2)]
        yt = [k.sb(f"fy{j}", [128, D], F32) for j in range(2)]
        junk = k.sb("fjunk", [128, D], BF16)
        st = [k.sb(f"fst{j}", [128, 4], F32) for j in range(2)]
        for t in range(16):
            x, y, s = xt[t % 2], yt[t % 2], st[t % 2]
            k.dma("sp", x[:], self.xt_ap(t), reads=[R(self.xres, t, t + 1)], writes=[x])
            k.op("dve", lambda s=s: nc.vector.memset(s[:], 0.0), writes=[s])
            k.op("act", lambda x=x, s=s: nc.scalar.activation(junk[:], x[:], AF.Square, accum_out=s[:, 0:1]),
                 reads=[x, s], writes=[junk, s])
            k.op("act", lambda s=s: nc.scalar.activation(s[:, 1:2], s[:, 0:1], AF.Ln, scale=1.0 / D, bias=EPS), reads=[s], writes=[s])
            k.op("act", lambda s=s: nc.scalar.activation(s[:, 2:3], s[:, 1:2], AF.Exp, scale=-0.5), reads=[s], writes=[s])
            k.op("dve", lambda x=x, y=y, s=s: nc.vector.scalar_tensor_tensor(y[:], x[:], s[:, 2:3], gn[:], ALU.mult, ALU.mult),
                 reads=[x, s, gn], writes=[y])
            k.dma("sp", self.out.ap()[t * 128:(t + 1) * 128, :], y[:], reads=[y], writes=[R(self.out, t, t + 1)])
        k.barrier()
        k.release(m)

    def build(self):
        k = self.k
        self.decl_inputs()
        self.setup_consts()
        for t in range(16):
            k.dma("sp", self.xres.ap()[t * 128:(t + 1) * 128, :], self.x_in.ap()[t * 128:(t + 1) * 128, :], writes=[R(self.xres, t, t + 1)])
        for t in range(2):
            k.dma("sp", self.xres.ap()[T + t * 128:T + (t + 1) * 128, :], self.ctx_in.ap()[t * 128:(t + 1) * 128, :], writes=[R(self.xres, 16 + t, 17 + t)])
        try:
            self.layers()
        except StopIteration:
            pass
        k.finish()
        k.close()

    def dump(self, name, buf, ap, shape):
        if not self.debug_x:
            return
        o = self.k.dram("dbg_" + name, list(shape), F32, kind="ExternalOutput")
        self.k.dma("pool", o.ap(), ap, reads=[buf], writes=[o])

    def chk(self, name):
        if self.stop == name:
            if name == "norm":
                self.dump("hT", self.hT, self.hT[:], [128, 8, NTOK])
                self.dump("mods", self.mods, self.mods[:], [128, 6, D])
            raise StopIteration

    def layers(self):
        self.chk("copy")
        for i in self.layer_list:
            last = i == DEPTH - 1
            kind, j = i % 3, i // 3
            tiles = list(range(NT))
            self.ada(i, 0)
            self.chk("ada")
            self.norm_to_hT(tiles, self.xt_ap)
            self.chk("norm")
            if kind == 0:
                self.mlstm(i, j, last)
            elif kind == 1:
                self.gla(i, j, last)
            else:
                self.na(i, j, last)
            self.chk("mixer")
            self.ada(i, 1)
            self.norm_to_hT(list(range(16)) if last else tiles, self.xt_ap)
            self.ffn(i, last)
            self.chk("ffn")
        self.final_norm()


def _consts():
    i = np.arange(128)
    ident = np.eye(128, dtype=np.float32)
    triu = (i[:, None] <= i[None, :]).astype(np.float32)
    tril = (i[:, None] >= i[None, :]).astype(np.float32)
    ones = np.ones((128, 128), np.float32)
    return np.stack([ident, triu, tril, ones, triu, tril]).astype(np.float32)


def prep_inputs(inp):
    f = lambda a: np.ascontiguousarray(np.asarray(a, dtype=np.float32))
    shared = {}
    for n in ("w_ada", "b_ada", "norm_mix", "norm_ffn", "w_up", "w_down", "a_w_in", "a_b_gate", "a_w_out",
              "b_w_in", "b_norm", "b_w_out", "c_w_in", "c_w_out"):
        shared[n] = f(inp[n])
    shared["norm_final"] = f(inp["norm_final"]).reshape(1, D)
    cw = np.concatenate([f(inp["ffn_conv_w"]), f(inp["ffn_conv_b"])[:, None, :]], axis=1)
    shared["ffn_cw"] = f(cw.reshape(DEPTH, 4, 44, 128).transpose(0, 3, 1, 2).reshape(DEPTH, 128, 4 * 44))
    acw = np.concatenate([f(inp["a_conv_w"]), f(inp["a_conv_b"])[:, None, :]], axis=1)
    shared["a_cw"] = f(acw.reshape(2, 4, 8, 128).transpose(0, 3, 1, 2).reshape(2, 128, 32))
    shared["b_w_a2e"] = f(np.concatenate([f(inp["b_w_a2"]), f(inp["b_b_a"])[:, :, None, :]], axis=2))
    qc = np.arange(64)
    dc = np.clip(qc[None, :] - qc[:, None] + 15, 0, 30)
    rp = f(inp["c_rpb"])
    rx = rp[:, :, :, dc]
    shared["c_rpbx"] = f(rx.transpose(0, 1, 3, 2, 4).reshape(1, 16, 64, 15 * 64))
    cst = np.clip(qc - 8, 0, 48)
    cm = np.where((qc[None, :] >= cst[:, None]) & (qc[None, :] < cst[:, None] + 16), 0.0, NEG).astype(np.float32)
    shared["cmask"] = f(np.concatenate([cm, cm], axis=0))
    shared["consts"] = _consts()
    cctx_t = f(np.asarray(inp["c_ctx"]).reshape(8, 128).T)
    maps = []
    for b in range(8):
        mdict = dict(shared)
        mdict["x"] = f(inp["x"][b])
        mdict["ctx"] = f(inp["ctx"][b])
        mdict["c_t"] = f(np.asarray(inp["c"][b]).reshape(8, 128).T)
        mdict["cctx_t"] = cctx_t
        maps.append(mdict)
    return maps


_CACHE = {}


def kernel(**inputs):
    if "mk" not in _CACHE:
        _CACHE["mk"] = MK()
    mk = _CACHE["mk"]
    maps = prep_inputs(inputs)
    res = run_bass_kernel_spmd(mk.nc, maps, core_ids=list(range(8)))
    return np.stack([np.asarray(r["out"], dtype=np.float32) for r in res.results], axis=0)
```

```python
import os
import numpy as np
import concourse.bass as bass
import concourse.mybir as mybir
from concourse.bass_utils import run_bass_kernel_spmd

F32 = mybir.dt.float32
BF16 = mybir.dt.bfloat16
AF = mybir.ActivationFunctionType
ALU = mybir.AluOpType
AX = mybir.AxisListType

ERA = 6000
NDMASEM = 12
INF = 1 << 60

D = 1024
T = 2048
TCX = 256
NTOK = T + TCX
NT = NTOK // 128
DEPTH = 4
FF = 2816
NFC = FF // 128
EPS = 1e-6
NEG = -30000.0


class Buf:
    def __init__(self, t, name):
        self.t = t
        self.name = name
        self.writes = []
        self.reads = []

    def __getitem__(self, idx):
        return self.t[idx]

    def ap(self):
        return self.t.ap()


class Acc:
    def __init__(self, buf, lo=0, hi=INF):
        self.buf, self.lo, self.hi = buf, lo, hi


def R(buf, lo=0, hi=INF):
    return Acc(buf, lo, hi)


def _acc(a):
    return a if isinstance(a, Acc) else Acc(a)


class KB:
    def __init__(self):
        self.nc = bass.Bass("TRN2", target_bir_lowering=False)
        nc = self.nc
        self.eng = {"pe": nc.tensor, "act": nc.scalar, "dve": nc.vector, "pool": nc.gpsimd, "sp": nc.sync}
        self.cnt = {e: 0 for e in self.eng}
        self.sems = {e: [] for e in self.eng}
        self.waited = {e: {} for e in self.eng}
        self.dma_n = {e: 0 for e in self.eng}
        self.dma_sems = {e: [] for e in self.eng}
        self.last_tok = {e: None for e in self.eng}
        self.dma_tok = {e: {} for e in self.eng}
        self._ctx = []
        self.nsem = 0
        self.sem_ids = {}

    def _enter(self, cm):
        v = cm.__enter__()
        self._ctx.append(cm)
        return v

    def mark(self):
        return len(self._ctx)

    def release(self, m):
        while len(self._ctx) > m:
            self._ctx.pop().__exit__(None, None, None)

    def close(self):
        self.release(0)

    def sem(self, name):
        self.nsem += 1
        s = self._enter(self.nc.semaphore(self._uname(name)))
        self.sem_ids[id(s)] = s
        return s

    def _uname(self, name):
        self.uid = getattr(self, "uid", 0) + 1
        return f"{name}_{self.uid}"

    def sb(self, name, shape, dt):
        name = self._uname(name)
        return Buf(self._enter(self.nc.sbuf_tensor(name, list(shape), dt)), name)

    def ps(self, name, shape, dt=F32):
        name = self._uname(name)
        return Buf(self._enter(self.nc.psum_tensor(name, list(shape), dt)), name)

    def dram(self, name, shape, dt, kind="Internal"):
        return Buf(self.nc.dram_tensor(name, list(shape), dt, kind=kind), name)

    def _eng_token(self, e):
        n = self.cnt[e]
        era = n // ERA
        while len(self.sems[e]) <= era:
            self.sems[e].append(self.sem(f"s_{e}_{len(self.sems[e])}"))
        self.cnt[e] = n + 1
        return (self.sems[e][era], (n % ERA) + 1)

    def _wait(self, e, token):
        sem, val = token
        w = self.waited[e]
        key = id(sem)
        if w.get(key, 0) >= val:
            return
        w[key] = val
        self.eng[e].wait_ge(sem, val)

    def _deps(self, e, reads, writes, is_dma):
        toks = []
        for a in reads:
            a = _acc(a)
            for (lo, hi, tok, we) in a.buf.writes:
                if lo < a.hi and a.lo < hi:
                    toks.append(tok)
        for a in writes:
            a = _acc(a)
            for (lo, hi, tok, we) in a.buf.writes:
                if lo < a.hi and a.lo < hi and (is_dma or we != e):
                    toks.append(tok)
            for (lo, hi, tok, re) in a.buf.reads:
                if lo < a.hi and a.lo < hi and (is_dma or re != e):
                    toks.append(tok)
        for t in toks:
            self._wait(e, t)

    def _record(self, e, tok, reads, writes, is_dma):
        tag = ("dma", e) if is_dma else e
        for a in reads:
            a = _acc(a)
            b = a.buf
            if not is_dma:
                b.reads = [r for r in b.reads if not (r[3] == tag and r[0] >= a.lo and r[1] <= a.hi)]
            b.reads.append((a.lo, a.hi, tok, tag))
        for a in writes:
            a = _acc(a)
            b = a.buf
            b.writes = [w for w in b.writes if not (w[0] >= a.lo and w[1] <= a.hi)]
            b.reads = [r for r in b.reads if not (r[0] >= a.lo and r[1] <= a.hi)]
            b.writes.append((a.lo, a.hi, tok, tag))

    def op(self, e, fn, reads=(), writes=(), rg=0):
        if e == "pe":
            if rg != getattr(self, "pe_rg", 0) and self.last_tok["pe"] is not None:
                self._wait("pe", self.last_tok["pe"])
            self.pe_rg = rg
        self._deps(e, reads, writes, False)
        ins = fn()
        tok = self._eng_token(e)
        ins.then_inc(tok[0], 1)
        self.last_tok[e] = tok
        self._record(e, tok, reads, writes, False)
        return tok

    def dma(self, e, out_ap, in_ap, reads=(), writes=(), **kw):
        n = self.dma_n[e]
        if not self.dma_sems[e]:
            self.dma_sems[e] = [self.sem(f"d_{e}_{i}") for i in range(NDMASEM)]
        sem = self.dma_sems[e][n % NDMASEM]
        prev = 16 * (n // NDMASEM)
        if prev:
            self._wait(e, (sem, prev))
        self._deps(e, reads, writes, True)
        self.eng[e].dma_start(out=out_ap, in_=in_ap, **kw).then_inc(sem, 16)
        self.dma_n[e] = n + 1
        tok = (sem, prev + 16)
        assert prev + 16 < 32000, "dma sem overflow"
        self.dma_tok[e][n % NDMASEM] = tok
        self._record(e, tok, reads, writes, True)
        return tok

    def barrier(self):
        toks = [t for t in self.last_tok.values() if t is not None]
        for e in self.dma_tok:
            toks += list(self.dma_tok[e].values())
        for e in self.eng:
            for t in toks:
                self._wait(e, t)

    def finish(self):
        self.barrier()


def bc_last(ap, n):
    return bass.AP(ap.tensor, ap.offset, [list(d) for d in ap.ap] + [[0, n]])


def bc_mid(ap, pos, n):
    l = [list(d) for d in ap.ap]
    l.insert(pos, [0, n])
    return bass.AP(ap.tensor, ap.offset, l)


class MK:
    def __init__(self, nlayers=DEPTH, debug_x=False, stop=None, layer_list=None):
        self.stop = stop
        self.layer_list = list(range(nlayers)) if layer_list is None else layer_list
        self.k = KB()
        self.nc = self.k.nc
        self.nlayers = nlayers
        self.debug_x = debug_x
        self.build()

    def decl_inputs(self):
        k = self.k
        I = lambda n, s: k.dram(n, s, F32, kind="ExternalInput")
        self.x_in = I("x", [T, D])
        self.ctx_in = I("ctx", [TCX, D])
        self.c_t = I("c_t", [128, 8])
        self.cctx_t = I("cctx_t", [128, 8])
        self.w_ada = I("w_ada", [DEPTH, D, 6 * D])
        self.b_ada = I("b_ada", [DEPTH, 6 * D])
        self.norm_mix = I("norm_mix", [DEPTH, D])
        self.norm_ffn = I("norm_ffn", [DEPTH, D])
        self.w_up = I("w_up", [DEPTH, D, 2 * FF])
        self.ffn_cw = I("ffn_cw", [DEPTH, 128, 4 * 44])
        self.w_down = I("w_down", [DEPTH, FF, D])
        self.a_w_in = I("a_w_in", [2, D, 3104])
        self.a_b_gate = I("a_b_gate", [2, 32])
        self.a_cw = I("a_cw", [2, 128, 4 * 8])
        self.a_w_out = I("a_w_out", [2, D, D])
        self.b_w_in = I("b_w_in", [1, D, 3104])
        self.b_w_a2 = I("b_w_a2e", [1, 2, 17, 512])
        self.b_norm = I("b_norm", [1, 256])
        self.b_w_out = I("b_w_out", [1, D, D])
        self.c_w_in = I("c_w_in", [1, D, 3 * D])
        self.c_rpbx = I("c_rpbx", [1, 16, 64, 15 * 64])
        self.cmask_in = I("cmask", [128, 64])
        self.c_w_out = I("c_w_out", [1, D, D])
        self.norm_final = I("norm_final", [1, D])
        self.consts = I("consts", [6, 128, 128])
        self.out = k.dram("out", [T, D], F32, kind="ExternalOutput")
        self.xres = k.dram("xres", [NTOK, D], F32, kind="ExternalOutput" if self.debug_x else "Internal")

    @staticmethod
    def swpipe(n, stages, skew=1):
        S = len(stages)
        for i in range(n + (S - 1) * skew):
            for si, fn in enumerate(stages):
                u = i - si * skew
                if 0 <= u < n:
                    fn(u)

    def xt_ap(self, t):
        return self.xres.ap()[t * 128:(t + 1) * 128, :]

    def bcast_row_dma(self, dstbuf, row_ap_1xn, n):
        src = bass.AP(row_ap_1xn.tensor, row_ap_1xn.offset, [[0, 128], [1, n]])
        self.k.dma("sp", dstbuf[:], src, writes=[dstbuf])

    def load_w(self, dstbuf, dst_ap, src2d, rng=None):
        src = src2d.rearrange("(kc k) n -> k kc n", k=128)
        w = [dstbuf] if rng is None else [R(dstbuf, *rng)]
        return self.k.dma("pool", dst_ap, src, writes=w)

    def setup_consts(self):
        k, nc = self.k, self.nc
        self.cf = k.sb("cf", [128, 6, 128], F32)
        self.cb = k.sb("cb", [128, 6, 128], BF16)
        k.dma("sp", self.cf[:], self.consts.ap().rearrange("c p n -> p c n"), writes=[self.cf])
        k.dma("pool", self.cb[:], self.consts.ap().rearrange("c p n -> p c n"), writes=[self.cb])
        self.ident_b = self.cb[:, 0, :]
        self.triu_f = self.cf[:, 1, :]
        self.tril_f = self.cf[:, 2, :]
        self.ones_f = self.cf[:, 3, :]
        self.sbc = k.sb("sbc", [128, 2, 8, 128], BF16)
        cs = k.sb("cs", [128, 2, 8], F32)
        k.dma("sp", cs[:, 0, :], self.c_t.ap(), writes=[cs])
        k.dma("sp", cs[:, 1, :], self.cctx_t.ap(), writes=[cs])
        cs2 = k.sb("cs2", [128, 2, 8], F32)
        k.op("act", lambda: nc.scalar.activation(cs2[:], cs[:], AF.Silu), reads=[cs], writes=[cs2])
        k.op("dve", lambda: nc.vector.tensor_copy(self.sbc[:].rearrange("p a b m -> p (a b) m"),
                                                  bc_last(cs2[:].rearrange("p a b -> p (a b)"), 128)),
             reads=[cs2], writes=[self.sbc])
        self.hT = k.sb("hT", [128, 8, NTOK], BF16)
        self.mods = k.sb("mods", [128, 6, D], F32)
        self.ssz = k.sb("ssz", [128, 4], F32)

    def ada(self, i, phase):
        k, nc = self.k, self.nc
        m = k.mark()
        wbuf = [k.sb(f"adaw{j}", [128, 8, 512], BF16) for j in range(2)]
        bb = k.sb("adab", [128, 3 * D], F32)
        gn = k.sb("adag", [128, D], F32)
        pp = [k.ps(f"adap{j}", [128, 512], F32) for j in range(2)]
        c0 = phase * 3 * D
        self.bcast_row_dma(bb, self.b_ada.ap()[i:i + 1, c0:c0 + 3 * D], 3 * D)
        g_src = (self.norm_mix if phase == 0 else self.norm_ffn).ap()[i:i + 1, :]
        self.bcast_row_dma(gn, g_src, D)
        n = 0
        for blk in range(6):
            wb = wbuf[blk % 2]
            self.load_w(wb, wb[:], self.w_ada.ap()[i, :, c0 + blk * 512:c0 + (blk + 1) * 512])
            part, half = blk // 2, blk % 2
            for lc in range(2):
                p = pp[n % 2]
                n += 1
                for kc in range(8):
                    k.op("pe", lambda kc=kc, p=p, wb=wb, lc=lc: nc.tensor.matmul(
                        p[:], self.sbc[:, lc, kc, :], wb[:, kc, :], start=(kc == 0), stop=(kc == 7)),
                        reads=[self.sbc, wb], writes=[p])
                bsl = bb[:, blk * 512:(blk + 1) * 512]
                dst = self.mods[:, lc * 3 + (1, 0, 2)[part], half * 512:(half + 1) * 512]
                if part == 1:
                    tmp = gn[:, half * 512:(half + 1) * 512]
                    k.op("dve", lambda p=p, bsl=bsl, dst=dst: nc.vector.scalar_tensor_tensor(
                        dst, p[:], 1.0, bsl, ALU.add, ALU.add), reads=[p, bb], writes=[self.mods])
                    k.op("dve", lambda dst=dst, tmp=tmp: nc.vector.tensor_tensor(dst, dst, tmp, ALU.mult),
                         reads=[self.mods, gn], writes=[self.mods])
                else:
                    k.op("dve", lambda p=p, bsl=bsl, dst=dst: nc.vector.tensor_tensor(dst, p[:], bsl, ALU.add),
                         reads=[p, bb], writes=[self.mods])
        k.barrier()
        k.release(m)

    def norm_to_hT(self, tiles, src_fn):
        k, nc = self.k, self.nc
        m = k.mark()
        NB = 3
        xt = [k.sb(f"nx{j}", [128, D], F32) for j in range(NB)]
        junk = k.sb("njunk", [128, D], BF16)
        tmp = [k.sb(f"ntmp{j}", [128, D], F32) for j in range(NB)]
        hb = [k.sb(f"nhb{j}", [128, D], BF16) for j in range(NB)]
        st = [k.sb(f"nst{j}", [128, 4], F32) for j in range(NB)]
        pt = [k.ps(f"npt{j}", [128, 8, 128], BF16) for j in range(2)]

        def stA(n):
            t = tiles[n]
            lc = 0 if t < 16 else 1
            x, s, tm, h = xt[n % NB], st[n % NB], tmp[n % NB], hb[n % NB]
            k.dma("sp", x[:], src_fn(t), reads=[R(self.xres, t, t + 1)], writes=[x])
            k.op("pool", lambda: nc.gpsimd.memset(s[:], 0.0), writes=[s])
            k.op("act", lambda: nc.scalar.activation(junk[:], x[:], AF.Square, accum_out=s[:, 0:1]), reads=[x, s], writes=[junk, s])
            k.op("act", lambda: nc.scalar.activation(s[:, 1:2], s[:, 0:1], AF.Ln, scale=1.0 / D, bias=EPS), reads=[s], writes=[s])
            k.op("act", lambda: nc.scalar.activation(s[:, 2:3], s[:, 1:2], AF.Exp, scale=-0.5), reads=[s], writes=[s])
            k.op("dve", lambda: nc.vector.scalar_tensor_tensor(tm[:], x[:], s[:, 2:3], self.mods[:, lc * 3 + 0, :], ALU.mult, ALU.mult),
                 reads=[x, s, self.mods], writes=[tm])
            k.op("pool", lambda: nc.gpsimd.tensor_tensor(h[:], tm[:], self.mods[:, lc * 3 + 1, :], ALU.add),
                 reads=[tm, self.mods], writes=[h])

        def stB(n):
            t = tiles[n]
            h, p = hb[n % NB], pt[n % 2]
            for kc in range(8):
                k.op("pe", lambda kc=kc: nc.tensor.transpose(p[:, kc, :], h[:, kc * 128:(kc + 1) * 128], self.ident_b),
                     reads=[h, self.cb], writes=[p])
            k.op("act", lambda: nc.scalar.copy(self.hT[:, :, t * 128:(t + 1) * 128], p[:]),
                 reads=[p], writes=[R(self.hT, t * 128, (t + 1) * 128)])

        self.swpipe(len(tiles), [stA, stB])
        k.barrier()
        k.release(m)

    def resid_update(self, t, ypsum, xb, tb):
        k, nc = self.k, self.nc
        lc = 0 if t < 16 else 1
        k.dma("sp", xb[:], self.xt_ap(t), reads=[R(self.xres, t, t + 1)], writes=[xb])
        k.op("dve", lambda: nc.vector.tensor_tensor(tb[:], ypsum[:], self.mods[:, lc * 3 + 2, :], ALU.mult),
             reads=[ypsum, self.mods], writes=[tb])
        k.op("dve", lambda: nc.vector.tensor_tensor(xb[:], xb[:], tb[:], ALU.add), reads=[xb, tb], writes=[xb])
        k.dma("sp", self.xt_ap(t), xb[:], reads=[xb], writes=[R(self.xres, t, t + 1)])

    def conv_evac(self, dst_f32, p, cw, j, ncols, has_left, has_right, nout):
        k, nc = self.k, self.nc
        o = 1 if has_left else 0
        k.op("act", lambda: nc.scalar.activation(dst_f32[:, 0:nout], p[:, o:o + nout], AF.Identity,
                                                 scale=cw[:, 1, j:j + 1], bias=cw[:, 3, j:j + 1]),
             reads=[p, cw], writes=[dst_f32])
        lo = 0 if has_left else 1
        k.op("dve", lambda: nc.vector.scalar_tensor_tensor(dst_f32[:, lo:nout], p[:, o + lo - 1:o + nout - 1],
                                                           cw[:, 0, j:j + 1], dst_f32[:, lo:nout], ALU.mult, ALU.add),
             reads=[p, cw, dst_f32], writes=[dst_f32])
        hi = nout if has_right else nout - 1
        k.op("dve", lambda: nc.vector.scalar_tensor_tensor(dst_f32[:, 0:hi], p[:, o + 1:o + hi + 1],
                                                           cw[:, 2, j:j + 1], dst_f32[:, 0:hi], ALU.mult, ALU.add),
             reads=[p, cw, dst_f32], writes=[dst_f32])

    @staticmethod
    def tblocks(tiles_lat=True, tiles_ctx=True):
        bl = []
        if tiles_lat:
            bl += [(s, s + 256, 0, T) for s in range(0, T, 256)]
        if tiles_ctx:
            bl += [(T, T + 256, T, NTOK)]
        return bl

    def ffn(self, i, last):
        k, nc = self.k, self.nc
        m = k.mark()
        cw = k.sb("fcw", [128, 4, 44], F32)
        k.dma("sp", cw[:], self.ffn_cw.ap()[i].rearrange("p (a j) -> p a j", a=4), writes=[cw])
        wd = k.sb("fwd", [128, NFC, D], BF16)
        wa = [k.sb(f"fwa{j}", [128, 8, 256], BF16) for j in range(2)]
        wg = [k.sb(f"fwg{j}", [128, 8, 256], BF16) for j in range(2)]
        actT = k.sb("factT", [128, NFC, 768], BF16)
        pa = [k.ps(f"fpa{j}", [128, 512], F32) for j in range(2)]
        pg = [k.ps(f"fpg{j}", [128, 512], F32) for j in range(2)]
        py = [k.ps(f"fpy{j}", [128, D], F32) for j in range(2)]
        ua = [k.sb(f"fua{j}", [128, 384], F32) for j in range(2)]
        ug = [k.sb(f"fug{j}", [128, 384], F32) for j in range(2)]
        xb = [k.sb(f"fxb{j}", [128, D], F32) for j in range(2)]
        tb1 = k.sb("ftb", [128, D], F32)
        tb = [tb1, tb1]
        groups = [(j, j + 2) for j in range(0, NFC, 2)]
        tgs = [[(0, 384, 0, T), (384, 768, 0, T)], [(768, 1152, 0, T), (1152, 1536, 0, T)],
               [(1536, 1920, 0, T), (1920, 2048, 0, T)] + ([] if last else [(T, NTOK, T, NTOK)])]
        cnt = 0
        gi = 0
        wd_loaded = False
        for tg in tgs:
            base = tg[0][0]
            for (j0, j1) in groups:
                wab, wgb = wa[gi % 2], wg[gi % 2]
                gi += 1
                nj = j1 - j0
                self.load_w(wab, wab[:, :, 0:nj * 128], self.w_up.ap()[i, :, j0 * 128:j1 * 128])
                self.load_w(wgb, wgb[:, :, 0:nj * 128], self.w_up.ap()[i, :, FF + j0 * 128:FF + j1 * 128])
                if not wd_loaded and gi == 2:
                    wd_loaded = True
                    for q in range(4):
                        jj0, jj1 = (0, 6, 12, 17, 22)[q], (0, 6, 12, 17, 22)[q + 1]
                        self.load_w(wd, wd[:, jj0:jj1, :], self.w_down.ap()[i, jj0 * 128:jj1 * 128, :], rng=(jj0, jj1))
                for (s, e, ss, se) in tg:
                    cs, ce = max(s - 1, ss), min(e + 1, se)
                    hl, hr = cs < s, ce > e
                    ncol = ce - cs
                    off = (s - base) if s < T else (512 + s - T)
                    for j in range(j0, j1):
                        A, G, UA, UG = pa[cnt % 2], pg[cnt % 2], ua[cnt % 2], ug[cnt % 2]
                        cnt += 1
                        for (P, W) in ((A, wab), (G, wgb)):
                            for kc in range(8):
                                k.op("pe", lambda P=P, W=W, kc=kc, j=j: nc.tensor.matmul(
                                    P[:, 0:ncol], W[:, kc, (j - j0) * 128:(j - j0 + 1) * 128], self.hT[:, kc, cs:ce],
                                    start=(kc == 0), stop=(kc == 7)),
                                    reads=[W, R(self.hT, cs, ce)], writes=[P])
                        nt_ = e - s
                        self.conv_evac(UA, A, cw, j, ncol, hl, hr, nt_)
                        self.conv_evac(UG, G, cw, 22 + j, ncol, hl, hr, nt_)
                        k.op("act", lambda UG=UG, nt_=nt_: nc.scalar.activation(UG[:, 0:nt_], UG[:, 0:nt_], AF.Silu), reads=[UG], writes=[UG])
                        k.op("dve", lambda UA=UA, UG=UG, j=j, off=off, nt_=nt_: nc.vector.tensor_tensor(
                            actT[:, j, off:off + nt_], UA[:, 0:nt_], UG[:, 0:nt_], ALU.mult),
                            reads=[UA, UG], writes=[R(actT, off, off + nt_)])
            tiles = sorted(set(tt for (s, e, _, _) in tg for tt in range(s // 128, e // 128)))
            for n, t in enumerate(tiles):
                Y = py[n % 2]
                off = (t * 128 - base) if t < 16 else (512 + t * 128 - T)
                for nh in range(2):
                    for j in range(NFC):
                        k.op("pe", lambda Y=Y, j=j, nh=nh, off=off: nc.tensor.matmul(
                            Y[:, nh * 512:(nh + 1) * 512], actT[:, j, off:off + 128], wd[:, j, nh * 512:(nh + 1) * 512],
                            start=(j == 0), stop=(j == NFC - 1)),
                            reads=[R(actT, off, off + 128), R(wd, j, j + 1)], writes=[R(Y, nh * 512, (nh + 1) * 512)])
                self.resid_update(t, Y, xb[n % 2], tb[n % 2])
        k.barrier()
        k.release(m)

    def out_proj_tile(self, t, ysb, nkc, wout, py, yT, ptr, xb, tb):
        k, nc = self.k, self.nc
        for kc in range(nkc):
            k.op("pe", lambda kc=kc: nc.tensor.transpose(ptr[:, kc, :], ysb[:, kc * 128:(kc + 1) * 128], self.ident_b),
                 reads=[ysb, self.cb], writes=[ptr])
        k.op("act", lambda: nc.scalar.copy(yT[:, 0:nkc, :], ptr[:, 0:nkc, :]), reads=[ptr], writes=[yT])
        for nh in range(2):
            for kc in range(nkc):
                k.op("pe", lambda kc=kc, nh=nh: nc.tensor.matmul(
                    py[:, nh * 512:(nh + 1) * 512], yT[:, kc, :], wout[:, kc, nh * 512:(nh + 1) * 512],
                    start=(kc == 0), stop=(kc == nkc - 1)),
                    reads=[yT, wout], writes=[R(py, nh * 512, (nh + 1) * 512)])
        if xb is not None:
            self.resid_update(t, py, xb, tb)

    def proj_tok(self, p_ap, pbuf, t, w, c0, n, prange=None):
        k, nc = self.k, self.nc
        for kc in range(8):
            k.op("pe", lambda kc=kc: nc.tensor.matmul(p_ap, self.hT[:, kc, t * 128:(t + 1) * 128], w[:, kc, c0:c0 + n],
                                                      start=(kc == 0), stop=(kc == 7)),
                 reads=[R(self.hT, t * 128, (t + 1) * 128), w], writes=[pbuf if prange is None else R(pbuf, *prange)])

    def mlstm(self, i, jm, last):
        k, nc = self.k, self.nc
        m0 = k.mark()
        W = self.a_w_in.ap()[jm]
        GT = k.sb("mGT", [128, NT, 32], F32)
        GP = k.sb("mGP", [128, NT, 3, 16], F32)
        bg = k.sb("mbg", [128, 32], F32)
        self.bcast_row_dma(bg, self.a_b_gate.ap()[jm:jm + 1, :], 32)
        cw = k.sb("mcw", [128, 4, 8], F32)
        k.dma("sp", cw[:], self.a_cw.ap()[jm].rearrange("p (a j) -> p a j", a=4), writes=[cw])
        fwd_order = [16, 17] + list(range(16))
        bwd_order = [17, 16] + list(range(15, -1, -1))
        for hg in range(2):
            m1 = k.mark()
            qkT = k.sb("mqkT", [128, 4, NTOK], BF16)
            V = k.sb("mV", [128, NT, 4, 130], BF16)
            Kt = k.sb("mKt", [128, NT, 256], BF16)
            H = k.sb("mH", [128, NT, 512], BF16)
            wq = k.sb("mwq", [128, 8, 512], BF16)
            wv = k.sb("mwv", [128, 8, 512], BF16)
            wo = k.sb("mwo", [128, 8, 512], BF16)
            wg_ = k.sb("mwg", [128, 8, 32], BF16)
            wout = k.sb("mwout", [128, 4, D], BF16)
            self.load_w(wq, wq[:, :, 0:256], W[:, hg * 256:(hg + 1) * 256], rng=(0, 256))
            self.load_w(wq, wq[:, :, 256:512], W[:, 512 + hg * 256:512 + (hg + 1) * 256], rng=(256, 512))
            self.load_w(wv, wv[:], W[:, 1024 + hg * 512:1024 + (hg + 1) * 512])
            self.load_w(wo, wo[:], W[:, 2048 + hg * 512:2048 + (hg + 1) * 512])
            self.load_w(wout, wout[:], self.a_w_out.ap()[jm, hg * 512:(hg + 1) * 512, :])
            if hg == 0:
                self.load_w(wg_, wg_[:], W[:, 3072:3104])
            k.op("dve", lambda: nc.vector.memset(V[:, :, :, 128:130], 1.0), writes=[V])
            m2 = k.mark()
            pq = [k.ps(f"mpq{j}", [128, 512], F32) for j in range(2)]
            uq = [k.sb(f"muq{j}", [128, 256], F32) for j in range(2)]
            cnt = 0
            for (s, e, ss, se) in self.tblocks():
                cs, ce = max(s - 1, ss), min(e + 1, se)
                hl, hr = cs < s, ce > e
                ncol = ce - cs
                for ch in range(4):
                    P, U = pq[cnt % 2], uq[cnt % 2]
                    cnt += 1
                    for kc in range(8):
                        k.op("pe", lambda P=P, kc=kc, ch=ch: nc.tensor.matmul(
                            P[:, 0:ncol], wq[:, kc, ch * 128:(ch + 1) * 128], self.hT[:, kc, cs:ce],
                            start=(kc == 0), stop=(kc == 7)), reads=[wq, R(self.hT, cs, ce)], writes=[P])
                    jcol = (ch // 2) * 4 + hg * 2 + (ch % 2)
                    self.conv_evac(U, P, cw, jcol, ncol, hl, hr, 256)
                    k.op("act", lambda U=U: nc.scalar.activation(U[:], U[:], AF.Silu), reads=[U], writes=[U])
                    sc = 0.125 if ch < 2 else 1.0
                    k.op("dve", lambda U=U, ch=ch, s=s, sc=sc: nc.vector.tensor_scalar(
                        qkT[:, ch, s:s + 256], U[:], sc, None, ALU.mult), reads=[U], writes=[R(qkT, s, s + 256)])
            k.barrier()
            if self.stop == "m_qk":
                self.dump("qkT", qkT, qkT[:], [128, 4, NTOK])
            self.chk("m_qk")
            k.release(m2)
            m2 = k.mark()
            pv = [k.ps(f"mpv{j}", [128, 512], F32) for j in range(2)]
            pgt = [k.ps(f"mpg{j}", [128, 512], F32) for j in range(2)]
            pk = [k.ps(f"mpk{j}", [128, 8, 128], BF16) for j in range(2)]
            for t in range(NT):
                P = pv[t % 2]
                self.proj_tok(P[:], P, t, wv, 0, 512)
                k.op("act", lambda P=P, t=t: nc.scalar.copy(V[:, t, :, 0:128], P[:].rearrange("p (h e) -> p h e", h=4)),
                     reads=[P], writes=[R(V, t, t + 1)])
                if hg == 0:
                    Pg = pgt[t % 2]
                    self.proj_tok(Pg[:, 0:32], Pg, t, wg_, 0, 32)
                    k.op("dve", lambda Pg=Pg, t=t: nc.vector.tensor_tensor(GT[:, t, :], Pg[:, 0:32], bg[:], ALU.add),
                         reads=[Pg, bg], writes=[R(GT, t, t + 1)])
                Pk = pk[t % 2]
                for c2 in range(2):
                    k.op("pe", lambda Pk=Pk, c2=c2, t=t: nc.tensor.transpose(
                        Pk[:, c2, :], qkT[:, 2 + c2, t * 128:(t + 1) * 128], self.ident_b),
                        reads=[R(qkT, t * 128, (t + 1) * 128), self.cb], writes=[Pk])
                k.op("act", lambda Pk=Pk, t=t: nc.scalar.copy(Kt[:, t, :].rearrange("p (a b) -> p a b", a=2), Pk[:, 0:2, :]),
                     reads=[Pk], writes=[R(Kt, t, t + 1)])
            k.barrier()
            if self.stop == "m_v":
                self.dump("V", V, V[:], [128, NT, 4, 130])
                self.dump("Kt", Kt, Kt[:], [128, NT, 256])
                self.dump("GT", GT, GT[:], [128, NT, 32])
            self.chk("m_v")
            k.release(m2)
            if hg == 0:
                m2 = k.mark()
                e1 = [k.sb(f"me1{j}", [128, 2, 8], F32) for j in range(2)]
                sp_ = [k.sb(f"msp{j}", [128, 2, 8], F32) for j in range(2)]
                aa = [k.sb(f"maa{j}", [128, 2, 8], F32) for j in range(2)]
                pgp = [k.ps(f"mpgp{j}", [128, 2, 256], F32) for j in range(2)]
                for t in range(NT):
                    E1, SP, A_, PG = e1[t % 2], sp_[t % 2], aa[t % 2], pgp[t % 2]
                    gv = GT[:, t, :].rearrange("p (d w h) -> p d w h", d=2, w=2)
                    k.op("act", lambda E1=E1, gv=gv: nc.scalar.activation(E1[:], gv[:, :, 1, :], AF.Exp, scale=-1.0),
                         reads=[R(GT, t, t + 1)], writes=[E1])
                    k.op("act", lambda E1=E1, SP=SP: nc.scalar.activation(SP[:], E1[:], AF.Ln, bias=1.0), reads=[E1], writes=[SP])
                    k.op("pe", lambda PG=PG, SP=SP: nc.tensor.matmul(PG[:, 0, 0:8], self.triu_f, SP[:, 0, :], start=True, stop=True),
                         reads=[self.cf, SP], writes=[R(PG, 0, 8)])
                    k.op("pe", lambda PG=PG, SP=SP: nc.tensor.matmul(PG[:, 0, 8:16], self.tril_f, SP[:, 1, :], start=True, stop=True),
                         reads=[self.cf, SP], writes=[R(PG, 8, 16)])
                    k.op("pe", lambda PG=PG, SP=SP: nc.tensor.matmul(PG[:, 1, 0:16], self.ones_f, SP[:].rearrange("p a b -> p (a b)"), start=True, stop=True),
                         reads=[self.cf, SP], writes=[R(PG, 16, 32)])
                    k.op("dve", lambda A_=A_, PG=PG, gv=gv: nc.vector.tensor_tensor(
                        A_[:], PG[:, 0, 0:16].rearrange("p (a b) -> p a b", a=2), gv[:, :, 0, :], ALU.add),
                        reads=[PG, R(GT, t, t + 1)], writes=[A_])
                    k.op("act", lambda A_=A_, t=t: nc.scalar.activation(GP[:, t, 0, :], A_[:].rearrange("p a b -> p (a b)"), AF.Exp),
                         reads=[A_], writes=[R(GP, t, t + 1)])
                    k.op("act", lambda PG=PG, t=t: nc.scalar.activation(GP[:, t, 1:3, :], PG[:, :, 0:16], AF.Exp, scale=-1.0),
                         reads=[PG], writes=[R(GP, t, t + 1)])
                k.barrier()
                if self.stop == "m_gate":
                    self.dump("GP", GP, GP[:], [128, NT, 3, 16])
                self.chk("m_gate")
                k.release(m2)
            m2 = k.mark()
            C32 = [k.sb(f"mC32{d}", [128, 2, 130], F32) for d in range(2)]
            Cb = [k.sb(f"mCb{d}", [128, 2, 130], BF16) for d in range(2)]
            Vp = [k.sb(f"mVp{d}", [128, 4, 130], BF16) for d in range(2)]
            ST = [k.sb(f"mST{d}", [128, 4, 128], BF16) for d in range(2)]
            dn = [k.sb(f"mdn{d}", [128, 4, 4], F32) for d in range(2)]
            hc = [k.sb(f"mhc{d}", [128, 512], F32) for d in range(2)]
            tmpC = [k.sb(f"mtC{d}", [128, 2, 130], F32) for d in range(2)]
            pqk = [k.ps(f"mpqk{d}", [128, 4, 128], F32) for d in range(2)]
            pnd = [k.ps(f"mpnd{d}", [128, 2, 512], F32) for d in range(2)]
            pdc1 = k.ps("mpdc", [128, 2, 512], F32)
            pdc = [pdc1, pdc1]
            for d in range(2):
                k.op("dve", lambda d=d: nc.vector.memset(C32[d][:], 0.0), writes=[C32[d]])
                k.op("dve", lambda d=d: nc.vector.memset(Cb[d][:], 0.0), writes=[Cb[d]])
            LVL = int(os.environ.get("SCAN_LVL", "9"))
            for step in range(NT):
                for d in range(2):
                    t = (fwd_order, bwd_order)[d][step]
                    mask = self.cf[:, 4 + d, :]
                    ea = GP[:, t, 0, d * 8 + hg * 4:d * 8 + hg * 4 + 4]
                    inter = GP[:, t, 1, d * 8 + hg * 4:d * 8 + hg * 4 + 4]
                    egl = GP[0:64, t, 2, d * 8 + hg * 4:d * 8 + hg * 4 + 4]
                    k.op("pool", lambda d=d, t=t, ea=ea: nc.gpsimd.tensor_tensor(
                        Vp[d][:, :, 0:129], V[:, t, :, 0:129], bc_last(ea, 129), ALU.mult),
                        reads=[R(V, t, t + 1), R(GP, t, t + 1)], writes=[Vp[d]])
                    if LVL < 2:
                        continue
                    for h in (0, 2, 1, 3):
                        pb = (h % 2) * 64
                        k.op("pe", lambda d=d, h=h, pb=pb, t=t: nc.tensor.matmul(
                            pqk[d][:, h, :], qkT[pb:pb + 64, 2 + h // 2, t * 128:(t + 1) * 128],
                            qkT[pb:pb + 64, h // 2, t * 128:(t + 1) * 128], start=True, stop=True),
                            reads=[R(qkT, t * 128, (t + 1) * 128)], writes=[R(pqk[d], h, h + 1)], rg=pb)
                    k.op("dve", lambda d=d, mask=mask: nc.vector.tensor_tensor(
                        ST[d][:], pqk[d][:], bc_mid(mask, 1, 4), ALU.mult), reads=[pqk[d], self.cf], writes=[ST[d]])
                    if LVL < 3:
                        continue
                    def qc_mm(h, start, stop):
                        pb = (h % 2) * 64
                        o_ap = pnd[d][:, h // 2, (h % 2) * 130:(h % 2) * 130 + 129]
                        k.op("pe", lambda d=d, h=h, pb=pb, t=t, o_ap=o_ap: nc.tensor.matmul(
                            o_ap, qkT[pb:pb + 64, h // 2, t * 128:(t + 1) * 128], Cb[d][pb:pb + 64, h // 2, 0:129], start=start, stop=stop),
                            reads=[R(qkT, t * 128, (t + 1) * 128), Cb[d]], writes=[R(pnd[d], h, h + 1)], rg=pb)

                    def sv_mm(h, start, stop):
                        o_ap = pnd[d][:, h // 2, (h % 2) * 130:(h % 2) * 130 + 129]
                        k.op("pe", lambda d=d, h=h, o_ap=o_ap: nc.tensor.matmul(
                            o_ap, ST[d][:, h, :], Vp[d][:, h, 0:129], start=start, stop=stop),
                            reads=[ST[d], Vp[d]], writes=[R(pnd[d], h, h + 1)])
                    qc_mm(1, True, False)
                    qc_mm(3, True, False)
                    sv_mm(1, False, True)
                    sv_mm(3, False, True)
                    sv_mm(0, True, False)
                    sv_mm(2, True, False)
                    qc_mm(0, False, True)
                    qc_mm(2, False, True)
                    if self.stop == "m_scan" and step == 1 and d == 0 and self.debug_x and os.environ.get("DBG_PND"):
                        dbgt = k.sb("dbgt", [128, 2, 512], F32)
                        k.op("act", lambda: nc.scalar.copy(dbgt[:], pnd[d][:]), reads=[pnd[d]], writes=[dbgt])
                        self.dump("pnd", dbgt, dbgt[:], [128, 2, 512])
                        self.dump("ST", ST[d], ST[d][:], [128, 4, 128])
                        self.dump("Vp", Vp[d], Vp[d][:], [128, 4, 130])
                        self.dump("Cb", Cb[d], Cb[d][:], [128, 2, 130])
                    if LVL < 4:
                        continue
                    den_ap = bass.AP(pnd[d][:, 0, 128:129].tensor, pnd[d][:, 0, 128:129].offset, [list(pnd[d][:, 0, 128:129].ap[0]), [512, 2], [130, 2]])
                    k.op("dve", lambda d=d, den_ap=den_ap, inter=inter: nc.vector.tensor_tensor(
                        dn[d][:, 0, :].rearrange("p (a b) -> p a b", a=2), den_ap, inter.rearrange("p (a b) -> p a b", a=2), ALU.mult),
                        reads=[pnd[d], R(GP, t, t + 1)], writes=[R(dn[d], 0, 1)])
                    k.op("dve", lambda d=d: nc.vector.scalar_tensor_tensor(dn[d][:, 1, :], dn[d][:, 0, :], -1.0, dn[d][:, 0, :], ALU.mult, ALU.max),
                         reads=[R(dn[d], 0, 1)], writes=[R(dn[d], 1, 2)])
                    k.op("dve", lambda d=d: nc.vector.tensor_scalar_max(dn[d][:, 1, :], dn[d][:, 1, :], 1.0),
                         reads=[R(dn[d], 1, 2)], writes=[R(dn[d], 1, 2)])
                    k.op("dve", lambda d=d: nc.vector.reciprocal(dn[d][:, 2, :], dn[d][:, 1, :]),
                         reads=[R(dn[d], 1, 2)], writes=[R(dn[d], 2, 3)])
                    k.op("dve", lambda d=d, inter=inter: nc.vector.tensor_tensor(dn[d][:, 3, :], dn[d][:, 2, :], inter, ALU.mult),
                         reads=[R(dn[d], 2, 3), R(GP, t, t + 1)], writes=[R(dn[d], 3, 4)])
                    if LVL < 5:
                        continue
                    num_ap = bass.AP(pnd[d][:, 0, 0:1].tensor, pnd[d][:, 0, 0:1].offset, [list(pnd[d][:, 0, 0:1].ap[0]), [512, 2], [130, 2], [1, 128]])
                    rec_b = bc_last(dn[d][:, 3, :].rearrange("p (a b) -> p a b", a=2), 128)
                    sf, sb_ = fwd_order.index(t), bwd_order.index(t)
                    first = (sf <= sb_) if d == 0 else (sb_ < sf)
                    if first:
                        k.op("dve", lambda t=t, num_ap=num_ap, rec_b=rec_b: nc.vector.tensor_tensor(
                            H[:, t, :].rearrange("p (a b e) -> p a b e", a=2, b=2), num_ap, rec_b, ALU.mult),
                            reads=[pnd[d], R(dn[d], 3, 4)], writes=[R(H, t, t + 1)])
                    else:
                        k.op("dve", lambda d=d, num_ap=num_ap, rec_b=rec_b: nc.vector.tensor_tensor(
                            hc[d][:].rearrange("p (a b e) -> p a b e", a=2, b=2), num_ap, rec_b, ALU.mult),
                            reads=[pnd[d], R(dn[d], 3, 4)], writes=[hc[d]])
                        k.op("pool", lambda d=d, t=t: nc.gpsimd.tensor_tensor(H[:, t, :], H[:, t, :], hc[d][:], ALU.add),
                             reads=[hc[d], R(H, t, t + 1)], writes=[R(H, t, t + 1)])
                    if LVL < 6:
                        continue
                    for h in (0, 2, 1, 3):
                        pb = (h % 2) * 64
                        o_ap = pdc[d][pb:pb + 64, h // 2, 0:129]
                        k.op("pe", lambda d=d, h=h, t=t, o_ap=o_ap, pb=pb: nc.tensor.matmul(
                            o_ap, Kt[:, t, h * 64:(h + 1) * 64], Vp[d][:, h, 0:129], start=True, stop=True,
                            tile_position=((0, pb) if pb else None)),
                            reads=[R(Kt, t, t + 1), Vp[d]], writes=[R(pdc[d], h, h + 1)], rg=("c", pb))
                    for hf in range(2):
                        pb = hf * 64
                        c0_ = d * 8 + hg * 4 + hf
                        eglh = GP[pb:pb + 64, t, 2, c0_:c0_ + 3:2]
                        k.op("dve", lambda d=d, pb=pb: nc.vector.tensor_tensor(
                            tmpC[d][pb:pb + 64, :, 0:129], pdc[d][pb:pb + 64, :, 0:129], C32[d][pb:pb + 64, :, 0:129], ALU.add),
                            reads=[pdc[d], C32[d]], writes=[R(tmpC[d], hf, hf + 1)])
                        k.op("pool", lambda d=d, pb=pb, eglh=eglh: nc.gpsimd.tensor_tensor(
                            C32[d][pb:pb + 64, :, 0:129], tmpC[d][pb:pb + 64, :, 0:129], bc_last(eglh, 129), ALU.mult),
                            reads=[R(tmpC[d], hf, hf + 1), R(GP, t, t + 1)], writes=[C32[d]])
                    k.op("act", lambda d=d: nc.scalar.copy(Cb[d][:], C32[d][:]), reads=[C32[d]], writes=[Cb[d]])
            k.barrier()
            if self.stop == "m_scan":
                self.dump("H", H, H[:], [128, NT, 512])
                self.dump("qkT", qkT, qkT[:], [128, 4, NTOK])
                self.dump("V", V, V[:], [128, NT, 4, 130])
                self.dump("Kt", Kt, Kt[:], [128, NT, 256])
                self.dump("GT", GT, GT[:], [128, NT, 32])
                self.dump("GP", GP, GP[:], [128, NT, 3, 16])
                self.dump("ST", ST[0], ST[0][:], [128, 4, 128])
                self.dump("C32", C32[0], C32[0][:], [128, 2, 130])
                self.dump("Vp", Vp[0], Vp[0][:], [128, 4, 130])
            self.chk("m_scan")
            k.release(m2)
            m2 = k.mark()
            po = [k.ps(f"mpo{j}", [128, 512], F32) for j in range(2)]
            py = [k.ps(f"mpy{j}", [128, D], F32) for j in range(2)]
            ptr = [k.ps(f"mptr{j}", [128, 8, 128], BF16) for j in range(2)]
            sg = [k.sb(f"msg{j}", [128, 512], BF16) for j in range(2)]
            ysb = [k.sb(f"mys{j}", [128, 512], BF16) for j in range(2)]
            yT = [k.sb(f"myT{j}", [128, 4, 128], BF16) for j in range(2)]
            xb = [k.sb(f"mxb{j}", [128, D], F32) for j in range(2)]
            tb1 = k.sb("mtb", [128, D], F32)
            tb = [tb1, tb1]
            otiles = list(range(16 if last else NT))

            def oA(n):
                t, b = otiles[n], n % 2
                self.proj_tok(po[b][:], po[b], t, wo, 0, 512)
                k.op("act", lambda: nc.scalar.activation(sg[b][:], po[b][:], AF.Sigmoid), reads=[po[b]], writes=[sg[b]])
                k.op("dve", lambda: nc.vector.tensor_tensor(ysb[b][:], H[:, t, :], sg[b][:], ALU.mult),
                     reads=[R(H, t, t + 1), sg[b]], writes=[ysb[b]])

            def oB(n):
                t, b = otiles[n], n % 2
                self.out_proj_tile(t, ysb[b], 4, wout, py[b], yT[b], ptr[b], None, None)

            def oC(n):
                t, b = otiles[n], n % 2
                self.resid_update(t, py[b], xb[b], tb[b])

            self.swpipe(len(otiles), [oA, oB, oC])
            k.barrier()
            k.release(m2)
            k.release(m1)
        k.release(m0)

    def proj_feat(self, p_ap, pbuf, w, c0, m, cs, ce):
        k, nc = self.k, self.nc
        for kc in range(8):
            k.op("pe", lambda kc=kc: nc.tensor.matmul(p_ap, w[:, kc, c0:c0 + m], self.hT[:, kc, cs:ce],
                                                      start=(kc == 0), stop=(kc == 7)),
                 reads=[w, R(self.hT, cs, ce)], writes=[pbuf])

    def gla(self, i, jm, last):
        k, nc = self.k, self.nc
        m0 = k.mark()
        W = self.b_w_in.ap()[jm]
        fwd_order = [16, 17] + list(range(16))
        bwd_order = [17, 16] + list(range(15, -1, -1))
        blocks = [(0, 512), (512, 1024), (1024, 1536), (1536, 2048), (2048, 2304)]
        gnb = k.sb("ggn", [128, 256], F32)
        self.bcast_row_dma(gnb, self.b_norm.ap()[jm:jm + 1, :], 256)
        wa2 = k.sb("gwa2", [17, 2, 512], F32)
        k.dma("sp", wa2[:], self.b_w_a2.ap()[jm].rearrange("d r c -> r d c"), writes=[wa2])
        for hg in range(2):
            m1 = k.mark()
            qT = k.sb("gqT", [128, 2, NTOK], BF16)
            kT = k.sb("gkT", [128, 2, NTOK], BF16)
            V = k.sb("gV", [128, NT, 512], BF16)
            SP = k.sb("gSP", [128, NT, 2, 256], F32)
            O = k.sb("gO", [128, NT, 512], BF16)
            m2 = k.mark()
            wqk = k.sb("gwqk", [128, 8, 512], BF16)
            wv = k.sb("gwv", [128, 8, 512], BF16)
            wa = k.sb("gwa", [128, 8, 32], BF16)
            aT = [k.sb(f"gaT{d}", [17, NTOK], F32) for d in range(2)]
            self.load_w(wqk, wqk[:, :, 0:256], W[:, hg * 256:(hg + 1) * 256], rng=(0, 256))
            self.load_w(wqk, wqk[:, :, 256:512], W[:, 512 + hg * 256:512 + (hg + 1) * 256], rng=(256, 512))
            self.load_w(wv, wv[:], W[:, 1024 + hg * 512:1024 + (hg + 1) * 512])
            self.load_w(wa, wa[:], W[:, 3072:3104])
            pq = [k.ps(f"gpq{j}", [128, 512], F32) for j in range(2)]
            pa_ = [k.ps(f"gpa{j}", [128, 512], F32) for j in range(2)]
            pz = [k.ps(f"gpz{j}", [128, 512], F32) for j in range(2)]
            e1 = [k.sb(f"ge1{j}", [128, 512], F32) for j in range(2)]
            for d in range(2):
                k.op("dve", lambda d=d: nc.vector.memset(aT[d][:], 1.0), writes=[aT[d]])
            cnt = 0
            for (cs, ce) in blocks:
                n = ce - cs
                for ch in range(4):
                    P = pq[cnt % 2]
                    cnt += 1
                    self.proj_feat(P[:, 0:n], P, wqk, ch * 128, 128, cs, ce)
                    if ch < 2:
                        k.op("act", lambda P=P, ch=ch: nc.scalar.activation(qT[:, ch, cs:ce], P[:, 0:n], AF.Copy, scale=128.0 ** -0.5),
                             reads=[P], writes=[R(qT, cs, ce)])
                    else:
                        k.op("dve", lambda P=P, ch=ch: nc.vector.tensor_copy(kT[:, ch - 2, cs:ce], P[:, 0:n]),
                             reads=[P], writes=[R(kT, cs, ce)])
                for d in range(2):
                    P = pa_[d]
                    self.proj_feat(P[0:16, 0:n], P, wa, d * 16, 16, cs, ce)
                    k.op("act", lambda P=P, d=d: nc.scalar.copy(aT[d][0:16, cs:ce], P[0:16, 0:n]), reads=[P], writes=[R(aT[d], cs, ce)])
            for t in range(NT):
                P = pq[t % 2]
                self.proj_tok(P[:], P, t, wv, 0, 512)
                k.op("act", lambda P=P, t=t: nc.scalar.copy(V[:, t, :], P[:]), reads=[P], writes=[R(V, t, t + 1)])
                for d in range(2):
                    Z = pz[d]
                    k.op("pe", lambda Z=Z, d=d, t=t: nc.tensor.matmul(Z[:, 0:256], aT[d][0:17, t * 128:(t + 1) * 128],
                                                                   wa2[0:17, d, hg * 256:(hg + 1) * 256], start=True, stop=True),
                         reads=[R(aT[d], t * 128, (t + 1) * 128), wa2], writes=[Z])
                    E = e1[d]
                    k.op("act", lambda Z=Z, E=E: nc.scalar.activation(E[:, 0:256], Z[:, 0:256], AF.Exp, scale=-1.0), reads=[Z], writes=[E])
                    k.op("act", lambda E=E: nc.scalar.activation(E[:, 256:512], E[:, 0:256], AF.Ln, bias=1.0), reads=[E], writes=[E])
                    k.op("dve", lambda E=E, d=d, t=t: nc.vector.tensor_scalar(SP[:, t, d, :], E[:, 256:512], 1.0 / 16.0, None, ALU.mult),
                         reads=[E], writes=[R(SP, t, t + 1)])
            k.barrier()
            k.release(m2)
            m2 = k.mark()
            S32 = [k.sb(f"gS32{d}", [128, 2, 256], F32) for d in range(2)]
            tS = [k.sb(f"gtS{d}", [128, 2, 256], F32) for d in range(2)]
            Sb = [k.sb(f"gSb{d}", [128, 2, 256], BF16) for d in range(2)]
            EG = [k.sb(f"gEG{d}", [128, 2, 128], F32) for d in range(2)]
            EN = [k.sb(f"gEN{d}", [128, 2, 128], F32) for d in range(2)]
            qt = [k.sb(f"gqt{d}", [128, 2, 128], BF16) for d in range(2)]
            kt = [k.sb(f"gkt{d}", [128, 2, 128], BF16) for d in range(2)]
            AT = [k.sb(f"gAT{d}", [128, 2, 128], BF16) for d in range(2)]
            Ktok = [k.sb(f"gKtok{d}", [128, 2, 128], BF16) for d in range(2)]
            pP = [k.ps(f"gpP{d}", [128, 4, 128], F32) for d in range(2)]
            pO = [k.ps(f"gpO{d}", [128, 2, 256], F32) for d in range(2)]
            pS = [k.ps(f"gpS{d}", [128, 2, 256], F32) for d in range(2)]
            pA = k.ps("gpA", [128, 4, 128], F32)
            pKt = k.ps("gpKt", [128, 8, 128], BF16)
            for d in range(2):
                k.op("dve", lambda d=d: nc.vector.memset(S32[d][:], 0.0), writes=[S32[d]])
                k.op("dve", lambda d=d: nc.vector.memset(Sb[d][:], 0.0), writes=[Sb[d]])
            for step in range(NT):
                for d in range(2):
                    t = (fwd_order, bwd_order)[d][step]
                    tri = self.cf[:, 1 + d, :]
                    mask = self.cf[:, 4 + d, :]
                    lastc = 127 if d == 0 else 0
                    tk = slice(t * 128, (t + 1) * 128)
                    for h in range(2):
                        k.op("pe", lambda d=d, h=h, t=t, tri=tri: nc.tensor.matmul(
                            pP[d][:, h, :], SP[:, t, d, h * 128:(h + 1) * 128], tri, start=True, stop=True),
                            reads=[R(SP, t, t + 1), self.cf], writes=[R(pP[d], h, h + 1)])
                    k.op("act", lambda d=d: nc.scalar.activation(EG[d][:], pP[d][:, 0:2, :], AF.Exp, scale=-1.0), reads=[pP[d]], writes=[EG[d]])
                    k.op("act", lambda d=d: nc.scalar.activation(EN[d][:], pP[d][:, 0:2, :], AF.Exp), reads=[pP[d]], writes=[EN[d]])
                    k.op("dve", lambda d=d, tk=tk: nc.vector.tensor_tensor(qt[d][:], qT[:, :, tk], EG[d][:], ALU.mult),
                         reads=[R(qT, t * 128, (t + 1) * 128), EG[d]], writes=[qt[d]])
                    k.op("pool", lambda d=d, tk=tk: nc.gpsimd.tensor_tensor(kt[d][:], kT[:, :, tk], EN[d][:], ALU.mult),
                         reads=[R(kT, t * 128, (t + 1) * 128), EN[d]], writes=[kt[d]])
                    for h in range(2):
                        k.op("pe", lambda d=d, h=h: nc.tensor.matmul(pA[:, h, :], kt[d][:, h, :], qt[d][:, h, :], start=True, stop=True),
                             reads=[kt[d], qt[d]], writes=[R(pA, h, h + 1)])
                    for h in range(2):
                        k.op("pe", lambda d=d, h=h: nc.tensor.transpose(pKt[:, h, :], kt[d][:, h, :], self.ident_b),
                             reads=[kt[d], self.cb], writes=[R(pKt, h, h + 1)])
                    k.op("dve", lambda d=d, mask=mask: nc.vector.tensor_tensor(AT[d][:], pA[:, 0:2, :], bc_mid(mask, 1, 2), ALU.mult),
                         reads=[pA, self.cf], writes=[AT[d]])
                    k.op("act", lambda d=d: nc.scalar.copy(Ktok[d][:], pKt[:, 0:2, :]), reads=[pKt], writes=[Ktok[d]])
                    for h in range(2):
                        k.op("pe", lambda d=d, h=h, t=t: nc.tensor.matmul(pO[d][:, h, :], AT[d][:, h, :], V[:, t, h * 256:(h + 1) * 256],
                                                                      start=True, stop=False),
                             reads=[AT[d], R(V, t, t + 1)], writes=[R(pO[d], h, h + 1)])
                        k.op("pe", lambda d=d, h=h: nc.tensor.matmul(pO[d][:, h, :], qt[d][:, h, :], Sb[d][:, h, :], start=False, stop=True),
                             reads=[qt[d], Sb[d]], writes=[R(pO[d], h, h + 1)])
                    for h in range(2):
                        k.op("pe", lambda d=d, h=h, t=t: nc.tensor.matmul(pS[d][:, h, :], Ktok[d][:, h, :], V[:, t, h * 256:(h + 1) * 256],
                                                                      start=True, stop=True),
                             reads=[Ktok[d], R(V, t, t + 1)], writes=[R(pS[d], h, h + 1)])
                    sf, sb_ = fwd_order.index(t), bwd_order.index(t)
                    first = (sf <= sb_) if d == 0 else (sb_ < sf)
                    if first:
                        k.op("dve", lambda d=d, t=t: nc.vector.tensor_copy(O[:, t, :], pO[d][:].rearrange("p a b -> p (a b)")),
                             reads=[pO[d]], writes=[R(O, t, t + 1)])
                    else:
                        k.op("dve", lambda d=d, t=t: nc.vector.tensor_tensor(O[:, t, :], pO[d][:].rearrange("p a b -> p (a b)"), O[:, t, :], ALU.add),
                             reads=[pO[d], R(O, t, t + 1)], writes=[R(O, t, t + 1)])
                    k.op("dve", lambda d=d: nc.vector.tensor_tensor(tS[d][:], pS[d][:], S32[d][:], ALU.add),
                         reads=[pS[d], S32[d]], writes=[tS[d]])
                    k.op("pool", lambda d=d, lastc=lastc: nc.gpsimd.tensor_tensor(S32[d][:], tS[d][:], bc_last(EG[d][:, :, lastc], 256), ALU.mult),
                         reads=[tS[d], EG[d]], writes=[S32[d]])
                    k.op("act", lambda d=d: nc.scalar.copy(Sb[d][:], S32[d][:]), reads=[S32[d]], writes=[Sb[d]])
            k.barrier()
            if self.stop == "g_scan":
                self.dump("O", O, O[:], [128, NT, 512])
            self.chk("g_scan")
            k.release(m2)
            m2 = k.mark()
            wr = k.sb("gwr", [128, 8, 512], BF16)
            wout = k.sb("gwout", [128, 4, D], BF16)
            self.load_w(wr, wr[:], W[:, 2048 + hg * 512:2048 + (hg + 1) * 512])
            self.load_w(wout, wout[:], self.b_w_out.ap()[jm, hg * 512:(hg + 1) * 512, :])
            pr = [k.ps(f"gpr{j}", [128, 512], F32) for j in range(2)]
            py = [k.ps(f"gpy{j}", [128, D], F32) for j in range(2)]
            ptr = [k.ps(f"gptr{j}", [128, 8, 128], BF16) for j in range(2)]
            sr = [k.sb(f"gsr{j}", [128, 512], BF16) for j in range(2)]
            y32 = [k.sb(f"gy32{j}", [128, 2, 256], F32) for j in range(2)]
            ysb = [k.sb(f"gys{j}", [128, 512], BF16) for j in range(2)]
            yT = [k.sb(f"gyT{j}", [128, 4, 128], BF16) for j in range(2)]
            xb = [k.sb(f"gxb{j}", [128, D], F32) for j in range(2)]
            tb1 = k.sb("gtb", [128, D], F32)
            st = [k.sb(f"gst{j}", [128, 8], F32) for j in range(2)]
            junk = k.sb("gjunk", [128, 256], BF16)
            otiles = list(range(16 if last else NT))

            def oA(n):
                t, b = otiles[n], n % 2
                self.proj_tok(pr[b][:], pr[b], t, wr, 0, 512)
                k.op("act", lambda: nc.scalar.activation(sr[b][:], pr[b][:], AF.Silu), reads=[pr[b]], writes=[sr[b]])
                k.op("pool", lambda: nc.gpsimd.memset(st[b][:], 0.0), writes=[st[b]])
                for h in range(2):
                    k.op("act", lambda h=h: nc.scalar.activation(junk[:], O[:, t, h * 256:(h + 1) * 256], AF.Square, accum_out=st[b][:, h:h + 1]),
                         reads=[R(O, t, t + 1), st[b]], writes=[junk, st[b]])
                k.op("act", lambda: nc.scalar.activation(st[b][:, 2:4], st[b][:, 0:2], AF.Ln, scale=1.0 / 256, bias=EPS), reads=[st[b]], writes=[st[b]])
                k.op("act", lambda: nc.scalar.activation(st[b][:, 4:6], st[b][:, 2:4], AF.Exp, scale=-0.5), reads=[st[b]], writes=[st[b]])
                k.op("dve", lambda: nc.vector.tensor_tensor(y32[b][:], O[:, t, :].rearrange("p (a e) -> p a e", a=2),
                                                           bc_last(st[b][:, 4:6], 256), ALU.mult),
                     reads=[R(O, t, t + 1), st[b]], writes=[y32[b]])
                k.op("pool", lambda: nc.gpsimd.tensor_tensor(y32[b][:], y32[b][:], bc_mid(gnb[:], 1, 2), ALU.mult),
                     reads=[y32[b], gnb], writes=[y32[b]])
                k.op("dve", lambda: nc.vector.tensor_tensor(ysb[b][:], y32[b][:].rearrange("p a e -> p (a e)"), sr[b][:], ALU.mult),
                     reads=[y32[b], sr[b]], writes=[ysb[b]])

            def oB(n):
                t, b = otiles[n], n % 2
                self.out_proj_tile(t, ysb[b], 4, wout, py[b], yT[b], ptr[b], None, None)

            def oC(n):
                t, b = otiles[n], n % 2
                self.resid_update(t, py[b], xb[b], tb1)

            self.swpipe(len(otiles), [oA, oB, oC])
            k.barrier()
            k.release(m2)
            k.release(m1)
        k.release(m0)

    def na(self, i, jm, last):
        k, nc = self.k, self.nc
        m0 = k.mark()
        W = self.c_w_in.ap()[jm]
        blocks = [(0, 512), (512, 1024), (1024, 1536), (1536, 2048), (2048, 2304)]
        cmask = k.sb("ncm", [128, 64], F32)
        k.dma("sp", cmask[:], self.cmask_in.ap(), writes=[cmask])
        for hg in range(2):
            m1 = k.mark()
            qT = k.sb("nqT", [128, 4, NTOK], BF16)
            kT = k.sb("nkT", [128, 4, NTOK], BF16)
            V = k.sb("nV", [128, NT, 512], BF16)
            BM = k.sb("nBM", [128, 8, 15, 64], BF16)
            src = self.c_rpbx.ap()[jm, hg * 8:(hg + 1) * 8].rearrange("h q (r c) -> q h r c", r=15)
            k.dma("pool", BM[0:64], src, writes=[R(BM, 0, 1)])
            k.dma("pool", BM[64:128], src, writes=[R(BM, 1, 2)])
            k.op("dve", lambda: nc.vector.tensor_tensor(BM[:].rearrange("p h r c -> p (h r) c"), BM[:].rearrange("p h r c -> p (h r) c"),
                                                       bc_mid(cmask[:], 1, 120), ALU.add), reads=[BM, cmask], writes=[BM])
            m2 = k.mark()
            wq = k.sb("nwq", [128, 8, 512], BF16)
            wk = k.sb("nwk", [128, 8, 512], BF16)
            wv = k.sb("nwv", [128, 8, 512], BF16)
            self.load_w(wq, wq[:], W[:, hg * 512:(hg + 1) * 512])
            self.load_w(wk, wk[:], W[:, D + hg * 512:D + (hg + 1) * 512])
            self.load_w(wv, wv[:], W[:, 2 * D + hg * 512:2 * D + (hg + 1) * 512])
            pq = [k.ps(f"npq{j}", [128, 512], F32) for j in range(2)]
            cnt = 0
            for (cs, ce) in blocks:
                n = ce - cs
                for ch in range(8):
                    P = pq[cnt % 2]
                    cnt += 1
                    self.proj_feat(P[:, 0:n], P, wq if ch < 4 else wk, (ch % 4) * 128, 128, cs, ce)
                    if ch < 4:
                        k.op("act", lambda P=P, ch=ch: nc.scalar.activation(qT[:, ch, cs:ce], P[:, 0:n], AF.Copy, scale=0.125),
                             reads=[P], writes=[R(qT, cs, ce)])
                    else:
                        k.op("dve", lambda P=P, ch=ch: nc.vector.tensor_copy(kT[:, ch - 4, cs:ce], P[:, 0:n]),
                             reads=[P], writes=[R(kT, cs, ce)])
            for t in range(NT):
                P = pq[t % 2]
                self.proj_tok(P[:], P, t, wv, 0, 512)
                k.op("act", lambda P=P, t=t: nc.scalar.copy(V[:, t, :], P[:]), reads=[P], writes=[R(V, t, t + 1)])
            k.barrier()
            k.release(m2)
            m2 = k.mark()
            wout = k.sb("nwout", [128, 4, D], BF16)
            self.load_w(wout, wout[:], self.c_w_out.ap()[jm, hg * 512:(hg + 1) * 512, :])
            Sc = [k.sb(f"nSc{j}", [128, 896], F32) for j in range(2)]
            Pb = [k.sb(f"nPb{j}", [128, 896], BF16) for j in range(2)]
            PT = [k.sb(f"nPT{j}", [128, 7, 128], BF16) for j in range(2)]
            pS = [k.ps(f"npS{j}", [128, 1024], F32) for j in range(2)]
            pT = [k.ps(f"npT{j}", [128, 8, 128], BF16) for j in range(1)]
            pO = k.ps("npO", [128, 8, 64], F32)
            py = [k.ps(f"npy{j}", [128, D], F32) for j in range(1)]
            ptr = pT
            ysb = [k.sb(f"nys{j}", [128, 512], BF16) for j in range(2)]
            yT = [k.sb(f"nyT{j}", [128, 4, 128], BF16) for j in range(2)]
            xb = [k.sb(f"nxb{j}", [128, D], F32) for j in range(2)]
            tb1 = k.sb("ntb", [128, D], F32)
            tiles_ = list(range(16 if last else NT))
            units = [(n_, t, h) for n_, t in enumerate(tiles_) for h in range(8)]
            st = [k.sb(f"nst4{j}", [128, 4], F32) for j in range(4)]
            Pb = Pb + [k.sb("nPb2", [128, 896], BF16)]

            def geo(t):
                if t < 16:
                    rs = [min(max(2 * t - 4, 0), 24), min(max(2 * t - 3, 0), 24)]
                    nl = 4 if t in (0, 1, 14, 15) else 5
                    return rs, rs[0], nl, 0, list(range(nl)) + [5, 6]
                return None, 0, 0, 640, [5, 6]

            def stA(u):
                n_, t, h = units[u]
                rs, ks, nl, c_lo, chunks = geo(t)
                if h == 0 and nl:
                    for sb_ in Sc:
                        for hf in range(2):
                            i0 = rs[hf] - ks
                            if i0 > 0:
                                k.op("dve", lambda sb_=sb_, hf=hf, i0=i0: nc.vector.memset(sb_[hf * 64:(hf + 1) * 64, 0:i0 * 64], NEG), writes=[sb_])
                            if (i0 + 8) * 64 < 640:
                                k.op("dve", lambda sb_=sb_, hf=hf, i0=i0: nc.vector.memset(sb_[hf * 64:(hf + 1) * 64, (i0 + 8) * 64:640], NEG), writes=[sb_])
                pb = (h % 2) * 64
                c = h // 2
                S_, P_, s_, ps_ = Sc[u % 2], Pb[u % 3], st[u % 4], pS[u % 2]
                qa = qT[pb:pb + 64, c, t * 128:(t + 1) * 128]
                if nl:
                    k0 = ks * 64
                    k.op("pe", lambda: nc.tensor.matmul(ps_[:, 0:512], qa, kT[pb:pb + 64, c, k0:k0 + 512], start=True, stop=True),
                         reads=[R(qT, t * 128, (t + 1) * 128), R(kT, k0, k0 + 512)], writes=[R(ps_, 0, 512)], rg=pb)
                    if nl == 5:
                        k.op("pe", lambda: nc.tensor.matmul(ps_[:, 512:640], qa, kT[pb:pb + 64, c, k0 + 512:k0 + 640], start=True, stop=True),
                             reads=[R(qT, t * 128, (t + 1) * 128), R(kT, k0 + 512, k0 + 640)], writes=[R(ps_, 512, 640)], rg=pb)
                k.op("pe", lambda: nc.tensor.matmul(ps_[:, 640:896], qa, kT[pb:pb + 64, c, T:NTOK], start=True, stop=True),
                     reads=[R(qT, t * 128, (t + 1) * 128), R(kT, T, NTOK)], writes=[R(ps_, 640, 896)], rg=pb)
                if nl:
                    for hf in range(2):
                        r = 2 * t + hf
                        i0 = rs[hf] - ks
                        dr0 = rs[hf] - r + 7
                        k.op("dve", lambda hf=hf, i0=i0, dr0=dr0: nc.vector.tensor_tensor(
                            S_[hf * 64:(hf + 1) * 64, i0 * 64:(i0 + 8) * 64], ps_[hf * 64:(hf + 1) * 64, i0 * 64:(i0 + 8) * 64],
                            BM[hf * 64:(hf + 1) * 64, h, dr0:dr0 + 8, :].rearrange("p r c -> p (r c)"), ALU.add),
                            reads=[ps_, BM], writes=[S_])
                k.op("act", lambda: nc.scalar.copy(S_[:, 640:896], ps_[:, 640:896]), reads=[ps_], writes=[S_])
                k.op("pool", lambda: nc.gpsimd.memset(s_[:], 0.0), writes=[s_])
                k.op("dve", lambda: nc.vector.tensor_reduce(s_[:, 0:1], S_[:, c_lo:896], AX.X, ALU.max, negate=True), reads=[S_], writes=[s_])
                k.op("act", lambda: nc.scalar.activation(P_[:, c_lo:896], S_[:, c_lo:896], AF.Exp, bias=s_[:, 0:1], accum_out=s_[:, 1:2]),
                     reads=[S_, s_], writes=[P_, s_])

            def stB(u):
                n_, t, h = units[u]
                rs, ks, nl, c_lo, chunks = geo(t)
                P_, PT_, pT_ = Pb[u % 3], PT[u % 2], pT[0]
                for cc in chunks:
                    k.op("pe", lambda cc=cc: nc.tensor.transpose(pT_[:, cc, :], P_[:, cc * 128:(cc + 1) * 128], self.ident_b),
                         reads=[P_, self.cb], writes=[R(pT_, cc, cc + 1)])
                if nl:
                    k.op("act", lambda: nc.scalar.copy(PT_[:, 0:nl, :], pT_[:, 0:nl, :]), reads=[pT_], writes=[PT_])
                k.op("dve", lambda: nc.vector.tensor_copy(PT_[:, 5:7, :], pT_[:, 5:7, :]), reads=[pT_], writes=[PT_])

            def stC(u):
                n_, t, h = units[u]
                rs, ks, nl, c_lo, chunks = geo(t)
                PT_, s_ = PT[u % 2], st[u % 4]
                for n2, cc in enumerate(chunks):
                    vt = (ks // 2 + cc) if cc < 5 else (16 + cc - 5)
                    k.op("pe", lambda cc=cc, vt=vt, n2=n2: nc.tensor.matmul(
                        pO[:, h, :], PT_[:, cc, :], V[:, vt, h * 64:(h + 1) * 64], start=(n2 == 0), stop=(n2 == len(chunks) - 1)),
                        reads=[PT_, R(V, vt, vt + 1)], writes=[R(pO, h, h + 1)])
                k.op("dve", lambda: nc.vector.reciprocal(s_[:, 2:3], s_[:, 1:2]), reads=[s_], writes=[s_])
                yb = ysb[n_ % 2]
                k.op("dve", lambda: nc.vector.tensor_scalar(yb[:, h * 64:(h + 1) * 64], pO[:, h, :], s_[:, 2:3], None, ALU.mult),
                     reads=[R(pO, h, h + 1), s_], writes=[yb])
                if h == 7:
                    self.out_proj_tile(t, ysb[n_ % 2], 4, wout, py[0], yT[n_ % 2], ptr[0], xb[n_ % 2], tb1)

            mode = os.environ.get("NA_PIPE", "4")
            if mode == "1":
                self.swpipe(len(units), [lambda u: (stA(u), stB(u), stC(u))])
            elif mode == "2":
                self.swpipe(len(units), [stA, lambda u: (stB(u), stC(u))])
            elif mode == "4":
                n_u = len(units)
                for i_ in range(n_u + 2):
                    if 0 <= i_ - 2 < n_u:
                        stC(i_ - 2)
                    if 0 <= i_ - 1 < n_u:
                        stB(i_ - 1)
                    if i_ < n_u:
                        stA(i_)
            else:
                self.swpipe(len(units), [stA, stB, stC])
            k.barrier()
            k.release(m2)
            k.release(m1)
        k.release(m0)

    def final_norm(self):
        k, nc = self.k, self.nc
        m = k.mark()
        gn = k.sb("fng", [128, D], F32)
        self.bcast_row_dma(gn, self.norm_final.ap()[0:1, :], D)
        xt = [k.sb(f"fx{j}", [128, D], F32) for j in range(2)]
        yt = [k.sb(f"fy{j}", [128, D], F32) for j in range(2)]
        junk = k.sb("fjunk", [128, D], BF16)
        st = [k.sb(f"fst{j}", [128, 4], F32) for j in range(2)]
        for t in range(16):
            x, y, s = xt[t % 2], yt[t % 2], st[t % 2]
            k.dma("sp", x[:], self.xt_ap(t), reads=[R(self.xres, t, t + 1)], writes=[x])
            k.op("dve", lambda s=s: nc.vector.memset(s[:], 0.0), writes=[s])
            k.op("act", lambda x=x, s=s: nc.scalar.activation(junk[:], x[:], AF.Square, accum_out=s[:, 0:1]),
                 reads=[x, s], writes=[junk, s])
            k.op("act", lambda s=s: nc.scalar.activation(s[:, 1:2], s[:, 0:1], AF.Ln, scale=1.0 / D, bias=EPS), reads=[s], writes=[s])
            k.op("act", lambda s=s: nc.scalar.activation(s[:, 2:3], s[:, 1:2], AF.Exp, scale=-0.5), reads=[s], writes=[s])
            k.op("dve", lambda x=x, y=y, s=s: nc.vector.scalar_tensor_tensor(y[:], x[:], s[:, 2:3], gn[:], ALU.mult, ALU.mult),
                 reads=[x, s, gn], writes=[y])
            k.dma("sp", self.out.ap()[t * 128:(t + 1) * 128, :], y[:], reads=[y], writes=[R(self.out, t, t + 1)])
        k.barrier()
        k.release(m)

    def build(self):
        k = self.k
        self.decl_inputs()
        self.setup_consts()
        for t in range(16):
            k.dma("sp", self.xres.ap()[t * 128:(t + 1) * 128, :], self.x_in.ap()[t * 128:(t + 1) * 128, :], writes=[R(self.xres, t, t + 1)])
        for t in range(2):
            k.dma("sp", self.xres.ap()[T + t * 128:T + (t + 1) * 128, :], self.ctx_in.ap()[t * 128:(t + 1) * 128, :], writes=[R(self.xres, 16 + t, 17 + t)])
        try:
            self.layers()
        except StopIteration:
            pass
        k.finish()
        k.close()

    def dump(self, name, buf, ap, shape):
        if not self.debug_x:
            return
        o = self.k.dram("dbg_" + name, list(shape), F32, kind="ExternalOutput")
        self.k.dma("pool", o.ap(), ap, reads=[buf], writes=[o])

    def chk(self, name):
        if self.stop == name:
            if name == "norm":
                self.dump("hT", self.hT, self.hT[:], [128, 8, NTOK])
                self.dump("mods", self.mods, self.mods[:], [128, 6, D])
            raise StopIteration

    def layers(self):
        self.chk("copy")
        for i in self.layer_list:
            last = i == DEPTH - 1
            kind, j = i % 3, i // 3
            tiles = list(range(NT))
            self.ada(i, 0)
            self.chk("ada")
            self.norm_to_hT(tiles, self.xt_ap)
            self.chk("norm")
            if kind == 0:
                self.mlstm(i, j, last)
            elif kind == 1:
                self.gla(i, j, last)
            else:
                self.na(i, j, last)
            self.chk("mixer")
            self.ada(i, 1)
            self.norm_to_hT(list(range(16)) if last else tiles, self.xt_ap)
            self.ffn(i, last)
            self.chk("ffn")
        self.final_norm()


def _consts():
    i = np.arange(128)
    ident = np.eye(128, dtype=np.float32)
    triu = (i[:, None] <= i[None, :]).astype(np.float32)
    tril = (i[:, None] >= i[None, :]).astype(np.float32)
    ones = np.ones((128, 128), np.float32)
    return np.stack([ident, triu, tril, ones, triu, tril]).astype(np.float32)


def prep_inputs(inp):
    f = lambda a: np.ascontiguousarray(np.asarray(a, dtype=np.float32))
    shared = {}
    for n in ("w_ada", "b_ada", "norm_mix", "norm_ffn", "w_up", "w_down", "a_w_in", "a_b_gate", "a_w_out",
              "b_w_in", "b_norm", "b_w_out", "c_w_in", "c_w_out"):
        shared[n] = f(inp[n])
    shared["norm_final"] = f(inp["norm_final"]).reshape(1, D)
    cw = np.concatenate([f(inp["ffn_conv_w"]), f(inp["ffn_conv_b"])[:, None, :]], axis=1)
    shared["ffn_cw"] = f(cw.reshape(DEPTH, 4, 44, 128).transpose(0, 3, 1, 2).reshape(DEPTH, 128, 4 * 44))
    acw = np.concatenate([f(inp["a_conv_w"]), f(inp["a_conv_b"])[:, None, :]], axis=1)
    shared["a_cw"] = f(acw.reshape(2, 4, 8, 128).transpose(0, 3, 1, 2).reshape(2, 128, 32))
    shared["b_w_a2e"] = f(np.concatenate([f(inp["b_w_a2"]), f(inp["b_b_a"])[:, :, None, :]], axis=2))
    qc = np.arange(64)
    dc = np.clip(qc[None, :] - qc[:, None] + 15, 0, 30)
    rp = f(inp["c_rpb"])
    rx = rp[:, :, :, dc]
    shared["c_rpbx"] = f(rx.transpose(0, 1, 3, 2, 4).reshape(1, 16, 64, 15 * 64))
    cst = np.clip(qc - 8, 0, 48)
    cm = np.where((qc[None, :] >= cst[:, None]) & (qc[None, :] < cst[:, None] + 16), 0.0, NEG).astype(np.float32)
    shared["cmask"] = f(np.concatenate([cm, cm], axis=0))
    shared["consts"] = _consts()
    cctx_t = f(np.asarray(inp["c_ctx"]).reshape(8, 128).T)
    maps = []
    for b in range(8):
        mdict = dict(shared)
        mdict["x"] = f(inp["x"][b])
        mdict["ctx"] = f(inp["ctx"][b])
        mdict["c_t"] = f(np.asarray(inp["c"][b]).reshape(8, 128).T)
        mdict["cctx_t"] = cctx_t
        maps.append(mdict)
    return maps


_CACHE = {}


def kernel(**inputs):
    if "mk" not in _CACHE:
        _CACHE["mk"] = MK()
    mk = _CACHE["mk"]
    maps = prep_inputs(inputs)
    res = run_bass_kernel_spmd(mk.nc, maps, core_ids=list(range(8)))
    return np.stack([np.asarray(r["out"], dtype=np.float32) for r in res.results], axis=0)
```

```python
import os
import numpy as np
import concourse.bass as bass
import concourse.mybir as mybir
from concourse.bass_utils import run_bass_kernel_spmd

F32 = mybir.dt.float32
BF16 = mybir.dt.bfloat16
AF = mybir.ActivationFunctionType
ALU = mybir.AluOpType
AX = mybir.AxisListType

ERA = 6000
NDMASEM = 12
INF = 1 << 60

D = 1024
T = 2048
TCX = 256
NTOK = T + TCX
NT = NTOK // 128
DEPTH = 4
FF = 2816
NFC = FF // 128
EPS = 1e-6
NEG = -30000.0


class Buf:
    def __init__(self, t, name):
        self.t = t
        self.name = name
        self.writes = []
        self.reads = []

    def __getitem__(self, idx):
        return self.t[idx]

    def ap(self):
        return self.t.ap()


class Acc:
    def __init__(self, buf, lo=0, hi=INF):
        self.buf, self.lo, self.hi = buf, lo, hi


def R(buf, lo=0, hi=INF):
    return Acc(buf, lo, hi)


def _acc(a):
    return a if isinstance(a, Acc) else Acc(a)


class KB:
    def __init__(self):
        self.nc = bass.Bass("TRN2", target_bir_lowering=False)
        nc = self.nc
        self.eng = {"pe": nc.tensor, "act": nc.scalar, "dve": nc.vector, "pool": nc.gpsimd, "sp": nc.sync}
        self.cnt = {e: 0 for e in self.eng}
        self.sems = {e: [] for e in self.eng}
        self.waited = {e: {} for e in self.eng}
        self.dma_n = {e: 0 for e in self.eng}
        self.dma_sems = {e: [] for e in self.eng}
        self.last_tok = {e: None for e in self.eng}
        self.dma_tok = {e: {} for e in self.eng}
        self._ctx = []
        self.nsem = 0
        self.sem_ids = {}

    def _enter(self, cm):
        v = cm.__enter__()
        self._ctx.append(cm)
        return v

    def mark(self):
        return len(self._ctx)

    def release(self, m):
        while len(self._ctx) > m:
            self._ctx.pop().__exit__(None, None, None)

    def close(self):
        self.release(0)

    def sem(self, name):
        self.nsem += 1
        s = self._enter(self.nc.semaphore(self._uname(name)))
        self.sem_ids[id(s)] = s
        return s

    def _uname(self, name):
        self.uid = getattr(self, "uid", 0) + 1
        return f"{name}_{self.uid}"

    def sb(self, name, shape, dt):
        name = self._uname(name)
        return Buf(self._enter(self.nc.sbuf_tensor(name, list(shape), dt)), name)

    def ps(self, name, shape, dt=F32):
        name = self._uname(name)
        return Buf(self._enter(self.nc.psum_tensor(name, list(shape), dt)), name)

    def dram(self, name, shape, dt, kind="Internal"):
        return Buf(self.nc.dram_tensor(name, list(shape), dt, kind=kind), name)

    def _eng_token(self, e):
        n = self.cnt[e]
        era = n // ERA
        while len(self.sems[e]) <= era:
            self.sems[e].append(self.sem(f"s_{e}_{len(self.sems[e])}"))
        self.cnt[e] = n + 1
        return (self.sems[e][era], (n % ERA) + 1)

    def _wait(self, e, token):
        sem, val = token
        w = self.waited[e]
        key = id(sem)
        if w.get(key, 0) >= val:
            return
        w[key] = val
        self.eng[e].wait_ge(sem, val)

    def _deps(self, e, reads, writes, is_dma):
        toks = []
        for a in reads:
            a = _acc(a)
            for (lo, hi, tok, we) in a.buf.writes:
                if lo < a.hi and a.lo < hi:
                    toks.append(tok)
        for a in writes:
            a = _acc(a)
            for (lo, hi, tok, we) in a.buf.writes:
                if lo < a.hi and a.lo < hi and (is_dma or we != e):
                    toks.append(tok)
            for (lo, hi, tok, re) in a.buf.reads:
                if lo < a.hi and a.lo < hi and (is_dma or re != e):
                    toks.append(tok)
        for t in toks:
            self._wait(e, t)

    def _record(self, e, tok, reads, writes, is_dma):
        tag = ("dma", e) if is_dma else e
        for a in reads:
            a = _acc(a)
            b = a.buf
            if not is_dma:
                b.reads = [r for r in b.reads if not (r[3] == tag and r[0] >= a.lo and r[1] <= a.hi)]
            b.reads.append((a.lo, a.hi, tok, tag))
        for a in writes:
            a = _acc(a)
            b = a.buf
            b.writes = [w for w in b.writes if not (w[0] >= a.lo and w[1] <= a.hi)]
            b.reads = [r for r in b.reads if not (r[0] >= a.lo and r[1] <= a.hi)]
            b.writes.append((a.lo, a.hi, tok, tag))

    def op(self, e, fn, reads=(), writes=(), rg=0):
        if e == "pe":
            if rg != getattr(self, "pe_rg", 0) and self.last_tok["pe"] is not None:
                self._wait("pe", self.last_tok["pe"])
            self.pe_rg = rg
        self._deps(e, reads, writes, False)
        ins = fn()
        tok = self._eng_token(e)
        ins.then_inc(tok[0], 1)
        self.last_tok[e] = tok
        self._record(e, tok, reads, writes, False)
        return tok

    def dma(self, e, out_ap, in_ap, reads=(), writes=(), **kw):
        n = self.dma_n[e]
        if not self.dma_sems[e]:
            self.dma_sems[e] = [self.sem(f"d_{e}_{i}") for i in range(NDMASEM)]
        sem = self.dma_sems[e][n % NDMASEM]
        prev = 16 * (n // NDMASEM)
        if prev:
            self._wait(e, (sem, prev))
        self._deps(e, reads, writes, True)
        self.eng[e].dma_start(out=out_ap, in_=in_ap, **kw).then_inc(sem, 16)
        self.dma_n[e] = n + 1
        tok = (sem, prev + 16)
        assert prev + 16 < 32000, "dma sem overflow"
        self.dma_tok[e][n % NDMASEM] = tok
        self._record(e, tok, reads, writes, True)
        return tok

    def barrier(self):
        toks = [t for t in self.last_tok.values() if t is not None]
        for e in self.dma_tok:
            toks += list(self.dma_tok[e].values())
        for e in self.eng:
            for t in toks:
                self._wait(e, t)

    def finish(self):
        self.barrier()


def bc_last(ap, n):
    return bass.AP(ap.tensor, ap.offset, [list(d) for d in ap.ap] + [[0, n]])


def bc_mid(ap, pos, n):
    l = [list(d) for d in ap.ap]
    l.insert(pos, [0, n])
    return bass.AP(ap.tensor, ap.offset, l)


class MK:
    def __init__(self, nlayers=DEPTH, debug_x=False, stop=None, layer_list=None):
        self.stop = stop
        self.layer_list = list(range(nlayers)) if layer_list is None else layer_list
        self.k = KB()
        self.nc = self.k.nc
        self.nlayers = nlayers
        self.debug_x = debug_x
        self.build()

    def decl_inputs(self):
        k = self.k
        I = lambda n, s: k.dram(n, s, F32, kind="ExternalInput")
        self.x_in = I("x", [T, D])
        self.ctx_in = I("ctx", [TCX, D])
        self.c_t = I("c_t", [128, 8])
        self.cctx_t = I("cctx_t", [128, 8])
        self.w_ada = I("w_ada", [DEPTH, D, 6 * D])
        self.b_ada = I("b_ada", [DEPTH, 6 * D])
        self.norm_mix = I("norm_mix", [DEPTH, D])
        self.norm_ffn = I("norm_ffn", [DEPTH, D])
        self.w_up = I("w_up", [DEPTH, D, 2 * FF])
        self.ffn_cw = I("ffn_cw", [DEPTH, 128, 4 * 44])
        self.w_down = I("w_down", [DEPTH, FF, D])
        self.a_w_in = I("a_w_in", [2, D, 3104])
        self.a_b_gate = I("a_b_gate", [2, 32])
        self.a_cw = I("a_cw", [2, 128, 4 * 8])
        self.a_w_out = I("a_w_out", [2, D, D])
        self.b_w_in = I("b_w_in", [1, D, 3104])
        self.b_w_a2 = I("b_w_a2e", [1, 2, 17, 512])
        self.b_norm = I("b_norm", [1, 256])
        self.b_w_out = I("b_w_out", [1, D, D])
        self.c_w_in = I("c_w_in", [1, D, 3 * D])
        self.c_rpbx = I("c_rpbx", [1, 16, 64, 15 * 64])
        self.cmask_in = I("cmask", [128, 64])
        self.c_w_out = I("c_w_out", [1, D, D])
        self.norm_final = I("norm_final", [1, D])
        self.consts = I("consts", [6, 128, 128])
        self.out = k.dram("out", [T, D], F32, kind="ExternalOutput")
        self.xres = k.dram("xres", [NTOK, D], F32, kind="ExternalOutput" if self.debug_x else "Internal")

    @staticmethod
    def swpipe(n, stages, skew=1):
        S = len(stages)
        for i in range(n + (S - 1) * skew):
            for si, fn in enumerate(stages):
                u = i - si * skew
                if 0 <= u < n:
                    fn(u)

    def xt_ap(self, t):
        return self.xres.ap()[t * 128:(t + 1) * 128, :]

    def bcast_row_dma(self, dstbuf, row_ap_1xn, n):
        src = bass.AP(row_ap_1xn.tensor, row_ap_1xn.offset, [[0, 128], [1, n]])
        self.k.dma("sp", dstbuf[:], src, writes=[dstbuf])

    def load_w(self, dstbuf, dst_ap, src2d, rng=None):
        src = src2d.rearrange("(kc k) n -> k kc n", k=128)
        w = [dstbuf] if rng is None else [R(dstbuf, *rng)]
        return self.k.dma("pool", dst_ap, src, writes=w)

    def setup_consts(self):
        k, nc = self.k, self.nc
        self.cf = k.sb("cf", [128, 6, 128], F32)
        self.cb = k.sb("cb", [128, 6, 128], BF16)
        k.dma("sp", self.cf[:], self.consts.ap().rearrange("c p n -> p c n"), writes=[self.cf])
        k.dma("pool", self.cb[:], self.consts.ap().rearrange("c p n -> p c n"), writes=[self.cb])
        self.ident_b = self.cb[:, 0, :]
        self.triu_f = self.cf[:, 1, :]
        self.tril_f = self.cf[:, 2, :]
        self.ones_f = self.cf[:, 3, :]
        self.sbc = k.sb("sbc", [128, 2, 8, 128], BF16)
        cs = k.sb("cs", [128, 2, 8], F32)
        k.dma("sp", cs[:, 0, :], self.c_t.ap(), writes=[cs])
        k.dma("sp", cs[:, 1, :], self.cctx_t.ap(), writes=[cs])
        cs2 = k.sb("cs2", [128, 2, 8], F32)
        k.op("act", lambda: nc.scalar.activation(cs2[:], cs[:], AF.Silu), reads=[cs], writes=[cs2])
        k.op("dve", lambda: nc.vector.tensor_copy(self.sbc[:].rearrange("p a b m -> p (a b) m"),
                                                  bc_last(cs2[:].rearrange("p a b -> p (a b)"), 128)),
             reads=[cs2], writes=[self.sbc])
        self.hT = k.sb("hT", [128, 8, NTOK], BF16)
        self.mods = k.sb("mods", [128, 6, D], F32)
        self.ssz = k.sb("ssz", [128, 4], F32)

    def ada(self, i, phase):
        k, nc = self.k, self.nc
        m = k.mark()
        wbuf = [k.sb(f"adaw{j}", [128, 8, 512], BF16) for j in range(2)]
        bb = k.sb("adab", [128, 3 * D], F32)
        gn = k.sb("adag", [128, D], F32)
        pp = [k.ps(f"adap{j}", [128, 512], F32) for j in range(2)]
        c0 = phase * 3 * D
        self.bcast_row_dma(bb, self.b_ada.ap()[i:i + 1, c0:c0 + 3 * D], 3 * D)
        g_src = (self.norm_mix if phase == 0 else self.norm_ffn).ap()[i:i + 1, :]
        self.bcast_row_dma(gn, g_src, D)
        n = 0
        for blk in range(6):
            wb = wbuf[blk % 2]
            self.load_w(wb, wb[:], self.w_ada.ap()[i, :, c0 + blk * 512:c0 + (blk + 1) * 512])
            part, half = blk // 2, blk % 2
            for lc in range(2):
                p = pp[n % 2]
                n += 1
                for kc in range(8):
                    k.op("pe", lambda kc=kc, p=p, wb=wb, lc=lc: nc.tensor.matmul(
                        p[:], self.sbc[:, lc, kc, :], wb[:, kc, :], start=(kc == 0), stop=(kc == 7)),
                        reads=[self.sbc, wb], writes=[p])
                bsl = bb[:, blk * 512:(blk + 1) * 512]
                dst = self.mods[:, lc * 3 + (1, 0, 2)[part], half * 512:(half + 1) * 512]
                if part == 1:
                    tmp = gn[:, half * 512:(half + 1) * 512]
                    k.op("dve", lambda p=p, bsl=bsl, dst=dst: nc.vector.scalar_tensor_tensor(
                        dst, p[:], 1.0, bsl, ALU.add, ALU.add), reads=[p, bb], writes=[self.mods])
                    k.op("dve", lambda dst=dst, tmp=tmp: nc.vector.tensor_tensor(dst, dst, tmp, ALU.mult),
                         reads=[self.mods, gn], writes=[self.mods])
                else:
                    k.op("dve", lambda p=p, bsl=bsl, dst=dst: nc.vector.tensor_tensor(dst, p[:], bsl, ALU.add),
                         reads=[p, bb], writes=[self.mods])
        k.barrier()
        k.release(m)

    def norm_to_hT(self, tiles, src_fn):
        k, nc = self.k, self.nc
        m = k.mark()
        NB = 3
        xt = [k.sb(f"nx{j}", [128, D], F32) for j in range(NB)]
        junk = k.sb("njunk", [128, D], BF16)
        tmp = [k.sb(f"ntmp{j}", [128, D], F32) for j in range(NB)]
        hb = [k.sb(f"nhb{j}", [128, D], BF16) for j in range(NB)]
        st = [k.sb(f"nst{j}", [128, 4], F32) for j in range(NB)]
        pt = [k.ps(f"npt{j}", [128, 8, 128], BF16) for j in range(2)]

        def stA(n):
            t = tiles[n]
            lc = 0 if t < 16 else 1
            x, s, tm, h = xt[n % NB], st[n % NB], tmp[n % NB], hb[n % NB]
            k.dma("sp", x[:], src_fn(t), reads=[R(self.xres, t, t + 1)], writes=[x])
            k.op("pool", lambda: nc.gpsimd.memset(s[:], 0.0), writes=[s])
            k.op("act", lambda: nc.scalar.activation(junk[:], x[:], AF.Square, accum_out=s[:, 0:1]), reads=[x, s], writes=[junk, s])
            k.op("act", lambda: nc.scalar.activation(s[:, 1:2], s[:, 0:1], AF.Ln, scale=1.0 / D, bias=EPS), reads=[s], writes=[s])
            k.op("act", lambda: nc.scalar.activation(s[:, 2:3], s[:, 1:2], AF.Exp, scale=-0.5), reads=[s], writes=[s])
            k.op("dve", lambda: nc.vector.scalar_tensor_tensor(tm[:], x[:], s[:, 2:3], self.mods[:, lc * 3 + 0, :], ALU.mult, ALU.mult),
                 reads=[x, s, self.mods], writes=[tm])
            k.op("pool", lambda: nc.gpsimd.tensor_tensor(h[:], tm[:], self.mods[:, lc * 3 + 1, :], ALU.add),
                 reads=[tm, self.mods], writes=[h])

        def stB(n):
            t = tiles[n]
            h, p = hb[n % NB], pt[n % 2]
            for kc in range(8):
                k.op("pe", lambda kc=kc: nc.tensor.transpose(p[:, kc, :], h[:, kc * 128:(kc + 1) * 128], self.ident_b),
                     reads=[h, self.cb], writes=[p])
            k.op("act", lambda: nc.scalar.copy(self.hT[:, :, t * 128:(t + 1) * 128], p[:]),
                 reads=[p], writes=[R(self.hT, t * 128, (t + 1) * 128)])

        self.swpipe(len(tiles), [stA, stB])
        k.barrier()
        k.release(m)

    def resid_update(self, t, ypsum, xb, tb):
        k, nc = self.k, self.nc
        lc = 0 if t < 16 else 1
        k.dma("sp", xb[:], self.xt_ap(t), reads=[R(self.xres, t, t + 1)], writes=[xb])
        k.op("dve", lambda: nc.vector.tensor_tensor(tb[:], ypsum[:], self.mods[:, lc * 3 + 2, :], ALU.mult),
             reads=[ypsum, self.mods], writes=[tb])
        k.op("dve", lambda: nc.vector.tensor_tensor(xb[:], xb[:], tb[:], ALU.add), reads=[xb, tb], writes=[xb])
        k.dma("sp", self.xt_ap(t), xb[:], reads=[xb], writes=[R(self.xres, t, t + 1)])

    def conv_evac(self, dst_f32, p, cw, j, ncols, has_left, has_right, nout):
        k, nc = self.k, self.nc
        o = 1 if has_left else 0
        k.op("act", lambda: nc.scalar.activation(dst_f32[:, 0:nout], p[:, o:o + nout], AF.Identity,
                                                 scale=cw[:, 1, j:j + 1], bias=cw[:, 3, j:j + 1]),
             reads=[p, cw], writes=[dst_f32])
        lo = 0 if has_left else 1
        k.op("dve", lambda: nc.vector.scalar_tensor_tensor(dst_f32[:, lo:nout], p[:, o + lo - 1:o + nout - 1],
                                                           cw[:, 0, j:j + 1], dst_f32[:, lo:nout], ALU.mult, ALU.add),
             reads=[p, cw, dst_f32], writes=[dst_f32])
        hi = nout if has_right else nout - 1
        k.op("dve", lambda: nc.vector.scalar_tensor_tensor(dst_f32[:, 0:hi], p[:, o + 1:o + hi + 1],
                                                           cw[:, 2, j:j + 1], dst_f32[:, 0:hi], ALU.mult, ALU.add),
             reads=[p, cw, dst_f32], writes=[dst_f32])

    @staticmethod
    def tblocks(tiles_lat=True, tiles_ctx=True):
        bl = []
        if tiles_lat:
            bl += [(s, s + 256, 0, T) for s in range(0, T, 256)]
        if tiles_ctx:
            bl += [(T, T + 256, T, NTOK)]
        return bl

    def ffn(self, i, last):
        k, nc = self.k, self.nc
        m = k.mark()
        cw = k.sb("fcw", [128, 4, 44], F32)
        k.dma("sp", cw[:], self.ffn_cw.ap()[i].rearrange("p (a j) -> p a j", a=4), writes=[cw])
        wd = k.sb("fwd", [128, NFC, D], BF16)
        wa = [k.sb(f"fwa{j}", [128, 8, 256], BF16) for j in range(2)]
        wg = [k.sb(f"fwg{j}", [128, 8, 256], BF16) for j in range(2)]
        actT = k.sb("factT", [128, NFC, 768], BF16)
        pa = [k.ps(f"fpa{j}", [128, 512], F32) for j in range(2)]
        pg = [k.ps(f"fpg{j}", [128, 512], F32) for j in range(2)]
        py = [k.ps(f"fpy{j}", [128, D], F32) for j in range(2)]
        ua = [k.sb(f"fua{j}", [128, 384], F32) for j in range(2)]
        ug = [k.sb(f"fug{j}", [128, 384], F32) for j in range(2)]
        xb = [k.sb(f"fxb{j}", [128, D], F32) for j in range(2)]
        tb1 = k.sb("ftb", [128, D], F32)
        tb = [tb1, tb1]
        groups = [(j, j + 2) for j in range(0, NFC, 2)]
        tgs = [[(0, 384, 0, T), (384, 768, 0, T)], [(768, 1152, 0, T), (1152, 1536, 0, T)],
               [(1536, 1920, 0, T), (1920, 2048, 0, T)] + ([] if last else [(T, NTOK, T, NTOK)])]
        cnt = 0
        gi = 0
        wd_loaded = False
        for tg in tgs:
            base = tg[0][0]
            for (j0, j1) in groups:
                wab, wgb = wa[gi % 2], wg[gi % 2]
                gi += 1
                nj = j1 - j0
                self.load_w(wab, wab[:, :, 0:nj * 128], self.w_up.ap()[i, :, j0 * 128:j1 * 128])
                self.load_w(wgb, wgb[:, :, 0:nj * 128], self.w_up.ap()[i, :, FF + j0 * 128:FF + j1 * 128])
                if not wd_loaded and gi == 2:
                    wd_loaded = True
                    for q in range(4):
                        jj0, jj1 = (0, 6, 12, 17, 22)[q], (0, 6, 12, 17, 22)[q + 1]
                        self.load_w(wd, wd[:, jj0:jj1, :], self.w_down.ap()[i, jj0 * 128:jj1 * 128, :], rng=(jj0, jj1))
                for (s, e, ss, se) in tg:
                    cs, ce = max(s - 1, ss), min(e + 1, se)
                    hl, hr = cs < s, ce > e
                    ncol = ce - cs
                    off = (s - base) if s < T else (512 + s - T)
                    for j in range(j0, j1):
                        A, G, UA, UG = pa[cnt % 2], pg[cnt % 2], ua[cnt % 2], ug[cnt % 2]
                        cnt += 1
                        for (P, W) in ((A, wab), (G, wgb)):
                            for kc in range(8):
                                k.op("pe", lambda P=P, W=W, kc=kc, j=j: nc.tensor.matmul(
                                    P[:, 0:ncol], W[:, kc, (j - j0) * 128:(j - j0 + 1) * 128], self.hT[:, kc, cs:ce],
                                    start=(kc == 0), stop=(kc == 7)),
                                    reads=[W, R(self.hT, cs, ce)], writes=[P])
                        nt_ = e - s
                        self.conv_evac(UA, A, cw, j, ncol, hl, hr, nt_)
                        self.conv_evac(UG, G, cw, 22 + j, ncol, hl, hr, nt_)
                        k.op("act", lambda UG=UG, nt_=nt_: nc.scalar.activation(UG[:, 0:nt_], UG[:, 0:nt_], AF.Silu), reads=[UG], writes=[UG])
                        k.op("dve", lambda UA=UA, UG=UG, j=j, off=off, nt_=nt_: nc.vector.tensor_tensor(
                            actT[:, j, off:off + nt_], UA[:, 0:nt_], UG[:, 0:nt_], ALU.mult),
                            reads=[UA, UG], writes=[R(actT, off, off + nt_)])
            tiles = sorted(set(tt for (s, e, _, _) in tg for tt in range(s // 128, e // 128)))
            for n, t in enumerate(tiles):
                Y = py[n % 2]
                off = (t * 128 - base) if t < 16 else (512 + t * 128 - T)
                for nh in range(2):
                    for j in range(NFC):
                        k.op("pe", lambda Y=Y, j=j, nh=nh, off=off: nc.tensor.matmul(
                            Y[:, nh * 512:(nh + 1) * 512], actT[:, j, off:off + 128], wd[:, j, nh * 512:(nh + 1) * 512],
                            start=(j == 0), stop=(j == NFC - 1)),
                            reads=[R(actT, off, off + 128), R(wd, j, j + 1)], writes=[R(Y, nh * 512, (nh + 1) * 512)])
                self.resid_update(t, Y, xb[n % 2], tb[n % 2])
        k.barrier()
        k.release(m)

    def out_proj_tile(self, t, ysb, nkc, wout, py, yT, ptr, xb, tb):
        k, nc = self.k, self.nc
        for kc in range(nkc):
            k.op("pe", lambda kc=kc: nc.tensor.transpose(ptr[:, kc, :], ysb[:, kc * 128:(kc + 1) * 128], self.ident_b),
                 reads=[ysb, self.cb], writes=[ptr])
        k.op("act", lambda: nc.scalar.copy(yT[:, 0:nkc, :], ptr[:, 0:nkc, :]), reads=[ptr], writes=[yT])
        for nh in range(2):
            for kc in range(nkc):
                k.op("pe", lambda kc=kc, nh=nh: nc.tensor.matmul(
                    py[:, nh * 512:(nh + 1) * 512], yT[:, kc, :], wout[:, kc, nh * 512:(nh + 1) * 512],
                    start=(kc == 0), stop=(kc == nkc - 1)),
                    reads=[yT, wout], writes=[R(py, nh * 512, (nh + 1) * 512)])
        if xb is not None:
            self.resid_update(t, py, xb, tb)

    def proj_tok(self, p_ap, pbuf, t, w, c0, n, prange=None):
        k, nc = self.k, self.nc
        for kc in range(8):
            k.op("pe", lambda kc=kc: nc.tensor.matmul(p_ap, self.hT[:, kc, t * 128:(t + 1) * 128], w[:, kc, c0:c0 + n],
                                                      start=(kc == 0), stop=(kc == 7)),
                 reads=[R(self.hT, t * 128, (t + 1) * 128), w], writes=[pbuf if prange is None else R(pbuf, *prange)])

    def mlstm(self, i, jm, last):
        k, nc = self.k, self.nc
        m0 = k.mark()
        W = self.a_w_in.ap()[jm]
        GT = k.sb("mGT", [128, NT, 32], F32)
        GP = k.sb("mGP", [128, NT, 3, 16], F32)
        bg = k.sb("mbg", [128, 32], F32)
        self.bcast_row_dma(bg, self.a_b_gate.ap()[jm:jm + 1, :], 32)
        cw = k.sb("mcw", [128, 4, 8], F32)
        k.dma("sp", cw[:], self.a_cw.ap()[jm].rearrange("p (a j) -> p a j", a=4), writes=[cw])
        fwd_order = [16, 17] + list(range(16))
        bwd_order = [17, 16] + list(range(15, -1, -1))
        for hg in range(2):
            m1 = k.mark()
            qkT = k.sb("mqkT", [128, 4, NTOK], BF16)
            V = k.sb("mV", [128, NT, 4, 130], BF16)
            Kt = k.sb("mKt", [128, NT, 256], BF16)
            H = k.sb("mH", [128, NT, 512], BF16)
            wq = k.sb("mwq", [128, 8, 512], BF16)
            wv = k.sb("mwv", [128, 8, 512], BF16)
            wo = k.sb("mwo", [128, 8, 512], BF16)
            wg_ = k.sb("mwg", [128, 8, 32], BF16)
            wout = k.sb("mwout", [128, 4, D], BF16)
            self.load_w(wq, wq[:, :, 0:256], W[:, hg * 256:(hg + 1) * 256], rng=(0, 256))
            self.load_w(wq, wq[:, :, 256:512], W[:, 512 + hg * 256:512 + (hg + 1) * 256], rng=(256, 512))
            self.load_w(wv, wv[:], W[:, 1024 + hg * 512:1024 + (hg + 1) * 512])
            self.load_w(wo, wo[:], W[:, 2048 + hg * 512:2048 + (hg + 1) * 512])
            self.load_w(wout, wout[:], self.a_w_out.ap()[jm, hg * 512:(hg + 1) * 512, :])
            if hg == 0:
                self.load_w(wg_, wg_[:], W[:, 3072:3104])
            k.op("dve", lambda: nc.vector.memset(V[:, :, :, 128:130], 1.0), writes=[V])
            m2 = k.mark()
            pq = [k.ps(f"mpq{j}", [128, 512], F32) for j in range(2)]
            uq = [k.sb(f"muq{j}", [128, 256], F32) for j in range(2)]
            cnt = 0
            for (s, e, ss, se) in self.tblocks():
                cs, ce = max(s - 1, ss), min(e + 1, se)
                hl, hr = cs < s, ce > e
                ncol = ce - cs
                for ch in range(4):
                    P, U = pq[cnt % 2], uq[cnt % 2]
                    cnt += 1
                    for kc in range(8):
                        k.op("pe", lambda P=P, kc=kc, ch=ch: nc.tensor.matmul(
                            P[:, 0:ncol], wq[:, kc, ch * 128:(ch + 1) * 128], self.hT[:, kc, cs:ce],
                            start=(kc == 0), stop=(kc == 7)), reads=[wq, R(self.hT, cs, ce)], writes=[P])
                    jcol = (ch // 2) * 4 + hg * 2 + (ch % 2)
                    self.conv_evac(U, P, cw, jcol, ncol, hl, hr, 256)
                    k.op("act", lambda U=U: nc.scalar.activation(U[:], U[:], AF.Silu), reads=[U], writes=[U])
                    sc = 0.125 if ch < 2 else 1.0
                    k.op("dve", lambda U=U, ch=ch, s=s, sc=sc: nc.vector.tensor_scalar(
                        qkT[:, ch, s:s + 256], U[:], sc, None, ALU.mult), reads=[U], writes=[R(qkT, s, s + 256)])
            k.barrier()
            if self.stop == "m_qk":
                self.dump("qkT", qkT, qkT[:], [128, 4, NTOK])
            self.chk("m_qk")
            k.release(m2)
            m2 = k.mark()
            pv = [k.ps(f"mpv{j}", [128, 512], F32) for j in range(2)]
            pgt = [k.ps(f"mpg{j}", [128, 512], F32) for j in range(2)]
            pk = [k.ps(f"mpk{j}", [128, 8, 128], BF16) for j in range(2)]
            for t in range(NT):
                P = pv[t % 2]
                self.proj_tok(P[:], P, t, wv, 0, 512)
                k.op("act", lambda P=P, t=t: nc.scalar.copy(V[:, t, :, 0:128], P[:].rearrange("p (h e) -> p h e", h=4)),
                     reads=[P], writes=[R(V, t, t + 1)])
                if hg == 0:
                    Pg = pgt[t % 2]
                    self.proj_tok(Pg[:, 0:32], Pg, t, wg_, 0, 32)
                    k.op("dve", lambda Pg=Pg, t=t: nc.vector.tensor_tensor(GT[:, t, :], Pg[:, 0:32], bg[:], ALU.add),
                         reads=[Pg, bg], writes=[R(GT, t, t + 1)])
                Pk = pk[t % 2]
                for c2 in range(2):
                    k.op("pe", lambda Pk=Pk, c2=c2, t=t: nc.tensor.transpose(
                        Pk[:, c2, :], qkT[:, 2 + c2, t * 128:(t + 1) * 128], self.ident_b),
                        reads=[R(qkT, t * 128, (t + 1) * 128), self.cb], writes=[Pk])
                k.op("act", lambda Pk=Pk, t=t: nc.scalar.copy(Kt[:, t, :].rearrange("p (a b) -> p a b", a=2), Pk[:, 0:2, :]),
                     reads=[Pk], writes=[R(Kt, t, t + 1)])
            k.barrier()
            if self.stop == "m_v":
                self.dump("V", V, V[:], [128, NT, 4, 130])
                self.dump("Kt", Kt, Kt[:], [128, NT, 256])
                self.dump("GT", GT, GT[:], [128, NT, 32])
            self.chk("m_v")
            k.release(m2)
            if hg == 0:
                m2 = k.mark()
                e1 = [k.sb(f"me1{j}", [128, 2, 8], F32) for j in range(2)]
                sp_ = [k.sb(f"msp{j}", [128, 2, 8], F32) for j in range(2)]
                aa = [k.sb(f"maa{j}", [128, 2, 8], F32) for j in range(2)]
                pgp = [k.ps(f"mpgp{j}", [128, 2, 256], F32) for j in range(2)]
                for t in range(NT):
                    E1, SP, A_, PG = e1[t % 2], sp_[t % 2], aa[t % 2], pgp[t % 2]
                    gv = GT[:, t, :].rearrange("p (d w h) -> p d w h", d=2, w=2)
                    k.op("act", lambda E1=E1, gv=gv: nc.scalar.activation(E1[:], gv[:, :, 1, :], AF.Exp, scale=-1.0),
                         reads=[R(GT, t, t + 1)], writes=[E1])
                    k.op("act", lambda E1=E1, SP=SP: nc.scalar.activation(SP[:], E1[:], AF.Ln, bias=1.0), reads=[E1], writes=[SP])
                    k.op("pe", lambda PG=PG, SP=SP: nc.tensor.matmul(PG[:, 0, 0:8], self.triu_f, SP[:, 0, :], start=True, stop=True),
                         reads=[self.cf, SP], writes=[R(PG, 0, 8)])
                    k.op("pe", lambda PG=PG, SP=SP: nc.tensor.matmul(PG[:, 0, 8:16], self.tril_f, SP[:, 1, :], start=True, stop=True),
                         reads=[self.cf, SP], writes=[R(PG, 8, 16)])
                    k.op("pe", lambda PG=PG, SP=SP: nc.tensor.matmul(PG[:, 1, 0:16], self.ones_f, SP[:].rearrange("p a b -> p (a b)"), start=True, stop=True),
                         reads=[self.cf, SP], writes=[R(PG, 16, 32)])
                    k.op("dve", lambda A_=A_, PG=PG, gv=gv: nc.vector.tensor_tensor(
                        A_[:], PG[:, 0, 0:16].rearrange("p (a b) -> p a b", a=2), gv[:, :, 0, :], ALU.add),
                        reads=[PG, R(GT, t, t + 1)], writes=[A_])
                    k.op("act", lambda A_=A_, t=t: nc.scalar.activation(GP[:, t, 0, :], A_[:].rearrange("p a b -> p (a b)"), AF.Exp),
                         reads=[A_], writes=[R(GP, t, t + 1)])
                    k.op("act", lambda PG=PG, t=t: nc.scalar.activation(GP[:, t, 1:3, :], PG[:, :, 0:16], AF.Exp, scale=-1.0),
                         reads=[PG], writes=[R(GP, t, t + 1)])
                k.barrier()
                if self.stop == "m_gate":
                    self.dump("GP", GP, GP[:], [128, NT, 3, 16])
                self.chk("m_gate")
                k.release(m2)
            m2 = k.mark()
            C32 = [k.sb(f"mC32{d}", [128, 2, 130], F32) for d in range(2)]
            Cb = [k.sb(f"mCb{d}", [128, 2, 130], BF16) for d in range(2)]
            Vp = [k.sb(f"mVp{d}", [128, 4, 130], BF16) for d in range(2)]
            ST = [k.sb(f"mST{d}", [128, 4, 128], BF16) for d in range(2)]
            dn = [k.sb(f"mdn{d}", [128, 4, 4], F32) for d in range(2)]
            hc = [k.sb(f"mhc{d}", [128, 512], F32) for d in range(2)]
            tmpC = [k.sb(f"mtC{d}", [128, 2, 130], F32) for d in range(2)]
            pqk = [k.ps(f"mpqk{d}", [128, 4, 128], F32) for d in range(2)]
            pnd = [k.ps(f"mpnd{d}", [128, 2, 512], F32) for d in range(2)]
            pdc1 = k.ps("mpdc", [128, 2, 512], F32)
            pdc = [pdc1, pdc1]
            for d in range(2):
                k.op("dve", lambda d=d: nc.vector.memset(C32[d][:], 0.0), writes=[C32[d]])
                k.op("dve", lambda d=d: nc.vector.memset(Cb[d][:], 0.0), writes=[Cb[d]])
            LVL = int(os.environ.get("SCAN_LVL", "9"))
            for step in range(NT):
                for d in range(2):
                    t = (fwd_order, bwd_order)[d][step]
                    mask = self.cf[:, 4 + d, :]
                    ea = GP[:, t, 0, d * 8 + hg * 4:d * 8 + hg * 4 + 4]
                    inter = GP[:, t, 1, d * 8 + hg * 4:d * 8 + hg * 4 + 4]
                    egl = GP[0:64, t, 2, d * 8 + hg * 4:d * 8 + hg * 4 + 4]
                    k.op("pool", lambda d=d, t=t, ea=ea: nc.gpsimd.tensor_tensor(
                        Vp[d][:, :, 0:129], V[:, t, :, 0:129], bc_last(ea, 129), ALU.mult),
                        reads=[R(V, t, t + 1), R(GP, t, t + 1)], writes=[Vp[d]])
                    if LVL < 2:
                        continue
                    for h in (0, 2, 1, 3):
                        pb = (h % 2) * 64
                        k.op("pe", lambda d=d, h=h, pb=pb, t=t: nc.tensor.matmul(
                            pqk[d][:, h, :], qkT[pb:pb + 64, 2 + h // 2, t * 128:(t + 1) * 128],
                            qkT[pb:pb + 64, h // 2, t * 128:(t + 1) * 128], start=True, stop=True),
                            reads=[R(qkT, t * 128, (t + 1) * 128)], writes=[R(pqk[d], h, h + 1)], rg=pb)
                    k.op("dve", lambda d=d, mask=mask: nc.vector.tensor_tensor(
                        ST[d][:], pqk[d][:], bc_mid(mask, 1, 4), ALU.mult), reads=[pqk[d], self.cf], writes=[ST[d]])
                    if LVL < 3:
                        continue
                    def qc_mm(h, start, stop):
                        pb = (h % 2) * 64
                        o_ap = pnd[d][:, h // 2, (h % 2) * 130:(h % 2) * 130 + 129]
                        k.op("pe", lambda d=d, h=h, pb=pb, t=t, o_ap=o_ap: nc.tensor.matmul(
                            o_ap, qkT[pb:pb + 64, h // 2, t * 128:(t + 1) * 128], Cb[d][pb:pb + 64, h // 2, 0:129], start=start, stop=stop),
                            reads=[R(qkT, t * 128, (t + 1) * 128), Cb[d]], writes=[R(pnd[d], h, h + 1)], rg=pb)

                    def sv_mm(h, start, stop):
                        o_ap = pnd[d][:, h // 2, (h % 2) * 130:(h % 2) * 130 + 129]
                        k.op("pe", lambda d=d, h=h, o_ap=o_ap: nc.tensor.matmul(
                            o_ap, ST[d][:, h, :], Vp[d][:, h, 0:129], start=start, stop=stop),
                            reads=[ST[d], Vp[d]], writes=[R(pnd[d], h, h + 1)])
                    qc_mm(1, True, False)
                    qc_mm(3, True, False)
                    sv_mm(1, False, True)
                    sv_mm(3, False, True)
                    sv_mm(0, True, False)
                    sv_mm(2, True, False)
                    qc_mm(0, False, True)
                    qc_mm(2, False, True)
                    if self.stop == "m_scan" and step == 1 and d == 0 and self.debug_x and os.environ.get("DBG_PND"):
                        dbgt = k.sb("dbgt", [128, 2, 512], F32)
                        k.op("act", lambda: nc.scalar.copy(dbgt[:], pnd[d][:]), reads=[pnd[d]], writes=[dbgt])
                        self.dump("pnd", dbgt, dbgt[:], [128, 2, 512])
                        self.dump("ST", ST[d], ST[d][:], [128, 4, 128])
                        self.dump("Vp", Vp[d], Vp[d][:], [128, 4, 130])
                        self.dump("Cb", Cb[d], Cb[d][:], [128, 2, 130])
                    if LVL < 4:
                        continue
                    den_ap = bass.AP(pnd[d][:, 0, 128:129].tensor, pnd[d][:, 0, 128:129].offset, [list(pnd[d][:, 0, 128:129].ap[0]), [512, 2], [130, 2]])
                    k.op("dve", lambda d=d, den_ap=den_ap, inter=inter: nc.vector.tensor_tensor(
                        dn[d][:, 0, :].rearrange("p (a b) -> p a b", a=2), den_ap, inter.rearrange("p (a b) -> p a b", a=2), ALU.mult),
                        reads=[pnd[d], R(GP, t, t + 1)], writes=[R(dn[d], 0, 1)])
                    k.op("dve", lambda d=d: nc.vector.scalar_tensor_tensor(dn[d][:, 1, :], dn[d][:, 0, :], -1.0, dn[d][:, 0, :], ALU.mult, ALU.max),
                         reads=[R(dn[d], 0, 1)], writes=[R(dn[d], 1, 2)])
                    k.op("dve", lambda d=d: nc.vector.tensor_scalar_max(dn[d][:, 1, :], dn[d][:, 1, :], 1.0),
                         reads=[R(dn[d], 1, 2)], writes=[R(dn[d], 1, 2)])
                    k.op("dve", lambda d=d: nc.vector.reciprocal(dn[d][:, 2, :], dn[d][:, 1, :]),
                         reads=[R(dn[d], 1, 2)], writes=[R(dn[d], 2, 3)])
                    k.op("dve", lambda d=d, inter=inter: nc.vector.tensor_tensor(dn[d][:, 3, :], dn[d][:, 2, :], inter, ALU.mult),
                         reads=[R(dn[d], 2, 3), R(GP, t, t + 1)], writes=[R(dn[d], 3, 4)])
                    if LVL < 5:
                        continue
                    num_ap = bass.AP(pnd[d][:, 0, 0:1].tensor, pnd[d][:, 0, 0:1].offset, [list(pnd[d][:, 0, 0:1].ap[0]), [512, 2], [130, 2], [1, 128]])
                    rec_b = bc_last(dn[d][:, 3, :].rearrange("p (a b) -> p a b", a=2), 128)
                    sf, sb_ = fwd_order.index(t), bwd_order.index(t)
                    first = (sf <= sb_) if d == 0 else (sb_ < sf)
                    if first:
                        k.op("dve", lambda t=t, num_ap=num_ap, rec_b=rec_b: nc.vector.tensor_tensor(
                            H[:, t, :].rearrange("p (a b e) -> p a b e", a=2, b=2), num_ap, rec_b, ALU.mult),
                            reads=[pnd[d], R(dn[d], 3, 4)], writes=[R(H, t, t + 1)])
                    else:
                        k.op("dve", lambda d=d, num_ap=num_ap, rec_b=rec_b: nc.vector.tensor_tensor(
                            hc[d][:].rearrange("p (a b e) -> p a b e", a=2, b=2), num_ap, rec_b, ALU.mult),
                            reads=[pnd[d], R(dn[d], 3, 4)], writes=[hc[d]])
                        k.op("pool", lambda d=d, t=t: nc.gpsimd.tensor_tensor(H[:, t, :], H[:, t, :], hc[d][:], ALU.add),
                             reads=[hc[d], R(H, t, t + 1)], writes=[R(H, t, t + 1)])
                    if LVL < 6:
                        continue
                    for h in (0, 2, 1, 3):
                        pb = (h % 2) * 64
                        o_ap = pdc[d][pb:pb + 64, h // 2, 0:129]
                        k.op("pe", lambda d=d, h=h, t=t, o_ap=o_ap, pb=pb: nc.tensor.matmul(
                            o_ap, Kt[:, t, h * 64:(h + 1) * 64], Vp[d][:, h, 0:129], start=True, stop=True,
                            tile_position=((0, pb) if pb else None)),
                            reads=[R(Kt, t, t + 1), Vp[d]], writes=[R(pdc[d], h, h + 1)], rg=("c", pb))
                    for hf in range(2):
                        pb = hf * 64
                        c0_ = d * 8 + hg * 4 + hf
                        eglh = GP[pb:pb + 64, t, 2, c0_:c0_ + 3:2]
                        k.op("dve", lambda d=d, pb=pb: nc.vector.tensor_tensor(
                            tmpC[d][pb:pb + 64, :, 0:129], pdc[d][pb:pb + 64, :, 0:129], C32[d][pb:pb + 64, :, 0:129], ALU.add),
                            reads=[pdc[d], C32[d]], writes=[R(tmpC[d], hf, hf + 1)])
                        k.op("pool", lambda d=d, pb=pb, eglh=eglh: nc.gpsimd.tensor_tensor(
                            C32[d][pb:pb + 64, :, 0:129], tmpC[d][pb:pb + 64, :, 0:129], bc_last(eglh, 129), ALU.mult),
                            reads=[R(tmpC[d], hf, hf + 1), R(GP, t, t + 1)], writes=[C32[d]])
                    k.op("act", lambda d=d: nc.scalar.copy(Cb[d][:], C32[d][:]), reads=[C32[d]], writes=[Cb[d]])
            k.barrier()
            if self.stop == "m_scan":
                self.dump("H", H, H[:], [128, NT, 512])
                self.dump("qkT", qkT, qkT[:], [128, 4, NTOK])
                self.dump("V", V, V[:], [128, NT, 4, 130])
                self.dump("Kt", Kt, Kt[:], [128, NT, 256])
                self.dump("GT", GT, GT[:], [128, NT, 32])
                self.dump("GP", GP, GP[:], [128, NT, 3, 16])
                self.dump("ST", ST[0], ST[0][:], [128, 4, 128])
                self.dump("C32", C32[0], C32[0][:], [128, 2, 130])
                self.dump("Vp", Vp[0], Vp[0][:], [128, 4, 130])
            self.chk("m_scan")
            k.release(m2)
            m2 = k.mark()
            po = [k.ps(f"mpo{j}", [128, 512], F32) for j in range(2)]
            py = [k.ps(f"mpy{j}", [128, D], F32) for j in range(2)]
            ptr = [k.ps(f"mptr{j}", [128, 8, 128], BF16) for j in range(2)]
            sg = [k.sb(f"msg{j}", [128, 512], BF16) for j in range(2)]
            ysb = [k.sb(f"mys{j}", [128, 512], BF16) for j in range(2)]
            yT = [k.sb(f"myT{j}", [128, 4, 128], BF16) for j in range(2)]
            xb = [k.sb(f"mxb{j}", [128, D], F32) for j in range(2)]
            tb1 = k.sb("mtb", [128, D], F32)
            tb = [tb1, tb1]
            otiles = list(range(16 if last else NT))

            def oA(n):
                t, b = otiles[n], n % 2
                self.proj_tok(po[b][:], po[b], t, wo, 0, 512)
                k.op("act", lambda: nc.scalar.activation(sg[b][:], po[b][:], AF.Sigmoid), reads=[po[b]], writes=[sg[b]])
                k.op("dve", lambda: nc.vector.tensor_tensor(ysb[b][:], H[:, t, :], sg[b][:], ALU.mult),
                     reads=[R(H, t, t + 1), sg[b]], writes=[ysb[b]])

            def oB(n):
                t, b = otiles[n], n % 2
                self.out_proj_tile(t, ysb[b], 4, wout, py[b], yT[b], ptr[b], None, None)

            def oC(n):
                t, b = otiles[n], n % 2
                self.resid_update(t, py[b], xb[b], tb[b])

            self.swpipe(len(otiles), [oA, oB, oC])
            k.barrier()
            k.release(m2)
            k.release(m1)
        k.release(m0)

    def proj_feat(self, p_ap, pbuf, w, c0, m, cs, ce):
        k, nc = self.k, self.nc
        for kc in range(8):
            k.op("pe", lambda kc=kc: nc.tensor.matmul(p_ap, w[:, kc, c0:c0 + m], self.hT[:, kc, cs:ce],
                                                      start=(kc == 0), stop=(kc == 7)),
                 reads=[w, R(self.hT, cs, ce)], writes=[pbuf])

    def gla(self, i, jm, last):
        k, nc = self.k, self.nc
        m0 = k.mark()
        W = self.b_w_in.ap()[jm]
        fwd_order = [16, 17] + list(range(16))
        bwd_order = [17, 16] + list(range(15, -1, -1))
        blocks = [(0, 512), (512, 1024), (1024, 1536), (1536, 2048), (2048, 2304)]
        gnb = k.sb("ggn", [128, 256], F32)
        self.bcast_row_dma(gnb, self.b_norm.ap()[jm:jm + 1, :], 256)
        wa2 = k.sb("gwa2", [17, 2, 512], F32)
        k.dma("sp", wa2[:], self.b_w_a2.ap()[jm].rearrange("d r c -> r d c"), writes=[wa2])
        for hg in range(2):
            m1 = k.mark()
            qT = k.sb("gqT", [128, 2, NTOK], BF16)
            kT = k.sb("gkT", [128, 2, NTOK], BF16)
            V = k.sb("gV", [128, NT, 512], BF16)
            SP = k.sb("gSP", [128, NT, 2, 256], F32)
            O = k.sb("gO", [128, NT, 512], BF16)
            m2 = k.mark()
            wqk = k.sb("gwqk", [128, 8, 512], BF16)
            wv = k.sb("gwv", [128, 8, 512], BF16)
            wa = k.sb("gwa", [128, 8, 32], BF16)
            aT = [k.sb(f"gaT{d}", [17, NTOK], F32) for d in range(2)]
            self.load_w(wqk, wqk[:, :, 0:256], W[:, hg * 256:(hg + 1) * 256], rng=(0, 256))
            self.load_w(wqk, wqk[:, :, 256:512], W[:, 512 + hg * 256:512 + (hg + 1) * 256], rng=(256, 512))
            self.load_w(wv, wv[:], W[:, 1024 + hg * 512:1024 + (hg + 1) * 512])
            self.load_w(wa, wa[:], W[:, 3072:3104])
            pq = [k.ps(f"gpq{j}", [128, 512], F32) for j in range(2)]
            pa_ = [k.ps(f"gpa{j}", [128, 512], F32) for j in range(2)]
            pz = [k.ps(f"gpz{j}", [128, 512], F32) for j in range(2)]
            e1 = [k.sb(f"ge1{j}", [128, 512], F32) for j in range(2)]
            for d in range(2):
                k.op("dve", lambda d=d: nc.vector.memset(aT[d][:], 1.0), writes=[aT[d]])
            cnt = 0
            for (cs, ce) in blocks:
                n = ce - cs
                for ch in range(4):
                    P = pq[cnt % 2]
                    cnt += 1
                    self.proj_feat(P[:, 0:n], P, wqk, ch * 128, 128, cs, ce)
                    if ch < 2:
                        k.op("act", lambda P=P, ch=ch: nc.scalar.activation(qT[:, ch, cs:ce], P[:, 0:n], AF.Copy, scale=128.0 ** -0.5),
                             reads=[P], writes=[R(qT, cs, ce)])
                    else:
                        k.op("dve", lambda P=P, ch=ch: nc.vector.tensor_copy(kT[:, ch - 2, cs:ce], P[:, 0:n]),
                             reads=[P], writes=[R(kT, cs, ce)])
                for d in range(2):
                    P = pa_[d]
                    self.proj_feat(P[0:16, 0:n], P, wa, d * 16, 16, cs, ce)
                    k.op("act", lambda P=P, d=d: nc.scalar.copy(aT[d][0:16, cs:ce], P[0:16, 0:n]), reads=[P], writes=[R(aT[d], cs, ce)])
            for t in range(NT):
                P = pq[t % 2]
                self.proj_tok(P[:], P, t, wv, 0, 512)
                k.op("act", lambda P=P, t=t: nc.scalar.copy(V[:, t, :], P[:]), reads=[P], writes=[R(V, t, t + 1)])
                for d in range(2):
                    Z = pz[d]
                    k.op("pe", lambda Z=Z, d=d, t=t: nc.tensor.matmul(Z[:, 0:256], aT[d][0:17, t * 128:(t + 1) * 128],
                                                                   wa2[0:17, d, hg * 256:(hg + 1) * 256], start=True, stop=True),
                         reads=[R(aT[d], t * 128, (t + 1) * 128), wa2], writes=[Z])
                    E = e1[d]
                    k.op("act", lambda Z=Z, E=E: nc.scalar.activation(E[:, 0:256], Z[:, 0:256], AF.Exp, scale=-1.0), reads=[Z], writes=[E])
                    k.op("act", lambda E=E: nc.scalar.activation(E[:, 256:512], E[:, 0:256], AF.Ln, bias=1.0), reads=[E], writes=[E])
                    k.op("dve", lambda E=E, d=d, t=t: nc.vector.tensor_scalar(SP[:, t, d, :], E[:, 256:512], 1.0 / 16.0, None, ALU.mult),
                         reads=[E], writes=[R(SP, t, t + 1)])
            k.barrier()
            k.release(m2)
            m2 = k.mark()
            S32 = [k.sb(f"gS32{d}", [128, 2, 256], F32) for d in range(2)]
            tS = [k.sb(f"gtS{d}", [128, 2, 256], F32) for d in range(2)]
            Sb = [k.sb(f"gSb{d}", [128, 2, 256], BF16) for d in range(2)]
            EG = [k.sb(f"gEG{d}", [128, 2, 128], F32) for d in range(2)]
            EN = [k.sb(f"gEN{d}", [128, 2, 128], F32) for d in range(2)]
            qt = [k.sb(f"gqt{d}", [128, 2, 128], BF16) for d in range(2)]
            kt = [k.sb(f"gkt{d}", [128, 2, 128], BF16) for d in range(2)]
            AT = [k.sb(f"gAT{d}", [128, 2, 128], BF16) for d in range(2)]
            Ktok = [k.sb(f"gKtok{d}", [128, 2, 128], BF16) for d in range(2)]
            pP = [k.ps(f"gpP{d}", [128, 4, 128], F32) for d in range(2)]
            pO = [k.ps(f"gpO{d}", [128, 2, 256], F32) for d in range(2)]
            pS = [k.ps(f"gpS{d}", [128, 2, 256], F32) for d in range(2)]
            pA = k.ps("gpA", [128, 4, 128], F32)
            pKt = k.ps("gpKt", [128, 8, 128], BF16)
            for d in range(2):
                k.op("dve", lambda d=d: nc.vector.memset(S32[d][:], 0.0), writes=[S32[d]])
                k.op("dve", lambda d=d: nc.vector.memset(Sb[d][:], 0.0), writes=[Sb[d]])
            for step in range(NT):
                for d in range(2):
                    t = (fwd_order, bwd_order)[d][step]
                    tri = self.cf[:, 1 + d, :]
                    mask = self.cf[:, 4 + d, :]
                    lastc = 127 if d == 0 else 0
                    tk = slice(t * 128, (t + 1) * 128)
                    for h in range(2):
                        k.op("pe", lambda d=d, h=h, t=t, tri=tri: nc.tensor.matmul(
                            pP[d][:, h, :], SP[:, t, d, h * 128:(h + 1) * 128], tri, start=True, stop=True),
                            reads=[R(SP, t, t + 1), self.cf], writes=[R(pP[d], h, h + 1)])
                    k.op("act", lambda d=d: nc.scalar.activation(EG[d][:], pP[d][:, 0:2, :], AF.Exp, scale=-1.0), reads=[pP[d]], writes=[EG[d]])
                    k.op("act", lambda d=d: nc.scalar.activation(EN[d][:], pP[d][:, 0:2, :], AF.Exp), reads=[pP[d]], writes=[EN[d]])
                    k.op("dve", lambda d=d, tk=tk: nc.vector.tensor_tensor(qt[d][:], qT[:, :, tk], EG[d][:], ALU.mult),
                         reads=[R(qT, t * 128, (t + 1) * 128), EG[d]], writes=[qt[d]])
                    k.op("pool", lambda d=d, tk=tk: nc.gpsimd.tensor_tensor(kt[d][:], kT[:, :, tk], EN[d][:], ALU.mult),
                         reads=[R(kT, t * 128, (t + 1) * 128), EN[d]], writes=[kt[d]])
                    for h in range(2):
                        k.op("pe", lambda d=d, h=h: nc.tensor.matmul(pA[:, h, :], kt[d][:, h, :], qt[d][:, h, :], start=True, stop=True),
                             reads=[kt[d], qt[d]], writes=[R(pA, h, h + 1)])
                    for h in range(2):
                        k.op("pe", lambda d=d, h=h: nc.tensor.transpose(pKt[:, h, :], kt[d][:, h, :], self.ident_b),
                             reads=[kt[d], self.cb], writes=[R(pKt, h, h + 1)])
                    k.op("dve", lambda d=d, mask=mask: nc.vector.tensor_tensor(AT[d][:], pA[:, 0:2, :], bc_mid(mask, 1, 2), ALU.mult),
                         reads=[pA, self.cf], writes=[AT[d]])
                    k.op("act", lambda d=d: nc.scalar.copy(Ktok[d][:], pKt[:, 0:2, :]), reads=[pKt], writes=[Ktok[d]])
                    for h in range(2):
                        k.op("pe", lambda d=d, h=h, t=t: nc.tensor.matmul(pO[d][:, h, :], AT[d][:, h, :], V[:, t, h * 256:(h + 1) * 256],
                                                                      start=True, stop=False),
                             reads=[AT[d], R(V, t, t + 1)], writes=[R(pO[d], h, h + 1)])
                        k.op("pe", lambda d=d, h=h: nc.tensor.matmul(pO[d][:, h, :], qt[d][:, h, :], Sb[d][:, h, :], start=False, stop=True),
                             reads=[qt[d], Sb[d]], writes=[R(pO[d], h, h + 1)])
                    for h in range(2):
                        k.op("pe", lambda d=d, h=h, t=t: nc.tensor.matmul(pS[d][:, h, :], Ktok[d][:, h, :], V[:, t, h * 256:(h + 1) * 256],
                                                                      start=True, stop=True),
                             reads=[Ktok[d], R(V, t, t + 1)], writes=[R(pS[d], h, h + 1)])
                    sf, sb_ = fwd_order.index(t), bwd_order.index(t)
                    first = (sf <= sb_) if d == 0 else (sb_ < sf)
                    if first:
                        k.op("dve", lambda d=d, t=t: nc.vector.tensor_copy(O[:, t, :], pO[d][:].rearrange("p a b -> p (a b)")),
                             reads=[pO[d]], writes=[R(O, t, t + 1)])
                    else:
                        k.op("dve", lambda d=d, t=t: nc.vector.tensor_tensor(O[:, t, :], pO[d][:].rearrange("p a b -> p (a b)"), O[:, t, :], ALU.add),
                             reads=[pO[d], R(O, t, t + 1)], writes=[R(O, t, t + 1)])
                    k.op("dve", lambda d=d: nc.vector.tensor_tensor(tS[d][:], pS[d][:], S32[d][:], ALU.add),
                         reads=[pS[d], S32[d]], writes=[tS[d]])
                    k.op("pool", lambda d=d, lastc=lastc: nc.gpsimd.tensor_tensor(S32[d][:], tS[d][:], bc_last(EG[d][:, :, lastc], 256), ALU.mult),
                         reads=[tS[d], EG[d]], writes=[S32[d]])
                    k.op("act", lambda d=d: nc.scalar.copy(Sb[d][:], S32[d][:]), reads=[S32[d]], writes=[Sb[d]])
            k.barrier()
            if self.stop == "g_scan":
                self.dump("O", O, O[:], [128, NT, 512])
            self.chk("g_scan")
            k.release(m2)
            m2 = k.mark()
            wr = k.sb("gwr", [128, 8, 512], BF16)
            wout = k.sb("gwout", [128, 4, D], BF16)
            self.load_w(wr, wr[:], W[:, 2048 + hg * 512:2048 + (hg + 1) * 512])
            self.load_w(wout, wout[:], self.b_w_out.ap()[jm, hg * 512:(hg + 1) * 512, :])
            pr = [k.ps(f"gpr{j}", [128, 512], F32) for j in range(2)]
            py = [k.ps(f"gpy{j}", [128, D], F32) for j in range(2)]
            ptr = [k.ps(f"gptr{j}", [128, 8, 128], BF16) for j in range(2)]
            sr = [k.sb(f"gsr{j}", [128, 512], BF16) for j in range(2)]
            y32 = [k.sb(f"gy32{j}", [128, 2, 256], F32) for j in range(2)]
            ysb = [k.sb(f"gys{j}", [128, 512], BF16) for j in range(2)]
            yT = [k.sb(f"gyT{j}", [128, 4, 128], BF16) for j in range(2)]
            xb = [k.sb(f"gxb{j}", [128, D], F32) for j in range(2)]
            tb1 = k.sb("gtb", [128, D], F32)
            st = [k.sb(f"gst{j}", [128, 8], F32) for j in range(2)]
            junk = k.sb("gjunk", [128, 256], BF16)
            otiles = list(range(16 if last else NT))

            def oA(n):
                t, b = otiles[n], n % 2
                self.proj_tok(pr[b][:], pr[b], t, wr, 0, 512)
                k.op("act", lambda: nc.scalar.activation(sr[b][:], pr[b][:], AF.Silu), reads=[pr[b]], writes=[sr[b]])
                k.op("pool", lambda: nc.gpsimd.memset(st[b][:], 0.0), writes=[st[b]])
                for h in range(2):
                    k.op("act", lambda h=h: nc.scalar.activation(junk[:], O[:, t, h * 256:(h + 1) * 256], AF.Square, accum_out=st[b][:, h:h + 1]),
                         reads=[R(O, t, t + 1), st[b]], writes=[junk, st[b]])
                k.op("act", lambda: nc.scalar.activation(st[b][:, 2:4], st[b][:, 0:2], AF.Ln, scale=1.0 / 256, bias=EPS), reads=[st[b]], writes=[st[b]])
                k.op("act", lambda: nc.scalar.activation(st[b][:, 4:6], st[b][:, 2:4], AF.Exp, scale=-0.5), reads=[st[b]], writes=[st[b]])
                k.op("dve", lambda: nc.vector.tensor_tensor(y32[b][:], O[:, t, :].rearrange("p (a e) -> p a e", a=2),
                                                           bc_last(st[b][:, 4:6], 256), ALU.mult),
                     reads=[R(O, t, t + 1), st[b]], writes=[y32[b]])
                k.op("pool", lambda: nc.gpsimd.tensor_tensor(y32[b][:], y32[b][:], bc_mid(gnb[:], 1, 2), ALU.mult),
                     reads=[y32[b], gnb], writes=[y32[b]])
                k.op("dve", lambda: nc.vector.tensor_tensor(ysb[b][:], y32[b][:].rearrange("p a e -> p (a e)"), sr[b][:], ALU.mult),
                     reads=[y32[b], sr[b]], writes=[ysb[b]])

            def oB(n):
                t, b = otiles[n], n % 2
                self.out_proj_tile(t, ysb[b], 4, wout, py[b], yT[b], ptr[b], None, None)

            def oC(n):
                t, b = otiles[n], n % 2
                self.resid_update(t, py[b], xb[b], tb1)

            self.swpipe(len(otiles), [oA, oB, oC])
            k.barrier()
            k.release(m2)
            k.release(m1)
        k.release(m0)

    def na(self, i, jm, last):
        k, nc = self.k, self.nc
        m0 = k.mark()
        W = self.c_w_in.ap()[jm]
        blocks = [(0, 512), (512, 1024), (1024, 1536), (1536, 2048), (2048, 2304)]
        cmask = k.sb("ncm", [128, 64], F32)
        k.dma("sp", cmask[:], self.cmask_in.ap(), writes=[cmask])
        for hg in range(2):
            m1 = k.mark()
            qT = k.sb("nqT", [128, 4, NTOK], BF16)
            kT = k.sb("nkT", [128, 4, NTOK], BF16)
            V = k.sb("nV", [128, NT, 512], BF16)
            BM = k.sb("nBM", [128, 8, 15, 64], BF16)
            src = self.c_rpbx.ap()[jm, hg * 8:(hg + 1) * 8].rearrange("h q (r c) -> q h r c", r=15)
            k.dma("pool", BM[0:64], src, writes=[R(BM, 0, 1)])
            k.dma("pool", BM[64:128], src, writes=[R(BM, 1, 2)])
            k.op("dve", lambda: nc.vector.tensor_tensor(BM[:].rearrange("p h r c -> p (h r) c"), BM[:].rearrange("p h r c -> p (h r) c"),
                                                       bc_mid(cmask[:], 1, 120), ALU.add), reads=[BM, cmask], writes=[BM])
            m2 = k.mark()
            wq = k.sb("nwq", [128, 8, 512], BF16)
            wk = k.sb("nwk", [128, 8, 512], BF16)
            wv = k.sb("nwv", [128, 8, 512], BF16)
            self.load_w(wq, wq[:], W[:, hg * 512:(hg + 1) * 512])
            self.load_w(wk, wk[:], W[:, D + hg * 512:D + (hg + 1) * 512])
            self.load_w(wv, wv[:], W[:, 2 * D + hg * 512:2 * D + (hg + 1) * 512])
            pq = [k.ps(f"npq{j}", [128, 512], F32) for j in range(2)]
            cnt = 0
            for (cs, ce) in blocks:
                n = ce - cs
                for ch in range(8):
                    P = pq[cnt % 2]
                    cnt += 1
                    self.proj_feat(P[:, 0:n], P, wq if ch < 4 else wk, (ch % 4) * 128, 128, cs, ce)
                    if ch < 4:
                        k.op("act", lambda P=P, ch=ch: nc.scalar.activation(qT[:, ch, cs:ce], P[:, 0:n], AF.Copy, scale=0.125),
                             reads=[P], writes=[R(qT, cs, ce)])
                    else:
                        k.op("dve", lambda P=P, ch=ch: nc.vector.tensor_copy(kT[:, ch - 4, cs:ce], P[:, 0:n]),
                             reads=[P], writes=[R(kT, cs, ce)])
            for t in range(NT):
                P = pq[t % 2]
                self.proj_tok(P[:], P, t, wv, 0, 512)
                k.op("act", lambda P=P, t=t: nc.scalar.copy(V[:, t, :], P[:]), reads=[P], writes=[R(V, t, t + 1)])
            k.barrier()
            k.release(m2)
            m2 = k.mark()
            wout = k.sb("nwout", [128, 4, D], BF16)
            self.load_w(wout, wout[:], self.c_w_out.ap()[jm, hg * 512:(hg + 1) * 512, :])
            Sc = [k.sb(f"nSc{j}", [128, 896], F32) for j in range(2)]
            Pb = [k.sb(f"nPb{j}", [128, 896], BF16) for j in range(2)]
            PT = [k.sb(f"nPT{j}", [128, 7, 128], BF16) for j in range(2)]
            pS = [k.ps(f"npS{j}", [128, 1024], F32) for j in range(2)]
            pT = [k.ps(f"npT{j}", [128, 8, 128], BF16) for j in range(1)]
            pO = k.ps("npO", [128, 8, 64], F32)
            py = [k.ps(f"npy{j}", [128, D], F32) for j in range(1)]
            ptr = pT
            ysb = [k.sb(f"nys{j}", [128, 512], BF16) for j in range(2)]
            yT = [k.sb(f"nyT{j}", [128, 4, 128], BF16) for j in range(2)]
            xb = [k.sb(f"nxb{j}", [128, D], F32) for j in range(2)]
            tb1 = k.sb("ntb", [128, D], F32)
            tiles_ = list(range(16 if last else NT))
            units = [(n_, t, h) for n_, t in enumerate(tiles_) for h in range(8)]
            st = [k.sb(f"nst4{j}", [128, 4], F32) for j in range(4)]
            Pb = Pb + [k.sb("nPb2", [128, 896], BF16)]

            def geo(t):
                if t < 16:
                    rs = [min(max(2 * t - 4, 0), 24), min(max(2 * t - 3, 0), 24)]
                    nl = 4 if t in (0, 1, 14, 15) else 5
                    return rs, rs[0], nl, 0, list(range(nl)) + [5, 6]
                return None, 0, 0, 640, [5, 6]

            def stA(u, part=7):
                n_, t, h = units[u]
                rs, ks, nl, c_lo, chunks = geo(t)
                if h == 0 and nl and (part & 2):
                    for sb_ in Sc:
                        for hf in range(2):
                            i0 = rs[hf] - ks
                            if i0 > 0:
                                k.op("dve", lambda sb_=sb_, hf=hf, i0=i0: nc.vector.memset(sb_[hf * 64:(hf + 1) * 64, 0:i0 * 64], NEG), writes=[R(sb_, 0, 640)])
                            if (i0 + 8) * 64 < 640:
                                k.op("dve", lambda sb_=sb_, hf=hf, i0=i0: nc.vector.memset(sb_[hf * 64:(hf + 1) * 64, (i0 + 8) * 64:640], NEG), writes=[R(sb_, 0, 640)])
                pb = (h % 2) * 64
                c = h // 2
                S_, P_, s_, ps_ = Sc[u % 2], Pb[u % 3], st[u % 4], pS[u % 2]
                qa = qT[pb:pb + 64, c, t * 128:(t + 1) * 128]
                if nl and (part & 1):
                    k0 = ks * 64
                    k.op("pe", lambda: nc.tensor.matmul(ps_[:, 0:512], qa, kT[pb:pb + 64, c, k0:k0 + 512], start=True, stop=True),
                         reads=[R(qT, t * 128, (t + 1) * 128), R(kT, k0, k0 + 512)], writes=[R(ps_, 0, 512)], rg=pb)
                    if nl == 5:
                        k.op("pe", lambda: nc.tensor.matmul(ps_[:, 512:640], qa, kT[pb:pb + 64, c, k0 + 512:k0 + 640], start=True, stop=True),
                             reads=[R(qT, t * 128, (t + 1) * 128), R(kT, k0 + 512, k0 + 640)], writes=[R(ps_, 512, 640)], rg=pb)
                if part & 1:
                    k.op("pe", lambda: nc.tensor.matmul(ps_[:, 640:896], qa, kT[pb:pb + 64, c, T:NTOK], start=True, stop=True),
                         reads=[R(qT, t * 128, (t + 1) * 128), R(kT, T, NTOK)], writes=[R(ps_, 640, 896)], rg=pb)
                if part & 4:
                    k.op("act", lambda: nc.scalar.copy(S_[:, 640:896], ps_[:, 640:896]), reads=[R(ps_, 640, 896)], writes=[R(S_, 640, 896)])
                if not (part & 2):
                    return
                if nl:
                    for hf in range(2):
                        r = 2 * t + hf
                        i0 = rs[hf] - ks
                        dr0 = rs[hf] - r + 7
                        k.op("dve", lambda hf=hf, i0=i0, dr0=dr0: nc.vector.tensor_tensor(
                            S_[hf * 64:(hf + 1) * 64, i0 * 64:(i0 + 8) * 64], ps_[hf * 64:(hf + 1) * 64, i0 * 64:(i0 + 8) * 64],
                            BM[hf * 64:(hf + 1) * 64, h, dr0:dr0 + 8, :].rearrange("p r c -> p (r c)"), ALU.add),
                            reads=[R(ps_, 0, 640), BM], writes=[R(S_, 0, 640)])
                k.op("pool", lambda: nc.gpsimd.memset(s_[:], 0.0), writes=[s_])
                k.op("dve", lambda: nc.vector.tensor_reduce(s_[:, 0:1], S_[:, c_lo:896], AX.X, ALU.max, negate=True), reads=[S_], writes=[s_])
                k.op("act", lambda: nc.scalar.activation(P_[:, c_lo:896], S_[:, c_lo:896], AF.Exp, bias=s_[:, 0:1], accum_out=s_[:, 1:2]),
                     reads=[S_, s_], writes=[P_, s_])

            def stB(u):
                n_, t, h = units[u]
                rs, ks, nl, c_lo, chunks = geo(t)
                P_, PT_, pT_ = Pb[u % 3], PT[u % 2], pT[0]
                for cc in chunks:
                    k.op("pe", lambda cc=cc: nc.tensor.transpose(pT_[:, cc, :], P_[:, cc * 128:(cc + 1) * 128], self.ident_b),
                         reads=[P_, self.cb], writes=[R(pT_, cc, cc + 1)])
                if nl:
                    k.op("act", lambda: nc.scalar.copy(PT_[:, 0:nl, :], pT_[:, 0:nl, :]), reads=[pT_], writes=[PT_])
                k.op(("act" if os.environ.get("PT_ACT") else "dve"), lambda: (nc.scalar.copy(PT_[:, 5:7, :], pT_[:, 5:7, :]) if os.environ.get("PT_ACT") else nc.vector.tensor_copy(PT_[:, 5:7, :], pT_[:, 5:7, :])), reads=[pT_], writes=[PT_])

            def stC(u):
                n_, t, h = units[u]
                rs, ks, nl, c_lo, chunks = geo(t)
                PT_, s_ = PT[u % 2], st[u % 4]
                for n2, cc in enumerate(chunks):
                    vt = (ks // 2 + cc) if cc < 5 else (16 + cc - 5)
                    k.op("pe", lambda cc=cc, vt=vt, n2=n2: nc.tensor.matmul(
                        pO[:, h, :], PT_[:, cc, :], V[:, vt, h * 64:(h + 1) * 64], start=(n2 == 0), stop=(n2 == len(chunks) - 1)),
                        reads=[PT_, R(V, vt, vt + 1)], writes=[R(pO, h, h + 1)])
                k.op("dve", lambda: nc.vector.reciprocal(s_[:, 2:3], s_[:, 1:2]), reads=[s_], writes=[s_])
                yb = ysb[n_ % 2]
                k.op("dve", lambda: nc.vector.tensor_scalar(yb[:, h * 64:(h + 1) * 64], pO[:, h, :], s_[:, 2:3], None, ALU.mult),
                     reads=[R(pO, h, h + 1), s_], writes=[yb])
                if h == 7:
                    self.out_proj_tile(t, ysb[n_ % 2], 4, wout, py[0], yT[n_ % 2], ptr[0], xb[n_ % 2], tb1)

            mode = os.environ.get("NA_PIPE", "5")
            if mode == "1":
                self.swpipe(len(units), [lambda u: (stA(u), stB(u), stC(u))])
            elif mode == "2":
                self.swpipe(len(units), [stA, lambda u: (stB(u), stC(u))])
            elif mode == "6":
                n_u = len(units)
                for i_ in range(n_u + 2):
                    if i_ < n_u:
                        stA(i_, 1 | 4)
                    if 0 <= i_ - 2 < n_u:
                        stC(i_ - 2)
                    if 0 <= i_ - 1 < n_u:
                        stB(i_ - 1)
                    if i_ < n_u:
                        stA(i_, 2)
            elif mode == "5":
                n_u = len(units)
                for i_ in range(n_u + 2):
                    if i_ < n_u:
                        stA(i_, 1)
                    if 0 <= i_ - 2 < n_u:
                        stC(i_ - 2)
                    if 0 <= i_ - 1 < n_u:
                        stB(i_ - 1)
                    if i_ < n_u:
                        stA(i_, 6)
            elif mode == "4":
                n_u = len(units)
                for i_ in range(n_u + 2):
                    if 0 <= i_ - 2 < n_u:
                        stC(i_ - 2)
                    if 0 <= i_ - 1 < n_u:
                        stB(i_ - 1)
                    if i_ < n_u:
                        stA(i_)
            else:
                self.swpipe(len(units), [stA, stB, stC])
            k.barrier()
            k.release(m2)
            k.release(m1)
        k.release(m0)

    def final_norm(self):
        k, nc = self.k, self.nc
        m = k.mark()
        gn = k.sb("fng", [128, D], F32)
        self.bcast_row_dma(gn, self.norm_final.ap()[0:1, :], D)
        xt = [k.sb(f"fx{j}", [128, D], F32) for j in range(2)]
        yt = [k.sb(f"fy{j}", [128, D], F32) for j in range(2)]
        junk = k.sb("fjunk", [128, D], BF16)
        st = [k.sb(f"fst{j}", [128, 4], F32) for j in range(2)]
        for t in range(16):
            x, y, s = xt[t % 2], yt[t % 2], st[t % 2]
            k.dma("sp", x[:], self.xt_ap(t), reads=[R(self.xres, t, t + 1)], writes=[x])
            k.op("dve", lambda s=s: nc.vector.memset(s[:], 0.0), writes=[s])
            k.op("act", lambda x=x, s=s: nc.scalar.activation(junk[:], x[:], AF.Square, accum_out=s[:, 0:1]),
                 reads=[x, s], writes=[junk, s])
            k.op("act", lambda s=s: nc.scalar.activation(s[:, 1:2], s[:, 0:1], AF.Ln, scale=1.0 / D, bias=EPS), reads=[s], writes=[s])
            k.op("act", lambda s=s: nc.scalar.activation(s[:, 2:3], s[:, 1:2], AF.Exp, scale=-0.5), reads=[s], writes=[s])
            k.op("dve", lambda x=x, y=y, s=s: nc.vector.scalar_tensor_tensor(y[:], x[:], s[:, 2:3], gn[:], ALU.mult, ALU.mult),
                 reads=[x, s, gn], writes=[y])
            k.dma("sp", self.out.ap()[t * 128:(t + 1) * 128, :], y[:], reads=[y], writes=[R(self.out, t, t + 1)])
        k.barrier()
        k.release(m)

    def build(self):
        k = self.k
        self.decl_inputs()
        self.setup_consts()
        for t in range(16):
            k.dma("sp", self.xres.ap()[t * 128:(t + 1) * 128, :], self.x_in.ap()[t * 128:(t + 1) * 128, :], writes=[R(self.xres, t, t + 1)])
        for t in range(2):
            k.dma("sp", self.xres.ap()[T + t * 128:T + (t + 1) * 128, :], self.ctx_in.ap()[t * 128:(t + 1) * 128, :], writes=[R(self.xres, 16 + t, 17 + t)])
        try:
            self.layers()
        except StopIteration:
            pass
        k.finish()
        k.close()

    def dump(self, name, buf, ap, shape):
        if not self.debug_x:
            return
        o = self.k.dram("dbg_" + name, list(shape), F32, kind="ExternalOutput")
        self.k.dma("pool", o.ap(), ap, reads=[buf], writes=[o])

    def chk(self, name):
        if self.stop == name:
            if name == "norm":
                self.dump("hT", self.hT, self.hT[:], [128, 8, NTOK])
                self.dump("mods", self.mods, self.mods[:], [128, 6, D])
            raise StopIteration

    def layers(self):
        self.chk("copy")
        for i in self.layer_list:
            last = i == DEPTH - 1
            kind, j = i % 3, i // 3
            tiles = list(range(NT))
            self.ada(i, 0)
            self.chk("ada")
            self.norm_to_hT(tiles, self.xt_ap)
            self.chk("norm")
            if kind == 0:
                self.mlstm(i, j, last)
            elif kind == 1:
                self.gla(i, j, last)
            else:
                self.na(i, j, last)
            self.chk("mixer")
            self.ada(i, 1)
            self.norm_to_hT(list(range(16)) if last else tiles, self.xt_ap)
            self.ffn(i, last)
            self.chk("ffn")
        self.final_norm()


def _consts():
    i = np.arange(128)
    ident = np.eye(128, dtype=np.float32)
    triu = (i[:, None] <= i[None, :]).astype(np.float32)
    tril = (i[:, None] >= i[None, :]).astype(np.float32)
    ones = np.ones((128, 128), np.float32)
    return np.stack([ident, triu, tril, ones, triu, tril]).astype(np.float32)


def prep_inputs(inp):
    f = lambda a: np.ascontiguousarray(np.asarray(a, dtype=np.float32))
    shared = {}
    for n in ("w_ada", "b_ada", "norm_mix", "norm_ffn", "w_up", "w_down", "a_w_in", "a_b_gate", "a_w_out",
              "b_w_in", "b_norm", "b_w_out", "c_w_in", "c_w_out"):
        shared[n] = f(inp[n])
    shared["norm_final"] = f(inp["norm_final"]).reshape(1, D)
    cw = np.concatenate([f(inp["ffn_conv_w"]), f(inp["ffn_conv_b"])[:, None, :]], axis=1)
    shared["ffn_cw"] = f(cw.reshape(DEPTH, 4, 44, 128).transpose(0, 3, 1, 2).reshape(DEPTH, 128, 4 * 44))
    acw = np.concatenate([f(inp["a_conv_w"]), f(inp["a_conv_b"])[:, None, :]], axis=1)
    shared["a_cw"] = f(acw.reshape(2, 4, 8, 128).transpose(0, 3, 1, 2).reshape(2, 128, 32))
    shared["b_w_a2e"] = f(np.concatenate([f(inp["b_w_a2"]), f(inp["b_b_a"])[:, :, None, :]], axis=2))
    qc = np.arange(64)
    dc = np.clip(qc[None, :] - qc[:, None] + 15, 0, 30)
    rp = f(inp["c_rpb"])
    rx = rp[:, :, :, dc]
    shared["c_rpbx"] = f(rx.transpose(0, 1, 3, 2, 4).reshape(1, 16, 64, 15 * 64))
    cst = np.clip(qc - 8, 0, 48)
    cm = np.where((qc[None, :] >= cst[:, None]) & (qc[None, :] < cst[:, None] + 16), 0.0, NEG).astype(np.float32)
    shared["cmask"] = f(np.concatenate([cm, cm], axis=0))
    shared["consts"] = _consts()
    cctx_t = f(np.asarray(inp["c_ctx"]).reshape(8, 128).T)
    maps = []
    for b in range(8):
        mdict = dict(shared)
        mdict["x"] = f(inp["x"][b])
        mdict["ctx"] = f(inp["ctx"][b])
        mdict["c_t"] = f(np.asarray(inp["c"][b]).reshape(8, 128).T)
        mdict["cctx_t"] = cctx_t
        maps.append(mdict)
    return maps


_CACHE = {}


def kernel(**inputs):
    if "mk" not in _CACHE:
        _CACHE["mk"] = MK()
    mk = _CACHE["mk"]
    maps = prep_inputs(inputs)
    res = run_bass_kernel_spmd(mk.nc, maps, core_ids=list(range(8)))
    return np.stack([np.asarray(r["out"], dtype=np.float32) for r in res.results], axis=0)
```
